# Optimizing a Trainium2 kernel written in Bass

```python
import jax, jax.numpy as jnp
from jax import lax
import numpy as np

D_MODEL = 1024
BATCH = 4
SEQ = 4096
DEPTH = 1

D_FF = 2816
ROPE_THETA = 10000.0
NORM_EPS = 1e-6
Q_BLOCK = 128
H_A = 8
Q_LORA = 384
KV_LORA = 256
D_NOPE = 64
D_ROPE_A = 32
D_V_A = 64
H_IDX = 8
D_IDX = 32
TOPK_MAX = 256
DIL_PAIRS = ((128, 1), (512, 4), (2048, 16))
N_GROUPS_B = 3
H_B = 4
D_HEAD_B = 64
N_MOD = 9
COLS = (Q_LORA, KV_LORA, D_ROPE_A, D_IDX, H_IDX, 3 * N_GROUPS_B * H_B * D_HEAD_B, 2 * D_MODEL)
D_IN = sum(COLS)
WIDTH_A = H_A * D_V_A
WIDTH_B = H_B * D_HEAD_B

kernel_name = "hybrid_dsa_dilated_macaron_block"


def rms_norm(x, g):
    xf = x.astype(jnp.float32)
    y = xf * lax.rsqrt(jnp.mean(xf * xf, axis=-1, keepdims=True) + NORM_EPS)
    return (y * g.astype(jnp.float32)).astype(x.dtype)


def rope(x, pos):
    d = x.shape[-1]
    half = d // 2
    inv = ROPE_THETA ** (-jnp.arange(half, dtype=jnp.float32) / half)
    ang = pos.astype(jnp.float32)[..., None] * inv
    ang = ang.reshape(ang.shape[:2] + (1,) * (x.ndim - 3) + (half,))
    cos, sin = jnp.cos(ang), jnp.sin(ang)
    xf = x.astype(jnp.float32)
    x1, x2 = xf[..., :half], xf[..., half:]
    return jnp.concatenate([x1 * cos - x2 * sin, x2 * cos + x1 * sin], axis=-1).astype(x.dtype)


def swiglu(h, w_gate, w_up, w_down):
    return (jax.nn.silu(h @ w_gate) * (h @ w_up)) @ w_down


def dsa_branch(c_q, c_kv, k_rope, k_idx, w_idx, pos, w_uq, w_uk, w_uv, w_iq):
    B, S, _ = c_q.shape
    topk = min(TOPK_MAX, S // 4)
    q = jnp.einsum('bsr,rhe->bshe', c_q, w_uq)
    q_nope, q_rope = q[..., :D_NOPE], rope(q[..., D_NOPE:], pos)
    k_rope = rope(k_rope[:, :, None, :], pos)[:, :, 0]
    q_lat = jnp.einsum('bshn,rhn->bshr', q_nope, w_uk)
    q_idx = rope(jnp.einsum('bsr,rhe->bshe', c_q, w_iq), pos)
    k_idx = rope(k_idx[:, :, None, :], pos)[:, :, 0]
    w_idx = w_idx * (H_IDX ** -0.5)
    attn_scale = (D_NOPE + D_ROPE_A) ** -0.5
    idx_scale = D_IDX ** -0.5
    key_pos = jnp.arange(S)
    gather = jax.vmap(lambda a, i: a[i])

    def block(i):
        t0 = i * Q_BLOCK
        tq = t0 + jnp.arange(Q_BLOCK)
        qi = lax.dynamic_slice_in_dim(q_idx, t0, Q_BLOCK, axis=1)
        wi = lax.dynamic_slice_in_dim(w_idx, t0, Q_BLOCK, axis=1)
        logits = jnp.einsum('bqhe,bse->bqhs', qi, k_idx).astype(jnp.float32) * idx_scale
        iscore = jnp.einsum('bqh,bqhs->bqs', wi.astype(jnp.float32), jax.nn.relu(logits))
        causal = key_pos[None, :] <= tq[:, None]
        iscore = jnp.where(causal[None], iscore, -jnp.inf)
        _, sel = lax.top_k(iscore, topk)
        valid = sel <= tq[None, :, None]
        ckv_sel = gather(c_kv, sel)
        kr_sel = gather(k_rope, sel)
        ql = lax.dynamic_slice_in_dim(q_lat, t0, Q_BLOCK, axis=1)
        qr = lax.dynamic_slice_in_dim(q_rope, t0, Q_BLOCK, axis=1)
        s = (jnp.einsum('bqhr,bqkr->bqhk', ql, ckv_sel)
             + jnp.einsum('bqhe,bqke->bqhk', qr, kr_sel)).astype(jnp.float32) * attn_scale
        s = jnp.where(valid[:, :, None, :], s, -jnp.inf)
        p = jax.nn.softmax(s, axis=-1)
        return jnp.einsum('bqhk,bqkr->bqhr', p.astype(c_kv.dtype), ckv_sel)

    o_lat = lax.map(block, jnp.arange(S // Q_BLOCK))
    o_lat = jnp.moveaxis(o_lat, 0, 1).reshape(B, S, H_A, KV_LORA)
    o = jnp.einsum('bshr,rhv->bshv', o_lat, w_uv)
    return o.reshape(B, S, WIDTH_A)


def dilated_group(q, k, v, window, dilation):
    B, S, H, dh = q.shape
    offs = jnp.arange(window // dilation + 1) * dilation
    scale = dh ** -0.5

    def block(i):
        t0 = i * Q_BLOCK
        tq = t0 + jnp.arange(Q_BLOCK)
        idx = tq[:, None] - offs[None, :]
        valid = idx >= 0
        idxc = jnp.maximum(idx, 0)
        kg = k[:, idxc]
        vg = v[:, idxc]
        qb = lax.dynamic_slice_in_dim(q, t0, Q_BLOCK, axis=1)
        s = jnp.einsum('bqhe,bqnhe->bqhn', qb, kg).astype(jnp.float32) * scale
        s = jnp.where(valid[None, :, None, :], s, -jnp.inf)
        lse = jax.nn.logsumexp(s, axis=-1)
        p = jnp.exp(s - lse[..., None])
        o = jnp.einsum('bqhn,bqnhe->bqhe', p.astype(v.dtype), vg)
        return o, lse

    o, lse = lax.map(block, jnp.arange(S // Q_BLOCK))
    o = jnp.moveaxis(o, 0, 1).reshape(B, S, H, dh)
    lse = jnp.moveaxis(lse, 0, 1).reshape(B, S, H)
    return o, lse


def dilated_branch(qkv, pos):
    B, S = qkv.shape[:2]
    q = rope(qkv[:, :, 0], pos)
    k = rope(qkv[:, :, 1], pos)
    v = qkv[:, :, 2]
    outs, lses = [], []
    for g, (window, dilation) in enumerate(DIL_PAIRS):
        o, lse = dilated_group(q[:, :, g], k[:, :, g], v[:, :, g], window, dilation)
        outs.append(o)
        lses.append(lse)
    wts = jax.nn.softmax(jnp.stack(lses, axis=0), axis=0)
    o = jnp.sum(wts[..., None].astype(v.dtype) * jnp.stack(outs, axis=0), axis=0)
    return o.reshape(B, S, WIDTH_B)


def hybrid_mixer(u, pos, w_in, g_cq, g_ckv, w_uq, w_uk, w_uv, w_iq, w_up_a, w_up_b, w_o):
    B, S, _ = u.shape
    proj = u @ w_in
    splits, acc = [], 0
    for n in COLS[:-1]:
        acc += n
        splits.append(acc)
    c_q, c_kv, k_rope, k_idx, w_idx, qkv_b, gates = jnp.split(proj, splits, axis=-1)
    c_q = rms_norm(c_q, g_cq)
    c_kv = rms_norm(c_kv, g_ckv)
    o_a = dsa_branch(c_q, c_kv, k_rope, k_idx, w_idx, pos, w_uq, w_uk, w_uv, w_iq)
    o_b = dilated_branch(qkv_b.reshape(B, S, 3, N_GROUPS_B, H_B, D_HEAD_B), pos)
    g_a, g_b = jnp.split(gates, 2, axis=-1)
    z = jax.nn.sigmoid(g_a) * (o_a @ w_up_a) + jax.nn.sigmoid(g_b) * (o_b @ w_up_b)
    return z @ w_o


def setup_inputs(seed: int = 0) -> dict:
    key = jax.random.key(seed)
    ks = jax.random.split(key, 32)
    L, D = DEPTH, D_MODEL

    def nrm(k, shape, fan_in):
        return jax.random.normal(k, shape, jnp.float32) * (fan_in ** -0.5)

    def gain(k, shape):
        return 1.0 + 0.05 * jax.random.normal(k, shape, jnp.float32)

    x = jax.random.normal(ks[0], (BATCH, SEQ, D), jnp.float32)
    c = jax.random.normal(ks[1], (BATCH, D), jnp.float32)
    positions = (jax.random.randint(ks[2], (BATCH, 1), 0, 512, dtype=jnp.int32)
                 + jnp.arange(SEQ, dtype=jnp.int32)[None, :])
    return {
        "x": x,
        "c": c,
        "positions": positions,
        "w_mod": 0.5 * nrm(ks[3], (L, D, N_MOD * D), D),
        "b_mod": 0.01 * jax.random.normal(ks[4], (L, N_MOD * D), jnp.float32),
        "g_pre_ffn1": gain(ks[5], (L, D)),
        "w_gate1": nrm(ks[6], (L, D, D_FF), D),
        "w_up1": nrm(ks[7], (L, D, D_FF), D),
        "w_down1": nrm(ks[8], (L, D_FF, D), D_FF),
        "g_post_ffn1": gain(ks[9], (L, D)),
        "g_pre_mix": gain(ks[10], (L, D)),
        "w_in": nrm(ks[11], (L, D, D_IN), D),
        "g_cq": gain(ks[12], (L, Q_LORA)),
        "g_ckv": gain(ks[13], (L, KV_LORA)),
        "w_uq": nrm(ks[14], (L, Q_LORA, H_A, D_NOPE + D_ROPE_A), Q_LORA),
        "w_uk": nrm(ks[15], (L, KV_LORA, H_A, D_NOPE), KV_LORA),
        "w_uv": nrm(ks[16], (L, KV_LORA, H_A, D_V_A), KV_LORA),
        "w_iq": nrm(ks[17], (L, Q_LORA, H_IDX, D_IDX), Q_LORA),
        "w_up_a": nrm(ks[18], (L, WIDTH_A, D), WIDTH_A),
        "w_up_b": nrm(ks[19], (L, WIDTH_B, D), WIDTH_B),
        "w_o": nrm(ks[20], (L, D, D), D),
        "g_post_mix": gain(ks[21], (L, D)),
        "g_pre_ffn2": gain(ks[22], (L, D)),
        "w_gate2": nrm(ks[23], (L, D, D_FF), D),
        "w_up2": nrm(ks[24], (L, D, D_FF), D),
        "w_down2": nrm(ks[25], (L, D_FF, D), D_FF),
        "g_post_ffn2": gain(ks[26], (L, D)),
    }


def reference(x, c, positions, w_mod, b_mod, g_pre_ffn1, w_gate1, w_up1, w_down1, g_post_ffn1,
              g_pre_mix, w_in, g_cq, g_ckv, w_uq, w_uk, w_uv, w_iq, w_up_a, w_up_b, w_o, g_post_mix,
              g_pre_ffn2, w_gate2, w_up2, w_down2, g_post_ffn2):
    B = x.shape[0]
    for l in range(DEPTH):
        mod = (jax.nn.silu(c) @ w_mod[l] + b_mod[l]).reshape(B, N_MOD, D_MODEL)
        sh1, sc1, gt1, sh2, sc2, gt2, sh3, sc3, gt3 = [mod[:, j, None, :] for j in range(N_MOD)]
        h = rms_norm(x, g_pre_ffn1[l]) * (1 + sc1) + sh1
        x = x + 0.5 * gt1 * rms_norm(swiglu(h, w_gate1[l], w_up1[l], w_down1[l]), g_post_ffn1[l])
        u = rms_norm(x, g_pre_mix[l]) * (1 + sc2) + sh2
        y = hybrid_mixer(u, positions, w_in[l], g_cq[l], g_ckv[l], w_uq[l], w_uk[l], w_uv[l], w_iq[l],
                         w_up_a[l], w_up_b[l], w_o[l])
        x = x + gt2 * rms_norm(y, g_post_mix[l])
        h = rms_norm(x, g_pre_ffn2[l]) * (1 + sc3) + sh3
        x = x + 0.5 * gt3 * rms_norm(swiglu(h, w_gate2[l], w_up2[l], w_down2[l]), g_post_ffn2[l])
    return x
```

```python
import numpy as np
from contextlib import ExitStack
import concourse.bass as bass
import concourse.mybir as mybir
from concourse.bass_utils import run_bass_kernel_spmd

F32 = mybir.dt.float32
BF16 = mybir.dt.bfloat16
I32 = mybir.dt.int32
ALU = mybir.AluOpType
AF = mybir.ActivationFunctionType

D = 1024
T = 4096
NT = 32
DFF = 2816
NF = 22
EPS = 1e-6
THETA = 10000.0
SEM_EPOCH = 12000
PI = float(np.pi)
TWO_PI = 2.0 * PI
C_HI = float(np.float32(6.28125))
C_LO = float(TWO_PI - 6.28125)
NEG = -1.0e30
BIS_ITERS = 16
BIS_W0 = 8.0
MASK_NEG = -30000.0


class Buf:
    __slots__ = ("name", "last_w", "readers")

    def __init__(self, name):
        self.name = name
        self.last_w = None
        self.readers = {}


class Tn:
    __slots__ = ("t", "b")

    def __init__(self, t, name):
        self.t = t
        self.b = Buf(name)


class Eng:
    def __init__(self, name):
        self.name = name
        self.is_pe = name == "pe"
        self.count = 0
        self.epoch = 0
        self.known = {}
        self.thunks = []
        self.dma_slot = 0
        self.dma_uses = {}


class FW:
    def __init__(self, nc, stack, n_dma_sems=12):
        self.nc = nc
        self.stack = stack
        self.engs = {n: Eng(n) for n in ("pe", "act", "dve", "pool", "sp")}
        self.sems = {}
        self.n_dma_sems = n_dma_sems

    def _sem(self, key):
        if key not in self.sems:
            self.sems[key] = self.stack.enter_context(self.nc.semaphore("s_%s_%d" % key))
        return self.sems[key]

    def _collect(self, E, reads, writes, skip_self_pe=False):
        waits = []

        def need(ev):
            key, val = ev
            if skip_self_pe and key[0] == "pe":
                return
            if E.known.get(key, 0) >= val:
                return
            E.known[key] = val
            waits.append((key, val))

        for b in reads:
            if b.last_w is not None:
                need(b.last_w)
        for b in writes:
            if b.last_w is not None:
                need(b.last_w)
            for k, v in b.readers.items():
                need((k, v))
        return waits

    def _record(self, ev, reads, writes):
        key, val = ev
        for b in reads:
            if b.readers.get(key, 0) < val:
                b.readers[key] = val
        for b in writes:
            b.last_w = ev
            b.readers = {}

    def op(self, eng, fn, reads=(), writes=(), pe_acc=False):
        E = self.engs[eng]
        reads = [x.b if isinstance(x, Tn) else x for x in reads]
        writes = [x.b if isinstance(x, Tn) else x for x in writes]
        waits = self._collect(E, reads, writes, skip_self_pe=(E.is_pe and pe_acc))
        if E.count >= SEM_EPOCH:
            E.epoch += 1
            E.count = 0
        E.count += 1
        key = (E.name, E.epoch)
        ev = (key, E.count)
        E.thunks.append((waits, fn, (key, 1)))
        self._record(ev, reads, writes)

    def dma(self, eng, fn, reads=(), writes=()):
        E = self.engs[eng]
        reads = [x.b if isinstance(x, Tn) else x for x in reads]
        writes = [x.b if isinstance(x, Tn) else x for x in writes]
        waits = self._collect(E, reads, writes)
        slot = E.dma_slot
        E.dma_slot = (E.dma_slot + 1) % self.n_dma_sems
        key = ("dma_" + eng, slot)
        uses = E.dma_uses.get(slot, 0)
        if uses > 0 and E.known.get(key, 0) < 16 * uses:
            waits.append((key, 16 * uses))
            E.known[key] = 16 * uses
        uses += 1
        E.dma_uses[slot] = uses
        ev = (key, 16 * uses)
        E.thunks.append((waits, fn, (key, 16)))
        self._record(ev, reads, writes)

    def wait_all(self, eng, bufs):
        E = self.engs[eng]
        bufs = [x.b if isinstance(x, Tn) else x for x in bufs]
        waits = self._collect(E, bufs, ())
        E.thunks.append((waits, None, None))

    def flush(self):
        nc = self.nc
        for E in self.engs.values():
            for waits, fn, inc in E.thunks:
                for key, _ in waits:
                    self._sem(key)
                if inc is not None:
                    self._sem(inc[0])
        sems = self.sems
        with nc.Block() as block:
            def replay(E):
                def run(eo):
                    for waits, fn, inc in E.thunks:
                        for key, val in waits:
                            eo.wait_ge(sems[key], val)
                        if fn is not None:
                            fn(eo).then_inc(sems[inc[0]], inc[1])
                return run

            block.tensor(replay(self.engs["pe"]))
            block.scalar(replay(self.engs["act"]))
            block.vector(replay(self.engs["dve"]))
            block.gpsimd(replay(self.engs["pool"]))
            block.sync(replay(self.engs["sp"]))
        for E in self.engs.values():
            E.thunks = []


def build_program(debug=False):
    nc = bass.Bass("TRN2", target_bir_lowering=False)

    def din(name, shape, dt=F32):
        return nc.dram_tensor(name, list(shape), dt, kind="ExternalInput").ap()

    def dscr(name, shape, dt):
        return nc.dram_tensor(name, list(shape), dt, kind=("ExternalOutput" if debug else "Internal")).ap()

    x_l = din("x_l", [T, D])
    pos_l = din("pos_l", [1, T], I32)
    c_col = din("c_col", [128, 8])
    kvalid_d = din("kvalid", [128, NT])
    kbias_d = din("kbias", [1, T])
    wmod_t = din("wmod_t", [18, 128, 8, 512])
    bmod = din("bmod", [1, 9 * D])
    gv = din("gv", [6, D])
    gcq = din("gcq", [1, 384])
    gckv = din("gckv", [1, 256])
    wgu1 = din("wgu1", [NF, 128, 2, 8, 128])
    wd1 = din("wd1", [128, NF, D])
    wgu2 = din("wgu2", [NF, 128, 2, 8, 128])
    wd2 = din("wd2", [128, NF, D])
    w_dsa_in = din("w_dsa_in", [128, 8, 904])
    w_q = din("w_q", [128, 3, 2048])
    w_kv = din("w_kv", [128, 2, 1024])
    w_dil = din("w_dil", [3, 128, 8, 1280])
    w_gates = din("w_gates", [128, 8, 2048])
    w_upa = din("w_upa", [128, 4, D])
    w_upb = din("w_upb", [64, 4, D])
    w_o = din("w_o", [128, 8, D])
    ident_d = din("ident", [128, 128])
    cvec_d = din("cvec", [128, 8])
    tri_d = din("tri", [128, 256])
    cm_d = din("cm", [128, 128])
    out_l = nc.dram_tensor("out_l", [2048, D], F32, kind="ExternalOutput").ap()

    modS = dscr("modS", [128, 9 * D], F32)
    x1S = dscr("x1S", [2048, D], F32)
    uTS = dscr("uTS", [128, 8, T], BF16)
    x2S = dscr("x2S", [2048, D], F32)
    h2TS = dscr("h2TS", [128, 8, 2048], BF16)
    obTS = dscr("obTS", [64, 4, 2048], BF16)
    oaTS = dscr("oaTS", [128, 4, 2048], BF16)
    cos64S = dscr("cos64S", [128, T], BF16)
    sin64S = dscr("sin64S", [128, T], BF16)
    cos32S = dscr("cos32S", [96, T], BF16)
    sin32S = dscr("sin32S", [96, T], BF16)

    with ExitStack() as top:
        fw = FW(nc, top)

        uniq = {"n": 0}

        def alloc(st, name, shape, dt):
            uniq["n"] += 1
            nm = "sb%d_%s" % (uniq["n"], name)
            return Tn(st.enter_context(nc.sbuf_tensor(nm, list(shape), dt)), nm)

        psf = [Tn(top.enter_context(nc.psum_tensor("psf%d" % i, [128, 512], F32)), "psf%d" % i) for i in range(7)]
        psb = [Tn(top.enter_context(nc.psum_tensor("psb%d" % i, [128, 1024], BF16)), "psb%d" % i) for i in range(1)]
        ctr = {"f": 0, "n": 7}

        def PSF():
            p = psf[ctr["f"] % ctr["n"]]
            ctr["f"] += 1
            return p

        def PSB():
            return psb[0]

        def PE(fn, r, w):
            fw.op("pe", fn, r, w, pe_acc=True)

        def ACT(fn, r, w):
            fw.op("act", fn, r, w)

        def DVE(fn, r, w):
            fw.op("dve", fn, r, w)

        def POOL(fn, r, w):
            fw.op("pool", fn, r, w)

        def LD(fn, r, w):
            fw.dma("sp", fn, r, w)

        def LDC(fn, r, w):
            fw.dma("pool", fn, r, w)

        ident = alloc(top, "ident", [128, 128], BF16)
        cvec = alloc(top, "cvec", [128, 8], F32)
        kvalid = alloc(top, "kvalid_sb", [128, NT], F32)
        ones8 = alloc(top, "ones8", [128, 8], F32)
        epsc = alloc(top, "epsc", [128, 1], F32)
        LDC(lambda e: e.dma_start(out=ident.t[:], in_=ident_d[:, :]), [], [ident])
        LD(lambda e: e.dma_start(out=cvec.t[:], in_=cvec_d[:, :]), [], [cvec])
        LD(lambda e: e.dma_start(out=kvalid.t[:], in_=kvalid_d[:, :]), [], [kvalid])
        DVE(lambda e: e.memset(ones8.t[:], 1.0), [], [ones8])
        DVE(lambda e: e.memset(epsc.t[:], EPS), [], [epsc])

        small = {"i": 0}

        def rstd_of(st_pool, ss_ap, ss_tn, n):
            r = st_pool[small["i"] % len(st_pool)]
            small["i"] += 1
            ACT(lambda e: e.activation(out=r.t[:], in_=ss_ap, func=AF.Sqrt, scale=1.0 / n, bias=epsc.t[:]), [ss_tn, epsc], [r])
            DVE(lambda e: e.reciprocal(out=r.t[:], in_=r.t[:]), [r], [r])
            return r

        def transpose_to(src, ncols_chunks, dst_ap, dst_tn, eng_copy="act"):
            pb = PSB()
            for k in range(ncols_chunks):
                PE(lambda e, k=k: e.transpose(pb.t[:, k * 128:(k + 1) * 128], src.t[:, k * 128:(k + 1) * 128], ident.t[:]),
                   [src, ident], [pb])
            view = pb.t[:, 0:ncols_chunks * 128].rearrange("p (k t) -> p k t", k=ncols_chunks)
            if eng_copy == "act":
                ACT(lambda e: e.activation(out=dst_ap, in_=view, func=AF.Copy), [pb], [dst_tn])
            else:
                DVE(lambda e: e.tensor_copy(out=dst_ap, in_=view), [pb], [dst_tn])

        tabS_b = Buf("tabS")

        def rope_chunk(tp_, nrows, inv_col, sgn_col, cosS, sinS, c):
            posi, xs, kf, ki, ang = tp_["posi"], tp_["xs"], tp_["kf"], tp_["ki"], tp_["ang"]
            R = slice(0, nrows)
            cs = slice(c * 1024, (c + 1) * 1024)
            LD(lambda e: e.dma_start(out=posi.t[R, :], in_=pos_l[0:1, cs].to_broadcast([nrows, 1024])), [], [posi])
            DVE(lambda e: e.tensor_copy(out=ang.t[R, :], in_=posi.t[R, :]), [posi], [ang])
            DVE(lambda e: e.tensor_scalar(out=ang.t[R, :], in0=ang.t[R, :], scalar1=cvec.t[R, inv_col:inv_col + 1], scalar2=None,
                                          op0=ALU.mult), [ang, cvec], [ang])
            for which in range(2):
                off = 0.0 if which == 1 else PI / 2.0
                dstS = sinS if which == 1 else cosS
                ot = tp_["ot"][which]
                DVE(lambda e, off=off: e.tensor_scalar(out=xs.t[R, :], in0=ang.t[R, :], scalar1=off, scalar2=None, op0=ALU.add), [ang], [xs])
                DVE(lambda e: e.tensor_scalar(out=kf.t[R, :], in0=xs.t[R, :], scalar1=1.0 / TWO_PI, scalar2=None, op0=ALU.mult), [xs], [kf])
                DVE(lambda e: e.tensor_copy(out=ki.t[R, :], in_=kf.t[R, :]), [kf], [ki])
                DVE(lambda e: e.tensor_copy(out=kf.t[R, :], in_=ki.t[R, :]), [ki], [kf])
                DVE(lambda e: e.scalar_tensor_tensor(out=xs.t[R, :], in0=kf.t[R, :], scalar=-C_HI, in1=xs.t[R, :], op0=ALU.mult, op1=ALU.add),
                    [kf, xs], [xs])
                DVE(lambda e: e.scalar_tensor_tensor(out=xs.t[R, :], in0=kf.t[R, :], scalar=-C_LO, in1=xs.t[R, :], op0=ALU.mult, op1=ALU.add),
                    [kf, xs], [xs])
                DVE(lambda e: e.tensor_scalar(out=kf.t[R, :], in0=xs.t[R, :], scalar1=PI, scalar2=-TWO_PI, op0=ALU.is_gt, op1=ALU.mult), [xs], [kf])
                DVE(lambda e: e.tensor_tensor(out=xs.t[R, :], in0=xs.t[R, :], in1=kf.t[R, :], op=ALU.add), [xs, kf], [xs])
                DVE(lambda e: e.tensor_scalar(out=kf.t[R, :], in0=xs.t[R, :], scalar1=-PI, scalar2=TWO_PI, op0=ALU.is_lt, op1=ALU.mult), [xs], [kf])
                DVE(lambda e: e.tensor_tensor(out=xs.t[R, :], in0=xs.t[R, :], in1=kf.t[R, :], op=ALU.add), [xs, kf], [xs])
                ACT(lambda e: e.activation(out=xs.t[R, :], in_=xs.t[R, :], func=AF.Sin), [xs], [xs])
                if which == 1:
                    DVE(lambda e, ot=ot: e.tensor_scalar(out=ot.t[R, :], in0=xs.t[R, :], scalar1=cvec.t[R, sgn_col:sgn_col + 1], scalar2=None,
                                                         op0=ALU.mult), [xs, cvec], [ot])
                else:
                    DVE(lambda e, ot=ot: e.tensor_copy(out=ot.t[R, :], in_=xs.t[R, :]), [xs], [ot])
                LD(lambda e, ot=ot, dstS=dstS: e.dma_start(out=dstS[0:nrows, cs], in_=ot.t[R, :]), [ot], [tabS_b])

        def rope_tables(st, nrows, inv_col, sgn_col, cos_t, sin_t):
            with ExitStack() as tmp:
                posi = alloc(tmp, "posi", [128, 1024], I32)
                xs = alloc(tmp, "rt_xs", [128, 1024], F32)
                kf = alloc(tmp, "rt_kf", [128, 1024], F32)
                ki = alloc(tmp, "rt_ki", [128, 1024], I32)
                ang = alloc(tmp, "rt_ang", [128, 1024], F32)
                R = slice(0, nrows)
                for c in range(4):
                    cs = slice(c * 1024, (c + 1) * 1024)
                    LD(lambda e, cs=cs: e.dma_start(out=posi.t[R, :], in_=pos_l[0:1, cs].to_broadcast([nrows, 1024])), [], [posi])
                    DVE(lambda e: e.tensor_copy(out=ang.t[R, :], in_=posi.t[R, :]), [posi], [ang])
                    DVE(lambda e: e.tensor_scalar(out=ang.t[R, :], in0=ang.t[R, :], scalar1=cvec.t[R, inv_col:inv_col + 1], scalar2=None,
                                                  op0=ALU.mult), [ang, cvec], [ang])
                    for which in range(2):
                        off = 0.0 if which == 1 else PI / 2.0
                        dst = sin_t if which == 1 else cos_t
                        DVE(lambda e, off=off: e.tensor_scalar(out=xs.t[R, :], in0=ang.t[R, :], scalar1=off, scalar2=None, op0=ALU.add),
                            [ang], [xs])
                        DVE(lambda e: e.tensor_scalar(out=kf.t[R, :], in0=xs.t[R, :], scalar1=1.0 / TWO_PI, scalar2=None, op0=ALU.mult),
                            [xs], [kf])
                        DVE(lambda e: e.tensor_copy(out=ki.t[R, :], in_=kf.t[R, :]), [kf], [ki])
                        DVE(lambda e: e.tensor_copy(out=kf.t[R, :], in_=ki.t[R, :]), [ki], [kf])
                        DVE(lambda e: e.scalar_tensor_tensor(out=xs.t[R, :], in0=kf.t[R, :], scalar=-C_HI, in1=xs.t[R, :], op0=ALU.mult,
                                                             op1=ALU.add), [kf, xs], [xs])
                        DVE(lambda e: e.scalar_tensor_tensor(out=xs.t[R, :], in0=kf.t[R, :], scalar=-C_LO, in1=xs.t[R, :], op0=ALU.mult,
                                                             op1=ALU.add), [kf, xs], [xs])
                        DVE(lambda e: e.tensor_scalar(out=kf.t[R, :], in0=xs.t[R, :], scalar1=PI, scalar2=-TWO_PI, op0=ALU.is_gt,
                                                      op1=ALU.mult), [xs], [kf])
                        DVE(lambda e: e.tensor_tensor(out=xs.t[R, :], in0=xs.t[R, :], in1=kf.t[R, :], op=ALU.add), [xs, kf], [xs])
                        DVE(lambda e: e.tensor_scalar(out=kf.t[R, :], in0=xs.t[R, :], scalar1=-PI, scalar2=TWO_PI, op0=ALU.is_lt,
                                                      op1=ALU.mult), [xs], [kf])
                        DVE(lambda e: e.tensor_tensor(out=xs.t[R, :], in0=xs.t[R, :], in1=kf.t[R, :], op=ALU.add), [xs, kf], [xs])
                        ACT(lambda e: e.activation(out=xs.t[R, :], in_=xs.t[R, :], func=AF.Sin), [xs], [xs])
                        if which == 1:
                            DVE(lambda e, cs=cs, dst=dst: e.tensor_scalar(out=dst.t[R, cs], in0=xs.t[R, :], scalar1=cvec.t[R, sgn_col:sgn_col + 1],
                                                                         scalar2=None, op0=ALU.mult), [xs, cvec], [dst])
                        else:
                            DVE(lambda e, cs=cs, dst=dst: e.tensor_copy(out=dst.t[R, cs], in_=xs.t[R, :]), [xs], [dst])
                fw.flush()

        with ExitStack() as ph:
            ccol = alloc(ph, "ccol", [128, 8], F32)
            scl = alloc(ph, "scl", [128, 8], F32)
            ones = alloc(ph, "ones", [128, 128], F32)
            scb = alloc(ph, "scb", [128, 8, 128], F32)
            mod = alloc(ph, "mod", [128, 9 * D], F32)
            wm = [alloc(ph, "wm%d" % i, [128, 8, 512], F32) for i in range(2)]
            bm = [alloc(ph, "bm%d" % i, [128, 512], F32) for i in range(2)]
            gt = [alloc(ph, "gt%d" % i, [128, D], F32) for i in range(2)]
            tp_ = {"posi": alloc(ph, "posi", [128, 1024], I32), "xs": alloc(ph, "rt_xs", [128, 1024], F32),
                   "kf": alloc(ph, "rt_kf", [128, 1024], F32), "ki": alloc(ph, "rt_ki", [128, 1024], I32),
                   "ang": alloc(ph, "rt_ang", [128, 1024], F32),
                   "ot": [alloc(ph, "rt_ot%d" % i, [128, 1024], BF16) for i in range(2)]}
            tjobs = [(128, 0, 1, cos64S, sin64S, c) for c in range(4)] + [(96, 2, 3, cos32S, sin32S, c) for c in range(4)]
            LD(lambda e: e.dma_start(out=ccol.t[:], in_=c_col[:, :]), [], [ccol])
            ACT(lambda e: e.activation(out=scl.t[:], in_=ccol.t[:], func=AF.Silu), [ccol], [scl])
            DVE(lambda e: e.memset(ones.t[:], 1.0), [], [ones])
            for kc in range(8):
                DVE(lambda e, kc=kc: e.tensor_scalar(out=scb.t[:, kc, :], in0=ones.t[:], scalar1=scl.t[:, kc:kc + 1], scalar2=None,
                                                     op0=ALU.mult), [ones, scl], [scb])
            for g in range(18):
                w = wm[g % 2]
                b = bm[g % 2]
                LD(lambda e, w=w, g=g: e.dma_start(out=w.t[:], in_=wmod_t[g]), [], [w])
                LD(lambda e, b=b, g=g: e.dma_start(out=b.t[:], in_=bmod[0:1, g * 512:(g + 1) * 512].to_broadcast([128, 512])), [], [b])
                ps = PSF()
                for kc in range(8):
                    PE(lambda e, kc=kc, w=w, ps=ps: e.matmul(ps.t[:], scb.t[:, kc, :], w.t[:, kc, :], start=(kc == 0), stop=(kc == 7)),
                       [scb, w], [ps])
                DVE(lambda e, g=g, ps=ps, b=b: e.tensor_tensor(out=mod.t[:, g * 512:(g + 1) * 512], in0=ps.t[:], in1=b.t[:], op=ALU.add),
                    [ps, b], [mod])
                if g % 2 == 1 and tjobs:
                    rope_chunk(tp_, *tjobs.pop(0))
            for i in range(3):
                coef = 1.0 if i == 1 else 0.5
                ga, gb = gt[0], gt[1]
                LD(lambda e, i=i, ga=ga: e.dma_start(out=ga.t[:], in_=gv[2 * i:2 * i + 1, :].to_broadcast([128, D])), [], [ga])
                LD(lambda e, i=i, gb=gb: e.dma_start(out=gb.t[:], in_=gv[2 * i + 1:2 * i + 2, :].to_broadcast([128, D])), [], [gb])
                s0 = (3 * i + 1) * D
                g0 = (3 * i + 2) * D
                DVE(lambda e, s0=s0, ga=ga: e.scalar_tensor_tensor(out=mod.t[:, s0:s0 + D], in0=mod.t[:, s0:s0 + D], scalar=1.0, in1=ga.t[:],
                                                                   op0=ALU.add, op1=ALU.mult), [mod, ga], [mod])
                DVE(lambda e, g0=g0, gb=gb, coef=coef: e.scalar_tensor_tensor(out=mod.t[:, g0:g0 + D], in0=mod.t[:, g0:g0 + D], scalar=coef,
                                                                              in1=gb.t[:], op0=ALU.mult, op1=ALU.mult), [mod, gb], [mod])
            modS_b = Buf("modS")
            LD(lambda e: e.dma_start(out=modS[:, :], in_=mod.t[:]), [mod], [modS_b])
            while tjobs:
                rope_chunk(tp_, *tjobs.pop(0))
            fw.wait_all("sp", [modS_b, tabS_b])
            fw.flush()

        x1S_b = Buf("x1S")
        uTS_b = Buf("uTS")
        x2S_b = Buf("x2S")
        h2TS_b = Buf("h2TS")
        obTS_b = Buf("obTS")
        oaTS_b = Buf("oaTS")
        out_b = Buf("out")

        def load_mod(st, name, idx):
            t = alloc(st, name, [128, D], F32)
            LD(lambda e: e.dma_start(out=t.t[:], in_=modS[:, idx * D:(idx + 1) * D]), [modS_b], [t])
            return t

        def ffn_phase(first):
            ntok = T if first else 2048
            ngrp = ntok // 1024
            wgu = wgu1 if first else wgu2
            wdd = wd1 if first else wd2
            with ExitStack() as ph:
                wd = alloc(ph, "wd", [128, NF, D], BF16)
                for f0 in range(0, NF, 2):
                    LDC(lambda e, f0=f0: e.dma_start(out=wd.t[:, f0:f0 + 2, :], in_=wdd[:, f0:f0 + 2, :]), [], [wd])
                wring = [alloc(ph, "wring%d" % i, [128, 2, 8, 128], BF16) for i in range(4)]
                hTs = [alloc(ph, "hT%d" % i, [128, 8, 1024], BF16) for i in range(2)]
                aT = alloc(ph, "aT", [128, NF, 1024], BF16)
                xring = [alloc(ph, "xr%d" % i, [128, D], F32) for i in range(2)]
                tmpf = alloc(ph, "tmpf", [128, D], F32)
                junk = alloc(ph, "junk", [128, D], BF16)
                hbr = [alloc(ph, "hb%d" % i, [128, D], BF16) for i in range(2)]
                sg = [alloc(ph, "sg%d" % i, [128, 512], F32) for i in range(2)]
                ssr = [alloc(ph, "ss%d" % i, [128, 2], F32) for i in range(4)]
                rsr = [alloc(ph, "rs%d" % i, [128, 1], F32) for i in range(4)]
                if first:
                    sh1 = load_mod(ph, "sh1", 0)
                    gm1 = load_mod(ph, "gm1", 1)
                    cp = load_mod(ph, "cp1", 2)
                    sh2 = load_mod(ph, "sh2", 3)
                    gm2 = load_mod(ph, "gm2", 4)
                    x1r = [alloc(ph, "x1t%d" % i, [128, D], F32) for i in range(2)]
                    uTt = [alloc(ph, "uTt%d" % i, [128, 8, 128], BF16) for i in range(2)]
                else:
                    cp = load_mod(ph, "cp3", 8)
                    x1r = [alloc(ph, "x1t%d" % i, [128, D], F32) for i in range(2)]
                ssi = {"i": 0}

                def sumsq(src_aps, src_tns):
                    ss = ssr[ssi["i"] % 4]
                    ssi["i"] += 1
                    DVE(lambda e: e.memset(ss.t[:], 0.0), [], [ss])
                    for i, ap in enumerate(src_aps):
                        w = ap.shape[-1]
                        ACT(lambda e, i=i, ap=ap, w=w: e.activation(out=junk.t[:, 0:w], in_=ap, func=AF.Square, accum_out=ss.t[:, i:i + 1]),
                            src_tns, [junk, ss])
                    if len(src_aps) == 2:
                        DVE(lambda e: e.tensor_tensor(out=ss.t[:, 0:1], in0=ss.t[:, 0:1], in1=ss.t[:, 1:2], op=ALU.add), [ss], [ss])
                    return ss

                xi = {"i": 0}
                hbi = {"i": 0}

                def next_x():
                    xt = xring[xi["i"] % 2]
                    xi["i"] += 1
                    return xt

                def next_hb():
                    h_ = hbr[hbi["i"] % 2]
                    hbi["i"] += 1
                    return h_

                def chain_h(G, t):
                    tile = G * 8 + t
                    xt = next_x()
                    LD(lambda e, xt=xt, tile=tile: e.dma_start(out=xt.t[:], in_=x_l[tile * 128:(tile + 1) * 128, :]), [], [xt])
                    ss = sumsq([xt.t[:]], [xt])
                    rs = rstd_of(rsr, ss.t[:, 0:1], ss, D)
                    DVE(lambda e, xt=xt, rs=rs: e.scalar_tensor_tensor(out=tmpf.t[:], in0=xt.t[:], scalar=rs.t[:, 0:1], in1=gm1.t[:],
                                                                       op0=ALU.mult, op1=ALU.mult), [xt, rs, gm1], [tmpf])
                    h_ = next_hb()
                    DVE(lambda e, h_=h_: e.tensor_tensor(out=h_.t[:], in0=tmpf.t[:], in1=sh1.t[:], op=ALU.add), [tmpf, sh1], [h_])
                    return h_

                def trans_h(G, t, h_):
                    hT_ = hTs[G % 2]
                    transpose_to(h_, 8, hT_.t[:, :, t * 128:(t + 1) * 128], hT_)

                def load_hT(G):
                    hT_ = hTs[G % 2]
                    LD(lambda e, G=G, hT_=hT_: e.dma_start(out=hT_.t[:], in_=h2TS[:, :, G * 1024:(G + 1) * 1024]), [h2TS_b], [hT_])

                if first:
                    prev = None
                    for t in range(8):
                        h_ = chain_h(0, t)
                        if prev is not None:
                            trans_h(0, prev[0], prev[1])
                        prev = (t, h_)
                    trans_h(0, prev[0], prev[1])
                else:
                    load_hT(0)
                SLOTS = {1: 0, 3: 1, 6: 2, 8: 3, 11: 4, 13: 5, 16: 6, 18: 7}
                for G in range(ngrp):
                    hT = hTs[G % 2]
                    prev = None
                    for f in range(NF):
                        wr = wring[f % 4]
                        LDC(lambda e, wr=wr, f=f: e.dma_start(out=wr.t[:], in_=wgu[f]), [], [wr])
                        for half in range(2):
                            hs = slice(half * 512, (half + 1) * 512)
                            gp = PSF()
                            up = PSF()
                            for kc in range(8):
                                PE(lambda e, kc=kc, gp=gp, wr=wr, hs=hs, hT=hT: e.matmul(gp.t[:], wr.t[:, 0, kc, :], hT.t[:, kc, hs], start=(kc == 0),
                                                                                          stop=(kc == 7)), [wr, hT], [gp])
                            for kc in range(8):
                                PE(lambda e, kc=kc, up=up, wr=wr, hs=hs, hT=hT: e.matmul(up.t[:], wr.t[:, 1, kc, :], hT.t[:, kc, hs], start=(kc == 0),
                                                                                          stop=(kc == 7)), [wr, hT], [up])
                            s_ = sg[half]
                            ACT(lambda e, s_=s_, gp=gp: e.activation(out=s_.t[:], in_=gp.t[:], func=AF.Silu), [gp], [s_])
                            DVE(lambda e, s_=s_, up=up, f=f, hs=hs: e.tensor_tensor(out=aT.t[:, f, hs], in0=s_.t[:], in1=up.t[:], op=ALU.mult),
                                [s_, up], [aT])
                        if G + 1 < ngrp:
                            if first and f in SLOTS:
                                t = SLOTS[f]
                                h_ = chain_h(G + 1, t)
                                if prev is not None:
                                    trans_h(G + 1, prev[0], prev[1])
                                prev = (t, h_)
                            if (not first) and f == 0:
                                load_hT(G + 1)
                    if prev is not None:
                        trans_h(G + 1, prev[0], prev[1])
                    pend = None
                    for t in range(8):
                        tile = G * 8 + t
                        xt = next_x()
                        if first:
                            LD(lambda e, xt=xt, tile=tile: e.dma_start(out=xt.t[:], in_=x_l[tile * 128:(tile + 1) * 128, :]), [], [xt])
                        else:
                            LD(lambda e, xt=xt, tile=tile: e.dma_start(out=xt.t[:], in_=x2S[tile * 128:(tile + 1) * 128, :]), [x2S_b], [xt])
                        yps = [PSF(), PSF()]
                        for half in range(2):
                            yp = yps[half]
                            for f in range(NF):
                                PE(lambda e, f=f, yp=yp, t=t, half=half: e.matmul(yp.t[:], aT.t[:, f, t * 128:(t + 1) * 128],
                                                                                   wd.t[:, f, half * 512:(half + 1) * 512], start=(f == 0),
                                                                                   stop=(f == NF - 1)), [aT, wd], [yp])
                        if pend is not None:
                            ptile, ph_ = pend
                            ut = uTt[ptile % 2]
                            transpose_to(ph_, 8, ut.t[:], ut)
                            LD(lambda e, ut=ut, ptile=ptile: e.dma_start(out=uTS[:, :, ptile * 128:(ptile + 1) * 128], in_=ut.t[:]), [ut], [uTS_b])
                            pend = None
                        ss = sumsq([yps[0].t[:], yps[1].t[:]], yps)
                        rs = rstd_of(rsr, ss.t[:, 0:1], ss, D)
                        for half in range(2):
                            hs = slice(half * 512, (half + 1) * 512)
                            DVE(lambda e, half=half, hs=hs, rs=rs, yp=yps[half]: e.scalar_tensor_tensor(
                                out=tmpf.t[:, hs], in0=yp.t[:], scalar=rs.t[:, 0:1], in1=cp.t[:, hs], op0=ALU.mult, op1=ALU.mult),
                                [yps[half], rs, cp], [tmpf])
                        x1t = x1r[t % 2]
                        DVE(lambda e, xt=xt, x1t=x1t: e.tensor_tensor(out=x1t.t[:], in0=tmpf.t[:], in1=xt.t[:], op=ALU.add), [tmpf, xt], [x1t])
                        if not first:
                            LD(lambda e, tile=tile, x1t=x1t: e.dma_start(out=out_l[tile * 128:(tile + 1) * 128, :], in_=x1t.t[:]), [x1t], [out_b])
                            continue
                        if tile >= 16:
                            LD(lambda e, tile=tile, x1t=x1t: e.dma_start(out=x1S[(tile - 16) * 128:(tile - 15) * 128, :], in_=x1t.t[:]), [x1t], [x1S_b])
                        ss = sumsq([x1t.t[:]], [x1t])
                        rs = rstd_of(rsr, ss.t[:, 0:1], ss, D)
                        DVE(lambda e, rs=rs, x1t=x1t: e.scalar_tensor_tensor(out=tmpf.t[:], in0=x1t.t[:], scalar=rs.t[:, 0:1], in1=gm2.t[:],
                                                                             op0=ALU.mult, op1=ALU.mult), [x1t, rs, gm2], [tmpf])
                        h_ = next_hb()
                        DVE(lambda e, h_=h_: e.tensor_tensor(out=h_.t[:], in0=tmpf.t[:], in1=sh2.t[:], op=ALU.add), [tmpf, sh2], [h_])
                        pend = (tile, h_)
                    if pend is not None:
                        ptile, ph_ = pend
                        ut = uTt[ptile % 2]
                        transpose_to(ph_, 8, ut.t[:], ut)
                        LD(lambda e, ut=ut, ptile=ptile: e.dma_start(out=uTS[:, :, ptile * 128:(ptile + 1) * 128], in_=ut.t[:]), [ut], [uTS_b])
                        pend = None
                    if G % 2 == 1:
                        fw.flush()
                fw.wait_all("sp", [x1S_b, uTS_b, out_b])
                fw.flush()

        ffn_phase(True)

        with ExitStack() as ph:
            cos64 = alloc(ph, "cos64", [128, T], BF16)
            sin64 = alloc(ph, "sin64", [128, T], BF16)
            LD(lambda e: e.dma_start(out=cos64.t[:], in_=cos64S[:, :]), [tabS_b], [cos64])
            LD(lambda e: e.dma_start(out=sin64.t[:], in_=sin64S[:, :]), [tabS_b], [sin64])
            acc = alloc(ph, "acc", [128, 4, 2048], F32)
            tri = alloc(ph, "tri", [128, 256], BF16)
            LDC(lambda e: e.dma_start(out=tri.t[:], in_=tri_d[:, :]), [], [tri])
            gs = ExitStack()
            wdl = alloc(gs, "wdl", [128, 8, 1280], BF16)
            KT = alloc(gs, "KT", [128, 2, T], BF16)
            QT = alloc(gs, "QT", [128, 4, 2048], BF16)
            VA = alloc(gs, "VA", [128, NT, 4, 128], BF16)
            uTh = alloc(gs, "uTh", [128, 8, 2048], BF16)
            t1 = [alloc(gs, "t1_%d" % i, [128, 512], F32) for i in range(2)]
            t2 = [alloc(gs, "t2_%d" % i, [128, 512], F32) for i in range(2)]
            Eb = [alloc(gs, "Eb%d" % i, [128, 4, 128], BF16) for i in range(2)]
            PTb = [alloc(gs, "PTb%d" % i, [128, 4, 128], BF16) for i in range(2)]
            for g, d in enumerate((1, 4, 16)):
                LDC(lambda e, g=g: e.dma_start(out=wdl.t[:], in_=w_dil[g]), [], [wdl])
                DVE(lambda e: e.memset(VA.t[:], 1.0), [], [VA])
                DVE(lambda e: e.memset(QT.t[:], 0.0), [], [QT])
                ci = 0
                for hf in range(2):
                    LD(lambda e, hf=hf: e.dma_start(out=uTh.t[:], in_=uTS[:, :, hf * 2048:(hf + 1) * 2048]), [uTS_b], [uTh])
                    for c in range(4):
                        t0 = hf * 2048 + c * 512
                        cs = slice(c * 512, (c + 1) * 512)
                        jobs = [("k", 512, 768)]
                        if hf == 1:
                            jobs.append(("q", 0, 256))
                        for (nm, c0, c1) in jobs:
                            for pp in range(2):
                                pa = PSF()
                                pr = PSF()
                                for kc in range(8):
                                    PE(lambda e, kc=kc, pa=pa, pp=pp, c0=c0, cs=cs: e.matmul(pa.t[:, :], wdl.t[:, kc, c0 + pp * 128:c0 + (pp + 1) * 128],
                                                                                           uTh.t[:, kc, cs], start=(kc == 0), stop=(kc == 7)),
                                       [wdl, uTh], [pa])
                                for kc in range(8):
                                    PE(lambda e, kc=kc, pr=pr, pp=pp, c1=c1, cs=cs: e.matmul(pr.t[:, :], wdl.t[:, kc, c1 + pp * 128:c1 + (pp + 1) * 128],
                                                                                           uTh.t[:, kc, cs], start=(kc == 0), stop=(kc == 7)),
                                       [wdl, uTh], [pr])
                                a1 = t1[ci % 2]
                                a2 = t2[ci % 2]
                                ci += 1
                                DVE(lambda e, a1=a1, pa=pa, t0=t0: e.tensor_tensor(out=a1.t[:], in0=pa.t[:, :], in1=cos64.t[:, t0:t0 + 512],
                                                                                  op=ALU.mult), [pa, cos64], [a1])
                                DVE(lambda e, a2=a2, pr=pr, t0=t0: e.tensor_tensor(out=a2.t[:], in0=pr.t[:, :], in1=sin64.t[:, t0:t0 + 512],
                                                                                  op=ALU.mult), [pr, sin64], [a2])
                                if nm == "k":
                                    dview = KT.t[:, pp, :].rearrange("p (r m) -> p m r", r=d)[:, t0 // d:(t0 + 512) // d, :]
                                    POOL(lambda e, a1=a1, a2=a2, dview=dview: e.tensor_tensor(
                                        out=dview, in0=a1.t[:].rearrange("p (m r) -> p m r", r=d), in1=a2.t[:].rearrange("p (m r) -> p m r", r=d),
                                        op=ALU.add), [a1, a2], [KT])
                                else:
                                    n0 = c * 512
                                    for hh in range(2):
                                        R_ = slice(hh * 64, (hh + 1) * 64)
                                        dview = QT.t[R_, 2 * pp + hh, :].rearrange("p (r m) -> p m r", r=d)[:, n0 // d:(n0 + 512) // d, :]
                                        POOL(lambda e, a1=a1, a2=a2, dview=dview, R_=R_: e.tensor_tensor(
                                            out=dview, in0=a1.t[R_, :].rearrange("p (m r) -> p m r", r=d), in1=a2.t[R_, :].rearrange("p (m r) -> p m r", r=d),
                                            op=ALU.add), [a1, a2], [QT])
                    nb = 16 // d
                    for r in range(d):
                        for mbl in range(nb):
                            blk = r * (32 // d) + hf * nb + mbl
                            st0 = r + d * 128 * mbl
                            sl = slice(st0, st0 + d * 127 + 1, d)
                            vp = PSF()
                            for kc in range(8):
                                PE(lambda e, kc=kc, vp=vp, sl=sl: e.matmul(vp.t[:, 0:256], uTh.t[:, kc, sl], wdl.t[:, kc, 1024:1280],
                                                                          start=(kc == 0), stop=(kc == 7)), [uTh, wdl], [vp])
                            kvc = hf * 16
                            ACT(lambda e, vp=vp, blk=blk, kvc=kvc: e.activation(out=VA.t[:, blk, :, 0:64],
                                                                                 in_=vp.t[:, 0:256].rearrange("p (h e) -> p h e", h=4),
                                                                                 func=AF.Copy, scale=kvalid.t[:, kvc:kvc + 1]), [vp, kvalid], [VA])
                            if hf == 0:
                                DVE(lambda e, blk=blk: e.tensor_scalar(out=VA.t[:, blk, :, 64:128], in0=VA.t[:, blk, :, 64:128],
                                                                       scalar1=kvalid.t[:, 0:1], scalar2=None, op0=ALU.mult), [VA, kvalid], [VA])
                ei = 0
                for r in range(d):
                    for mbl in range(nb):
                        qb = r * nb + mbl
                        blk_same = r * (32 // d) + nb + mbl
                        op_ = PSF()
                        for bi, (blk, m0) in enumerate(((blk_same - 1, 0), (blk_same, 128))):
                            sp_ = PSF()
                            for h in range(4):
                                PE(lambda e, h=h, sp_=sp_, blk=blk, qb=qb: e.matmul(sp_.t[:, h * 128:(h + 1) * 128], KT.t[:, h // 2, blk * 128:(blk + 1) * 128],
                                                                                    QT.t[:, h, qb * 128:(qb + 1) * 128], start=True, stop=True),
                                   [KT, QT], [sp_])
                            E_ = Eb[ei % 2]
                            P_ = PTb[ei % 2]
                            ei += 1
                            ACT(lambda e, E_=E_, sp_=sp_: e.activation(out=E_.t[:], in_=sp_.t[:].rearrange("p (h q) -> p h q", h=4), func=AF.Exp,
                                                                        scale=0.125), [sp_], [E_])
                            POOL(lambda e, E_=E_, P_=P_, m0=m0: e.tensor_tensor(out=P_.t[:], in0=E_.t[:],
                                                                                 in1=tri.t[:, m0:m0 + 128].unsqueeze(1).to_broadcast([128, 4, 128]),
                                                                                 op=ALU.mult), [E_, tri], [P_])
                            for h in range(4):
                                PE(lambda e, h=h, op_=op_, P_=P_, blk=blk, bi=bi: e.matmul(op_.t[:, h * 128:(h + 1) * 128], VA.t[:, blk, h, :], P_.t[:, h, :],
                                                                                           start=(bi == 0 and h == 0), stop=(bi == 1)), [VA, P_], [op_])
                        st0 = r + d * 128 * mbl
                        aview = acc.t[:, :, st0:st0 + d * 127 + 1:d]
                        oview = op_.t[:].rearrange("p (h q) -> p h q", h=4)
                        if g == 0:
                            DVE(lambda e, aview=aview, oview=oview: e.tensor_copy(out=aview, in_=oview), [op_], [acc])
                        else:
                            DVE(lambda e, aview=aview, oview=oview: e.tensor_tensor(out=aview, in0=aview, in1=oview, op=ALU.add), [op_, acc], [acc])
                fw.flush()
            gs.close()
            rd = alloc(ph, "rd", [128, 4, 512], F32)
            rd0 = alloc(ph, "rd0", [64, 4, 512], F32)
            obt = alloc(ph, "obt", [64, 4, 512], BF16)
            for c in range(4):
                cs = slice(c * 512, (c + 1) * 512)
                DVE(lambda e, cs=cs: e.reciprocal(out=rd.t[64:128, :, :], in_=acc.t[64:128, :, cs]), [acc], [rd])
                DVE(lambda e: e.tensor_copy(out=rd0.t[0:64, :, :], in_=rd.t[64:128, :, :]), [rd], [rd0])
                DVE(lambda e, cs=cs: e.tensor_tensor(out=obt.t[:], in0=acc.t[0:64, :, cs], in1=rd0.t[:], op=ALU.mult), [acc, rd0], [obt])
                LD(lambda e, cs=cs: e.dma_start(out=obTS[:, :, cs], in_=obt.t[:]), [obt], [obTS_b])
            fw.wait_all("sp", [obTS_b])
            fw.flush()

        with ExitStack() as ph:
            cosq = alloc(ph, "cosq", [96, 2048], BF16)
            sinq = alloc(ph, "sinq", [96, 2048], BF16)
            kb0 = alloc(ph, "kb0", [128, 1], F32)
            DVE(lambda e: e.tensor_scalar(out=kb0.t[:], in0=kvalid.t[:, 0:1], scalar1=-1.0, scalar2=1.0e30, op0=ALU.add, op1=ALU.mult), [kvalid], [kb0])
            cm = alloc(ph, "cm", [128, 128], F32)
            LD(lambda e: e.dma_start(out=cm.t[:], in_=cm_d[:, :]), [], [cm])
            KH = alloc(ph, "KH", [96, 8, T], BF16)
            VA = alloc(ph, "VA2", [128, NT, 8, 65], BF16)
            KI = alloc(ph, "KI", [32, T], BF16)
            CQNT = alloc(ph, "CQNT", [128, 3, 2048], BF16)
            WSC = alloc(ph, "WSC", [128, 16, 8], F32)
            ssr = [alloc(ph, "bss%d" % i, [128, 2], F32) for i in range(4)]
            rsr = [alloc(ph, "brs%d" % i, [128, 1], F32) for i in range(4)]
            ks = ExitStack()
            cos32 = alloc(ks, "cos32", [96, T], BF16)
            sin32 = alloc(ks, "sin32", [96, T], BF16)
            LD(lambda e: e.dma_start(out=cos32.t[:], in_=cos32S[:, :]), [tabS_b], [cos32])
            LD(lambda e: e.dma_start(out=sin32.t[:], in_=sin32S[:, :]), [tabS_b], [sin32])
            LD(lambda e: e.dma_start(out=cosq.t[:], in_=cos32S[:, 2048:T]), [tabS_b], [cosq])
            LD(lambda e: e.dma_start(out=sinq.t[:], in_=sin32S[:, 2048:T]), [tabS_b], [sinq])
            wdi = alloc(ks, "wdi", [128, 8, 904], BF16)
            wkv = alloc(ks, "wkv", [128, 2, 1024], BF16)
            LDC(lambda e: e.dma_start(out=wdi.t[:], in_=w_dsa_in[:, :, :]), [], [wdi])
            LDC(lambda e: e.dma_start(out=wkv.t[:], in_=w_kv[:, :, :]), [], [wkv])
            gqb = alloc(ks, "gqb", [128, 384], F32)
            gkb = alloc(ks, "gkb", [128, 256], F32)
            LD(lambda e: e.dma_start(out=gqb.t[:], in_=gcq[0:1, :].to_broadcast([128, 384])), [], [gqb])
            LD(lambda e: e.dma_start(out=gkb.t[:], in_=gckv[0:1, :].to_broadcast([128, 256])), [], [gkb])
            if True:
                uTc = [alloc(ks, "uTc%d" % i, [128, 8, 512], BF16) for i in range(2)]
                ckvnT = [alloc(ks, "ckvnT%d" % i, [128, 2, 512], BF16) for i in range(2)]
                junk = alloc(ks, "kjunk", [128, 384], BF16)
                nb16 = alloc(ks, "nb16", [128, 384], BF16)
                r1 = alloc(ks, "r1", [96, 512], F32)
                r2 = alloc(ks, "r2", [96, 512], F32)
                r3 = alloc(ks, "r3", [96, 512], BF16)
                si = 0
                for c in range(8):
                    u = uTc[c % 2]
                    ck = ckvnT[c % 2]
                    cs = slice(c * 512, (c + 1) * 512)
                    LD(lambda e, u=u, cs=cs: e.dma_start(out=u.t[:], in_=uTS[:, :, cs]), [uTS_b], [u])
                    for t in range(4):
                        tile = c * 4 + t
                        ts_ = slice(t * 128, (t + 1) * 128)
                        jobs = [("kv", 0, 256, gkb)]
                        if tile >= 16:
                            jobs.append(("q", 520, 384, gqb))
                        for (nm, c0, wdt, gb_) in jobs:
                            cp_ = PSF()
                            for kc in range(8):
                                PE(lambda e, kc=kc, cp_=cp_, u=u, ts_=ts_, c0=c0, wdt=wdt: e.matmul(cp_.t[:, 0:wdt], u.t[:, kc, ts_], wdi.t[:, kc, c0:c0 + wdt],
                                                                                                 start=(kc == 0), stop=(kc == 7)), [u, wdi], [cp_])
                            ss = ssr[si % 4]
                            si += 1
                            DVE(lambda e, ss=ss: e.memset(ss.t[:], 0.0), [], [ss])
                            ACT(lambda e, ss=ss, cp_=cp_, wdt=wdt: e.activation(out=junk.t[:, 0:wdt], in_=cp_.t[:, 0:wdt], func=AF.Square,
                                                                                 accum_out=ss.t[:, 0:1]), [cp_], [junk, ss])
                            rs = rstd_of(rsr, ss.t[:, 0:1], ss, wdt)
                            DVE(lambda e, rs=rs, cp_=cp_, wdt=wdt, gb_=gb_: e.scalar_tensor_tensor(out=nb16.t[:, 0:wdt], in0=cp_.t[:, 0:wdt], scalar=rs.t[:, 0:1],
                                                                                                 in1=gb_.t[:, 0:wdt], op0=ALU.mult, op1=ALU.mult),
                                [cp_, rs, gb_], [nb16])
                            if nm == "kv":
                                transpose_to(nb16, 2, ck.t[:, :, ts_], ck)
                            else:
                                q0 = (tile - 16) * 128
                                transpose_to(nb16, 3, CQNT.t[:, :, q0:q0 + 128], CQNT)
                        if tile >= 16:
                            wp = PSF()
                            for kc in range(8):
                                PE(lambda e, kc=kc, wp=wp, u=u, ts_=ts_: e.matmul(wp.t[:, 0:8], u.t[:, kc, ts_], wdi.t[:, kc, 512:520], start=(kc == 0),
                                                                                   stop=(kc == 7)), [u, wdi], [wp])
                            ACT(lambda e, wp=wp, tile=tile: e.activation(out=WSC.t[:, tile - 16, :], in_=wp.t[:, 0:8], func=AF.Copy,
                                                                         scale=float(8 ** -0.5 * 32 ** -0.5)), [wp], [WSC])
                        vp = PSF()
                        for rc in range(2):
                            PE(lambda e, rc=rc, vp=vp, ck=ck, ts_=ts_: e.matmul(vp.t[:], ck.t[:, rc, ts_], wkv.t[:, rc, 512:1024], start=(rc == 0),
                                                                                 stop=(rc == 1)), [ck, wkv], [vp])
                        ACT(lambda e, vp=vp, tile=tile: e.activation(out=VA.t[:, tile, :, 0:64], in_=vp.t[:].rearrange("p (h e) -> p h e", h=8),
                                                                     func=AF.Copy, scale=kvalid.t[:, tile:tile + 1]), [vp, kvalid], [VA])
                        DVE(lambda e, tile=tile: e.tensor_scalar(out=VA.t[:, tile, :, 64:65], in0=ones8.t[:, :].unsqueeze(2), scalar1=kvalid.t[:, tile:tile + 1],
                                                                 scalar2=None, op0=ALU.mult), [ones8, kvalid], [VA])
                    for h in range(8):
                        kp = PSF()
                        for rc in range(2):
                            PE(lambda e, rc=rc, kp=kp, ck=ck, h=h: e.matmul(kp.t[0:64, :], wkv.t[:, rc, h * 64:(h + 1) * 64], ck.t[:, rc, :], start=(rc == 0),
                                                                             stop=(rc == 1)), [ck, wkv], [kp])
                        if h % 2 == 0:
                            ACT(lambda e, kp=kp, h=h, cs=cs: e.activation(out=KH.t[0:64, h, cs], in_=kp.t[0:64, :], func=AF.Copy), [kp], [KH])
                        else:
                            DVE(lambda e, kp=kp, h=h, cs=cs: e.tensor_copy(out=KH.t[0:64, h, cs], in_=kp.t[0:64, :]), [kp], [KH])
                    for (c0, c1, R, dst_is_kh) in ((256, 352, slice(64, 96), True), (448, 480, slice(0, 32), False)):
                        nr = 96 if dst_is_kh else 32
                        pa = PSF()
                        pr = PSF()
                        for kc in range(8):
                            PE(lambda e, kc=kc, pa=pa, u=u, c0=c0, nr=nr: e.matmul(pa.t[0:nr, :], wdi.t[:, kc, c0:c0 + nr], u.t[:, kc, :], start=(kc == 0),
                                                                                   stop=(kc == 7)), [u, wdi], [pa])
                        for kc in range(8):
                            PE(lambda e, kc=kc, pr=pr, u=u, c1=c1, nr=nr: e.matmul(pr.t[0:nr, :], wdi.t[:, kc, c1:c1 + nr], u.t[:, kc, :], start=(kc == 0),
                                                                                   stop=(kc == 7)), [u, wdi], [pr])
                        DVE(lambda e, pa=pa, R=R, cs=cs: e.tensor_tensor(out=r1.t[R, :], in0=pa.t[R, :], in1=cos32.t[R, cs], op=ALU.mult), [pa, cos32], [r1])
                        DVE(lambda e, pr=pr, R=R, cs=cs: e.tensor_tensor(out=r2.t[R, :], in0=pr.t[R, :], in1=sin32.t[R, cs], op=ALU.mult), [pr, sin32], [r2])
                        if dst_is_kh:
                            DVE(lambda e, R=R: e.tensor_tensor(out=r3.t[R, :], in0=r1.t[R, :], in1=r2.t[R, :], op=ALU.add), [r1, r2], [r3])
                            DVE(lambda e, R=R, cs=cs: e.tensor_copy(out=KH.t[R, :, cs], in_=r3.t[R, :].unsqueeze(1).to_broadcast([32, 8, 512])), [r3], [KH])
                        else:
                            DVE(lambda e, R=R, cs=cs: e.tensor_tensor(out=KI.t[R, cs], in0=r1.t[R, :], in1=r2.t[R, :], op=ALU.add), [r1, r2], [KI])
                fw.flush()
                ks.close()
            with ExitStack() as ms:
                ctr["n"] = 5
                wq = alloc(ms, "wq", [128, 3, 2048], BF16)
                LDC(lambda e: e.dma_start(out=wq.t[:], in_=w_q[:, :, :]), [], [wq])
                QH = [alloc(ms, "QH%d" % i, [96, 8, 128], BF16) for i in range(2)]
                QI = [alloc(ms, "QI%d" % i, [32, 8, 128], BF16) for i in range(2)]
                q1 = alloc(ms, "q1", [96, 4, 128], F32)
                q2 = alloc(ms, "q2", [96, 4, 128], F32)
                isc = alloc(ms, "isc", [128, T], F32)
                Rb = [alloc(ms, "Rb%d" % i, [128, 512], BF16) for i in range(3)]
                mk = alloc(ms, "mk", [128, T], BF16)
                MTs = [alloc(ms, "MT%d" % i, [128, NT, 128], BF16) for i in range(2)]
                mid = alloc(ms, "mid", [128, 1], F32)
                cnt = alloc(ms, "cnt", [128, BIS_ITERS], F32)
                stp = alloc(ms, "stp", [128, 1], F32)
                PTb = [alloc(ms, "P2_%d" % i, [128, 4, 128], BF16) for i in range(3)]
                rden = alloc(ms, "rden", [128, 8], F32)
                oab = alloc(ms, "oab", [128, 512], BF16)
                oat = [alloc(ms, "oat%d" % i, [128, 4, 128], BF16) for i in range(2)]
                attn_scale = float(96 ** -0.5)
                ri = 0
                ei = 0
                pi_ = 0
                def stageA(qt):
                    nonlocal ri
                    NKB = 17 + qt
                    N = NKB * 128
                    q0 = qt * 128
                    qtok = (16 + qt) * 128
                    qh = QH[qt % 2]
                    qi = QI[qt % 2]
                    MT = MTs[qt % 2]
                    for hg in range(2):
                        pa = PSF()
                        pr = PSF()
                        for hl in range(4):
                            h = hg * 4 + hl
                            for rc in range(3):
                                PE(lambda e, rc=rc, pa=pa, hl=hl, h=h, q0=q0: e.matmul(pa.t[0:96, hl * 128:(hl + 1) * 128], wq.t[:, rc, h * 96:(h + 1) * 96],
                                                                                    CQNT.t[:, rc, q0:q0 + 128], start=(rc == 0), stop=(rc == 2)), [wq, CQNT], [pa])
                            for rc in range(3):
                                PE(lambda e, rc=rc, pr=pr, hl=hl, h=h, q0=q0: e.matmul(pr.t[0:96, hl * 128:(hl + 1) * 128], wq.t[:, rc, 768 + h * 96:768 + (h + 1) * 96],
                                                                                    CQNT.t[:, rc, q0:q0 + 128], start=(rc == 0), stop=(rc == 2)), [wq, CQNT], [pr])
                        hsl = slice(hg * 4, hg * 4 + 4)
                        ACT(lambda e, pa=pa, qh=qh, hsl=hsl: e.activation(out=qh.t[0:64, hsl, :], in_=pa.t[0:64, :].rearrange("p (h q) -> p h q", h=4),
                                                                          func=AF.Copy), [pa], [qh])
                        R = slice(64, 96)
                        cosb = cosq.t[R, q0:q0 + 128].unsqueeze(1).to_broadcast([32, 4, 128])
                        sinb = sinq.t[R, q0:q0 + 128].unsqueeze(1).to_broadcast([32, 4, 128])
                        DVE(lambda e, pa=pa, cosb=cosb, R=R: e.tensor_tensor(out=q1.t[R, :, :], in0=pa.t[R, :].rearrange("p (h q) -> p h q", h=4), in1=cosb,
                                                                            op=ALU.mult), [pa, cosq], [q1])
                        DVE(lambda e, pr=pr, sinb=sinb, R=R: e.tensor_tensor(out=q2.t[R, :, :], in0=pr.t[R, :].rearrange("p (h q) -> p h q", h=4), in1=sinb,
                                                                            op=ALU.mult), [pr, sinq], [q2])
                        DVE(lambda e, qh=qh, hsl=hsl, R=R: e.tensor_tensor(out=qh.t[R, hsl, :], in0=q1.t[R, :, :], in1=q2.t[R, :, :], op=ALU.add), [q1, q2], [qh])
                        pa = PSF()
                        pr = PSF()
                        for hl in range(4):
                            h = hg * 4 + hl
                            for rc in range(3):
                                PE(lambda e, rc=rc, pa=pa, hl=hl, h=h, q0=q0: e.matmul(pa.t[0:32, hl * 128:(hl + 1) * 128], wq.t[:, rc, 1536 + h * 32:1536 + (h + 1) * 32],
                                                                                    CQNT.t[:, rc, q0:q0 + 128], start=(rc == 0), stop=(rc == 2)), [wq, CQNT], [pa])
                            for rc in range(3):
                                PE(lambda e, rc=rc, pr=pr, hl=hl, h=h, q0=q0: e.matmul(pr.t[0:32, hl * 128:(hl + 1) * 128], wq.t[:, rc, 1792 + h * 32:1792 + (h + 1) * 32],
                                                                                    CQNT.t[:, rc, q0:q0 + 128], start=(rc == 0), stop=(rc == 2)), [wq, CQNT], [pr])
                        R = slice(0, 32)
                        cosb = cosq.t[R, q0:q0 + 128].unsqueeze(1).to_broadcast([32, 4, 128])
                        sinb = sinq.t[R, q0:q0 + 128].unsqueeze(1).to_broadcast([32, 4, 128])
                        DVE(lambda e, pa=pa, cosb=cosb, R=R: e.tensor_tensor(out=q1.t[R, :, :], in0=pa.t[R, :].rearrange("p (h q) -> p h q", h=4), in1=cosb,
                                                                            op=ALU.mult), [pa, cosq], [q1])
                        DVE(lambda e, pr=pr, sinb=sinb, R=R: e.tensor_tensor(out=q2.t[R, :, :], in0=pr.t[R, :].rearrange("p (h q) -> p h q", h=4), in1=sinb,
                                                                            op=ALU.mult), [pr, sinq], [q2])
                        DVE(lambda e, qi=qi, hsl=hsl, R=R: e.tensor_tensor(out=qi.t[R, hsl, :], in0=q1.t[R, :, :], in1=q2.t[R, :, :], op=ALU.add), [q1, q2], [qi])
                    nch = (NKB + 3) // 4
                    for c in range(nch):
                        w_ = min(512, N - c * 512)
                        cs = slice(c * 512, c * 512 + w_)
                        for h in range(8):
                            lp = PSF()
                            PE(lambda e, lp=lp, qi=qi, h=h, cs=cs, w_=w_: e.matmul(lp.t[:, 0:w_], qi.t[0:32, h, :], KI.t[0:32, cs], start=True, stop=True),
                               [qi, KI], [lp])
                            rb = Rb[ri % 3]
                            ri += 1
                            ACT(lambda e, lp=lp, rb=rb, w_=w_: e.activation(out=rb.t[:, 0:w_], in_=lp.t[:, 0:w_], func=AF.Relu), [lp], [rb])
                            if h == 0:
                                sc2 = kb0.t[:, 0:1] if c < 4 else 0.0
                                DVE(lambda e, rb=rb, cs=cs, w_=w_, qt=qt, sc2=sc2: e.tensor_scalar(
                                    out=isc.t[:, cs], in0=rb.t[:, 0:w_], scalar1=WSC.t[:, qt, 0:1], scalar2=sc2, op0=ALU.mult, op1=ALU.add),
                                    [rb, WSC, kb0], [isc])
                            else:
                                DVE(lambda e, rb=rb, h=h, cs=cs, w_=w_, qt=qt: e.scalar_tensor_tensor(
                                    out=isc.t[:, cs], in0=rb.t[:, 0:w_], scalar=WSC.t[:, qt, h:h + 1], in1=isc.t[:, cs], op0=ALU.mult, op1=ALU.add),
                                    [rb, WSC, isc], [isc])
                    dsl = slice(N - 128, N)
                    DVE(lambda e, dsl=dsl: e.tensor_tensor(out=isc.t[:, dsl], in0=isc.t[:, dsl], in1=cm.t[:], op=ALU.add), [isc, cm], [isc])
                    DVE(lambda e: e.memset(mid.t[:], 0.0), [], [mid])
                    DVE(lambda e: e.memset(cnt.t[:], 0.0), [], [cnt])
                    wdt = BIS_W0
                    for it in range(BIS_ITERS):
                        wdt *= 0.5
                        DVE(lambda e, N=N, it=it: e.tensor_scalar(out=mk.t[:, 0:N], in0=isc.t[:, 0:N], scalar1=mid.t[:, 0:1], scalar2=0.0, op0=ALU.is_ge,
                                                                  op1=ALU.add, accum_out=cnt.t[:, it:it + 1]), [isc, mid], [mk, cnt])
                        DVE(lambda e, wdt=wdt, it=it: e.tensor_scalar(out=stp.t[:], in0=cnt.t[:, it:it + 1], scalar1=255.5, scalar2=2.0 * wdt, op0=ALU.is_ge,
                                                                      op1=ALU.mult), [cnt], [stp])
                        DVE(lambda e, wdt=wdt: e.scalar_tensor_tensor(out=mid.t[:], in0=stp.t[:], scalar=-wdt, in1=mid.t[:], op0=ALU.add,
                                                                      op1=ALU.add), [stp, mid], [mid])
                def stageA2(qt):
                    NKB = 17 + qt
                    N = NKB * 128
                    MT = MTs[qt % 2]
                    DVE(lambda e, N=N: e.tensor_scalar(out=mk.t[:, 0:N], in0=isc.t[:, 0:N], scalar1=mid.t[:, 0:1], scalar2=MASK_NEG, op0=ALU.is_lt,
                                                       op1=ALU.mult), [isc, mid], [mk])
                    for k0 in range(0, NKB, 8):
                        nk = min(8, NKB - k0)
                        pb = PSB()
                        for k in range(nk):
                            PE(lambda e, k=k, k0=k0, pb=pb: e.transpose(pb.t[:, k * 128:(k + 1) * 128], mk.t[:, (k0 + k) * 128:(k0 + k + 1) * 128], ident.t[:]),
                               [mk, ident], [pb])
                        ACT(lambda e, pb=pb, k0=k0, nk=nk: e.activation(out=MT.t[:, k0:k0 + nk, :], in_=pb.t[:, 0:nk * 128].rearrange("p (k q) -> p k q", k=nk),
                                                                        func=AF.Copy), [pb], [MT])
                def stageB(qt):
                    nonlocal pi_
                    NKB = 17 + qt
                    q0 = qt * 128
                    qh = QH[qt % 2]
                    MT = MTs[qt % 2]
                    accp = [psf[5], psf[6]]
                    steps = [(kb, hg) for kb in range(NKB) for hg in range(2)]

                    def emit_scores(kb, hg):
                        ks_ = slice(kb * 128, (kb + 1) * 128)
                        sp_ = PSF()
                        for hl in range(4):
                            h = hg * 4 + hl
                            PE(lambda e, sp_=sp_, hl=hl, h=h, ks_=ks_: e.matmul(sp_.t[:, hl * 128:(hl + 1) * 128], KH.t[0:96, h, ks_], qh.t[0:96, h, :],
                                                                                 start=(hl == 0), stop=False), [KH, qh], [sp_])
                        for hl in range(4):
                            PE(lambda e, sp_=sp_, hl=hl, kb=kb: e.matmul(sp_.t[:, hl * 128:(hl + 1) * 128], ident.t[:], MT.t[:, kb, :], start=False, stop=True),
                               [ident, MT], [sp_])
                        return sp_

                    sp_next = emit_scores(*steps[0])
                    for si_, (kb, hg) in enumerate(steps):
                        sp_ = sp_next
                        if si_ + 1 < len(steps):
                            sp_next = emit_scores(*steps[si_ + 1])
                        P_ = PTb[pi_ % 3]
                        pi_ += 1
                        ACT(lambda e, P_=P_, sp_=sp_: e.activation(out=P_.t[:], in_=sp_.t[:].rearrange("p (h q) -> p h q", h=4), func=AF.Exp,
                                                                    scale=attn_scale), [sp_], [P_])
                        ap_ = accp[hg]
                        for hl in range(4):
                            h = hg * 4 + hl
                            PE(lambda e, ap_=ap_, hl=hl, h=h, kb=kb, P_=P_, NKB=NKB: e.matmul(ap_.t[:, hl * 65:(hl + 1) * 65], P_.t[:, hl, :], VA.t[:, kb, h, :],
                                                                                           start=(kb == 0 and hl == 0), stop=(kb == NKB - 1)), [P_, VA], [ap_])
                    for hg in range(2):
                        ap_ = accp[hg]
                        av = ap_.t[:, 0:260].rearrange("p (h e) -> p h e", h=4)
                        DVE(lambda e, av=av, hg=hg: e.reciprocal(out=rden.t[:, hg * 4:(hg + 1) * 4], in_=av[:, :, 64]), [ap_], [rden])
                        for hl in range(4):
                            h = hg * 4 + hl
                            ACT(lambda e, av=av, hl=hl, h=h: e.activation(out=oab.t[:, h * 64:(h + 1) * 64], in_=av[:, hl, 0:64], func=AF.Copy,
                                                                          scale=rden.t[:, h:h + 1]), [ap_, rden], [oab])
                    ot = oat[qt % 2]
                    transpose_to(oab, 4, ot.t[:], ot, eng_copy="dve")
                    LD(lambda e, ot=ot, q0=q0: e.dma_start(out=oaTS[:, :, q0:q0 + 128], in_=ot.t[:]), [ot], [oaTS_b])
                stageA(0)
                stageA2(0)
                for qt in range(16):
                    if qt + 1 < 16:
                        stageA(qt + 1)
                    stageB(qt)
                    if qt + 1 < 16:
                        stageA2(qt + 1)
                    if qt % 2 == 1:
                        fw.flush()
                fw.wait_all("sp", [oaTS_b])
                fw.flush()
                ctr["n"] = 7

        with ExitStack() as ph:
            wg_ = alloc(ph, "wgates", [128, 8, 2048], BF16)
            wua = alloc(ph, "wua", [128, 4, D], BF16)
            wub = alloc(ph, "wub", [64, 4, D], BF16)
            wo_ = alloc(ph, "wo", [128, 8, D], BF16)
            for kc in range(8):
                LDC(lambda e, kc=kc: e.dma_start(out=wg_.t[:, kc, :], in_=w_gates[:, kc, :]), [], [wg_])
            LDC(lambda e: e.dma_start(out=wua.t[:], in_=w_upa[:, :, :]), [], [wua])
            LDC(lambda e: e.dma_start(out=wub.t[:], in_=w_upb[:, :, :]), [], [wub])
            for kc in range(0, 8, 2):
                LDC(lambda e, kc=kc: e.dma_start(out=wo_.t[:, kc:kc + 2, :], in_=w_o[:, kc:kc + 2, :]), [], [wo_])
            cp2 = load_mod(ph, "cp2", 5)
            sh3 = load_mod(ph, "sh3", 6)
            gm3 = load_mod(ph, "gm3", 7)
            uT = [alloc(ph, "muT%d" % i, [128, 8, 128], BF16) for i in range(2)]
            oaT = [alloc(ph, "moa%d" % i, [128, 4, 128], BF16) for i in range(2)]
            obT = [alloc(ph, "mob%d" % i, [64, 4, 128], BF16) for i in range(2)]
            x1r = [alloc(ph, "mx1%d" % i, [128, D], F32) for i in range(2)]
            sga = alloc(ph, "sga", [128, D], F32)
            sgb = alloc(ph, "sgb", [128, D], F32)
            zf = alloc(ph, "zf", [128, D], F32)
            zb = alloc(ph, "zb", [128, D], BF16)
            zT = alloc(ph, "zT", [128, 8, 128], BF16)
            tmpf = alloc(ph, "mtmp", [128, D], F32)
            x2t = alloc(ph, "x2t", [128, D], F32)
            hb = alloc(ph, "mhb", [128, D], BF16)
            h2t = [alloc(ph, "h2t%d" % i, [128, 8, 128], BF16) for i in range(2)]
            junk = alloc(ph, "mjunk", [128, D], BF16)
            ssr = [alloc(ph, "mss%d" % i, [128, 2], F32) for i in range(4)]
            rsr = [alloc(ph, "mrs%d" % i, [128, 1], F32) for i in range(4)]
            si = 0
            for qt in range(16):
                u = uT[qt % 2]
                oa = oaT[qt % 2]
                ob = obT[qt % 2]
                x1 = x1r[qt % 2]
                tok = (16 + qt) * 128
                q0 = qt * 128
                LD(lambda e, u=u, tok=tok: e.dma_start(out=u.t[:], in_=uTS[:, :, tok:tok + 128]), [uTS_b], [u])
                LD(lambda e, oa=oa, q0=q0: e.dma_start(out=oa.t[:], in_=oaTS[:, :, q0:q0 + 128]), [oaTS_b], [oa])
                LD(lambda e, ob=ob, q0=q0: e.dma_start(out=ob.t[:], in_=obTS[:, :, q0:q0 + 128]), [obTS_b], [ob])
                LD(lambda e, x1=x1, q0=q0: e.dma_start(out=x1.t[:], in_=x1S[q0:q0 + 128, :]), [x1S_b], [x1])
                for half in range(2):
                    hs = slice(half * 512, (half + 1) * 512)
                    pga = PSF()
                    pgb = PSF()
                    for kc in range(8):
                        PE(lambda e, kc=kc, pga=pga, u=u, half=half: e.matmul(pga.t[:], u.t[:, kc, :], wg_.t[:, kc, half * 512:(half + 1) * 512], start=(kc == 0),
                                                                               stop=(kc == 7)), [u, wg_], [pga])
                    for kc in range(8):
                        PE(lambda e, kc=kc, pgb=pgb, u=u, half=half: e.matmul(pgb.t[:], u.t[:, kc, :], wg_.t[:, kc, 1024 + half * 512:1024 + (half + 1) * 512],
                                                                               start=(kc == 0), stop=(kc == 7)), [u, wg_], [pgb])
                    ACT(lambda e, pga=pga, hs=hs: e.activation(out=sga.t[:, hs], in_=pga.t[:], func=AF.Sigmoid), [pga], [sga])
                    ACT(lambda e, pgb=pgb, hs=hs: e.activation(out=sgb.t[:, hs], in_=pgb.t[:], func=AF.Sigmoid), [pgb], [sgb])
                    pza = PSF()
                    pzb = PSF()
                    for c4 in range(4):
                        PE(lambda e, c4=c4, pza=pza, oa=oa, hs=hs: e.matmul(pza.t[:], oa.t[:, c4, :], wua.t[:, c4, hs], start=(c4 == 0), stop=(c4 == 3)),
                           [oa, wua], [pza])
                    for c4 in range(4):
                        PE(lambda e, c4=c4, pzb=pzb, ob=ob, hs=hs: e.matmul(pzb.t[:], ob.t[0:64, c4, :], wub.t[0:64, c4, hs], start=(c4 == 0), stop=(c4 == 3)),
                           [ob, wub], [pzb])
                    DVE(lambda e, pza=pza, hs=hs: e.tensor_tensor(out=zf.t[:, hs], in0=sga.t[:, hs], in1=pza.t[:], op=ALU.mult), [sga, pza], [zf])
                    DVE(lambda e, pzb=pzb, hs=hs: e.tensor_tensor(out=tmpf.t[:, hs], in0=sgb.t[:, hs], in1=pzb.t[:], op=ALU.mult), [sgb, pzb], [tmpf])
                    DVE(lambda e, hs=hs: e.tensor_tensor(out=zb.t[:, hs], in0=zf.t[:, hs], in1=tmpf.t[:, hs], op=ALU.add), [zf, tmpf], [zb])
                transpose_to(zb, 8, zT.t[:], zT)
                yps = [PSF(), PSF()]
                for half in range(2):
                    yp = yps[half]
                    for kc in range(8):
                        PE(lambda e, kc=kc, yp=yp, half=half: e.matmul(yp.t[:], zT.t[:, kc, :], wo_.t[:, kc, half * 512:(half + 1) * 512], start=(kc == 0),
                                                                        stop=(kc == 7)), [zT, wo_], [yp])
                ss = ssr[si % 4]
                si += 1
                DVE(lambda e, ss=ss: e.memset(ss.t[:], 0.0), [], [ss])
                for half in range(2):
                    ACT(lambda e, half=half, ss=ss, yp=yps[half]: e.activation(out=junk.t[:, 0:512], in_=yp.t[:], func=AF.Square, accum_out=ss.t[:, half:half + 1]),
                        [yps[half]], [junk, ss])
                DVE(lambda e, ss=ss: e.tensor_tensor(out=ss.t[:, 0:1], in0=ss.t[:, 0:1], in1=ss.t[:, 1:2], op=ALU.add), [ss], [ss])
                rs = rstd_of(rsr, ss.t[:, 0:1], ss, D)
                for half in range(2):
                    hs = slice(half * 512, (half + 1) * 512)
                    DVE(lambda e, hs=hs, rs=rs, yp=yps[half]: e.scalar_tensor_tensor(out=tmpf.t[:, hs], in0=yp.t[:], scalar=rs.t[:, 0:1], in1=cp2.t[:, hs],
                                                                                     op0=ALU.mult, op1=ALU.mult), [yps[half], rs, cp2], [tmpf])
                DVE(lambda e, x1=x1: e.tensor_tensor(out=x2t.t[:], in0=tmpf.t[:], in1=x1.t[:], op=ALU.add), [tmpf, x1], [x2t])
                LD(lambda e, q0=q0: e.dma_start(out=x2S[q0:q0 + 128, :], in_=x2t.t[:]), [x2t], [x2S_b])
                ss = ssr[si % 4]
                si += 1
                DVE(lambda e, ss=ss: e.memset(ss.t[:], 0.0), [], [ss])
                ACT(lambda e, ss=ss: e.activation(out=junk.t[:], in_=x2t.t[:], func=AF.Square, accum_out=ss.t[:, 0:1]), [x2t], [junk, ss])
                rs = rstd_of(rsr, ss.t[:, 0:1], ss, D)
                DVE(lambda e, rs=rs: e.scalar_tensor_tensor(out=tmpf.t[:], in0=x2t.t[:], scalar=rs.t[:, 0:1], in1=gm3.t[:], op0=ALU.mult, op1=ALU.mult),
                    [x2t, rs, gm3], [tmpf])
                DVE(lambda e: e.tensor_tensor(out=hb.t[:], in0=tmpf.t[:], in1=sh3.t[:], op=ALU.add), [tmpf, sh3], [hb])
                ht = h2t[qt % 2]
                transpose_to(hb, 8, ht.t[:], ht)
                LD(lambda e, ht=ht, q0=q0: e.dma_start(out=h2TS[:, :, q0:q0 + 128], in_=ht.t[:]), [ht], [h2TS_b])
            fw.wait_all("sp", [x2S_b, h2TS_b])
            fw.flush()

        ffn_phase(False)
    return nc


def _tile_k(w):
    K, N = w.shape
    return np.ascontiguousarray(w.reshape(K // 128, 128, N).transpose(1, 0, 2))


def _swap(w, half):
    return np.concatenate([w[..., half:2 * half], w[..., :half]], axis=-1)


_NC_CACHE = {}


def _prep_shared(inp):
    f = np.float32
    A = {}
    w_mod = inp["w_mod"][0]
    A["wmod_t"] = np.ascontiguousarray(w_mod.reshape(8, 128, 18, 512).transpose(2, 1, 0, 3))
    A["bmod"] = np.ascontiguousarray(inp["b_mod"][0:1])
    A["gv"] = np.ascontiguousarray(np.stack([inp["g_pre_ffn1"][0], inp["g_post_ffn1"][0], inp["g_pre_mix"][0], inp["g_post_mix"][0],
                                             inp["g_pre_ffn2"][0], inp["g_post_ffn2"][0]]).astype(f))
    A["gcq"] = np.ascontiguousarray(inp["g_cq"][0:1])
    A["gckv"] = np.ascontiguousarray(inp["g_ckv"][0:1])
    for i, (g, u, dn) in enumerate((("w_gate1", "w_up1", "w_down1"), ("w_gate2", "w_up2", "w_down2"))):
        wg = inp[g][0].reshape(8, 128, NF, 128)
        wu = inp[u][0].reshape(8, 128, NF, 128)
        wgu = np.stack([wg, wu], 0)
        A["wgu%d" % (i + 1)] = np.ascontiguousarray(wgu.transpose(3, 2, 0, 1, 4))
        A["wd%d" % (i + 1)] = _tile_k(inp[dn][0])
    w_in = inp["w_in"][0]
    z64 = np.zeros((D, 64), f)
    kr = w_in[:, 640:672]
    ki = w_in[:, 672:704]
    cols = [w_in[:, 384:640], np.concatenate([z64, kr], 1), np.concatenate([z64, _swap(kr, 16)], 1), ki, _swap(ki, 16), w_in[:, 704:712],
            w_in[:, 0:384]]
    A["w_dsa_in"] = _tile_k(np.concatenate(cols, 1))
    w_uq = inp["w_uq"][0]
    uq_rot = np.concatenate([np.zeros((384, 8, 64), f), _swap(w_uq[:, :, 64:96], 16)], -1)
    w_iq = inp["w_iq"][0]
    A["w_q"] = _tile_k(np.concatenate([w_uq.reshape(384, 768), uq_rot.reshape(384, 768), w_iq.reshape(384, 256),
                                       _swap(w_iq, 16).reshape(384, 256)], 1))
    A["w_kv"] = _tile_k(np.concatenate([inp["w_uk"][0].reshape(256, 512), inp["w_uv"][0].reshape(256, 512)], 1))
    wd = []
    for g in range(3):
        parts = []
        for j in range(3):
            base = 712 + (j * 3 + g) * 256
            wj = w_in[:, base:base + 256].reshape(D, 4, 64)
            parts.append(wj.reshape(D, 256))
            if j < 2:
                parts.append(_swap(wj, 32).reshape(D, 256))
        wd.append(_tile_k(np.concatenate(parts, 1)))
    A["w_dil"] = np.ascontiguousarray(np.stack(wd, 0))
    A["w_gates"] = _tile_k(w_in[:, 3016:5064])
    A["w_upa"] = _tile_k(inp["w_up_a"][0])
    A["w_upb"] = np.ascontiguousarray(inp["w_up_b"][0].reshape(4, 64, D).transpose(1, 0, 2))
    A["w_o"] = _tile_k(inp["w_o"][0])
    A["ident"] = np.eye(128, dtype=f)
    p = np.arange(128)
    cvec = np.zeros((128, 8), f)
    cvec[:, 0] = THETA ** (-(p % 32).astype(np.float64) / 32.0)
    cvec[:, 1] = np.where((p % 64) < 32, -1.0, 1.0)
    cvec[:, 2] = THETA ** (-(p % 16).astype(np.float64) / 16.0)
    cvec[:, 3] = np.where((p % 32) < 16, -1.0, 1.0)
    A["cvec"] = cvec
    s = p[:, None]
    q = p[None, :]
    A["tri"] = np.concatenate([(s >= q), (s <= q)], 1).astype(f)
    A["cm"] = np.where(p[None, :] <= p[:, None], 0.0, NEG).astype(f)
    return {k: np.ascontiguousarray(v, dtype=f) for k, v in A.items()}


def _run(inputs, debug=False):
    inp = {k: np.asarray(v) for k, v in inputs.items()}
    shared = _prep_shared(inp)
    x = inp["x"].astype(np.float32)
    c = inp["c"].astype(np.float32)
    pos = inp["positions"].astype(np.int32)
    in_maps = []
    for core in range(8):
        b, j = core // 2, core % 2
        m = dict(shared)
        if j == 1:
            xl = x[b]
            pl = pos[b]
            kval = np.ones((128, NT), np.float32)
            kb = np.zeros((1, T), np.float32)
        else:
            xl = np.concatenate([x[b, 2048:], x[b, :2048]], 0)
            pl = np.concatenate([pos[b, 2048:], pos[b, :2048]], 0)
            kval = np.ones((128, NT), np.float32)
            kval[:, :16] = 0.0
            kb = np.zeros((1, T), np.float32)
            kb[:, :2048] = NEG
        m["x_l"] = np.ascontiguousarray(xl)
        m["pos_l"] = np.ascontiguousarray(pl.reshape(1, T))
        m["c_col"] = np.ascontiguousarray(c[b].reshape(8, 128).T)
        m["kvalid"] = kval
        m["kbias"] = kb
        in_maps.append(m)
    key = bool(debug)
    if key not in _NC_CACHE:
        _NC_CACHE[key] = build_program(debug=debug)
    nc = _NC_CACHE[key]
    res = run_bass_kernel_spmd(nc, in_maps, core_ids=list(range(8)))
    out = np.zeros((4, T, D), np.float32)
    for core in range(8):
        b, j = core // 2, core % 2
        o = np.asarray(res.results[core]["out_l"], dtype=np.float32)
        if j == 1:
            out[b, 2048:] = o
        else:
            out[b, :2048] = o
    return out, res


def kernel(**inputs):
    out, _ = _run(inputs, debug=False)
    return out
```

```python
import numpy as np
from contextlib import ExitStack
import concourse.bass as bass
import concourse.mybir as mybir
from concourse.bass_utils import run_bass_kernel_spmd

F32 = mybir.dt.float32
BF16 = mybir.dt.bfloat16
I32 = mybir.dt.int32
ALU = mybir.AluOpType
AF = mybir.ActivationFunctionType

D = 1024
T = 4096
NT = 32
DFF = 2816
NF = 22
EPS = 1e-6
THETA = 10000.0
SEM_EPOCH = 12000
PI = float(np.pi)
TWO_PI = 2.0 * PI
C_HI = float(np.float32(6.28125))
C_LO = float(TWO_PI - 6.28125)
NEG = -1.0e30
BIS_ITERS = 16
BIS_W0 = 8.0
MASK_NEG = -30000.0


class Buf:
    __slots__ = ("name", "last_w", "readers")

    def __init__(self, name):
        self.name = name
        self.last_w = None
        self.readers = {}


class Tn:
    __slots__ = ("t", "b")

    def __init__(self, t, name):
        self.t = t
        self.b = Buf(name)


class Eng:
    def __init__(self, name):
        self.name = name
        self.is_pe = name == "pe"
        self.count = 0
        self.epoch = 0
        self.known = {}
        self.thunks = []
        self.dma_slot = 0
        self.dma_uses = {}


class FW:
    def __init__(self, nc, stack, n_dma_sems=12):
        self.nc = nc
        self.stack = stack
        self.engs = {n: Eng(n) for n in ("pe", "act", "dve", "pool", "sp")}
        self.sems = {}
        self.n_dma_sems = n_dma_sems

    def _sem(self, key):
        if key not in self.sems:
            self.sems[key] = self.stack.enter_context(self.nc.semaphore("s_%s_%d" % key))
        return self.sems[key]

    def _collect(self, E, reads, writes, skip_self_pe=False):
        waits = []

        def need(ev):
            key, val = ev
            if skip_self_pe and key[0] == "pe":
                return
            if E.known.get(key, 0) >= val:
                return
            E.known[key] = val
            waits.append((key, val))

        for b in reads:
            if b.last_w is not None:
                need(b.last_w)
        for b in writes:
            if b.last_w is not None:
                need(b.last_w)
            for k, v in b.readers.items():
                need((k, v))
        return waits

    def _record(self, ev, reads, writes):
        key, val = ev
        for b in reads:
            if b.readers.get(key, 0) < val:
                b.readers[key] = val
        for b in writes:
            b.last_w = ev
            b.readers = {}

    def op(self, eng, fn, reads=(), writes=(), pe_acc=False):
        E = self.engs[eng]
        reads = [x.b if isinstance(x, Tn) else x for x in reads]
        writes = [x.b if isinstance(x, Tn) else x for x in writes]
        waits = self._collect(E, reads, writes, skip_self_pe=(E.is_pe and pe_acc))
        if E.count >= SEM_EPOCH:
            E.epoch += 1
            E.count = 0
        E.count += 1
        key = (E.name, E.epoch)
        ev = (key, E.count)
        E.thunks.append((waits, fn, (key, 1)))
        self._record(ev, reads, writes)

    def dma(self, eng, fn, reads=(), writes=()):
        E = self.engs[eng]
        reads = [x.b if isinstance(x, Tn) else x for x in reads]
        writes = [x.b if isinstance(x, Tn) else x for x in writes]
        waits = self._collect(E, reads, writes)
        slot = E.dma_slot
        E.dma_slot = (E.dma_slot + 1) % self.n_dma_sems
        key = ("dma_" + eng, slot)
        uses = E.dma_uses.get(slot, 0)
        if uses > 0 and E.known.get(key, 0) < 16 * uses:
            waits.append((key, 16 * uses))
            E.known[key] = 16 * uses
        uses += 1
        E.dma_uses[slot] = uses
        ev = (key, 16 * uses)
        E.thunks.append((waits, fn, (key, 16)))
        self._record(ev, reads, writes)

    def wait_all(self, eng, bufs):
        E = self.engs[eng]
        bufs = [x.b if isinstance(x, Tn) else x for x in bufs]
        waits = self._collect(E, bufs, ())
        E.thunks.append((waits, None, None))

    def flush(self):
        nc = self.nc
        for E in self.engs.values():
            for waits, fn, inc in E.thunks:
                for key, _ in waits:
                    self._sem(key)
                if inc is not None:
                    self._sem(inc[0])
        sems = self.sems
        with nc.Block() as block:
            def replay(E):
                def run(eo):
                    for waits, fn, inc in E.thunks:
                        for key, val in waits:
                            eo.wait_ge(sems[key], val)
                        if fn is not None:
                            fn(eo).then_inc(sems[inc[0]], inc[1])
                return run

            block.tensor(replay(self.engs["pe"]))
            block.scalar(replay(self.engs["act"]))
            block.vector(replay(self.engs["dve"]))
            block.gpsimd(replay(self.engs["pool"]))
            block.sync(replay(self.engs["sp"]))
        for E in self.engs.values():
            E.thunks = []


def build_program(debug=False):
    nc = bass.Bass("TRN2", target_bir_lowering=False)

    def din(name, shape, dt=F32):
        return nc.dram_tensor(name, list(shape), dt, kind="ExternalInput").ap()

    def dscr(name, shape, dt):
        return nc.dram_tensor(name, list(shape), dt, kind=("ExternalOutput" if debug else "Internal")).ap()

    x_l = din("x_l", [T, D])
    pos_l = din("pos_l", [1, T], I32)
    c_col = din("c_col", [128, 8])
    kvalid_d = din("kvalid", [128, NT])
    kbias_d = din("kbias", [1, T])
    wmod_t = din("wmod_t", [18, 128, 8, 512])
    bmod = din("bmod", [1, 9 * D])
    gv = din("gv", [6, D])
    gcq = din("gcq", [1, 384])
    gckv = din("gckv", [1, 256])
    wgu1 = din("wgu1", [NF, 128, 2, 8, 128])
    wd1 = din("wd1", [128, NF, D])
    wgu2 = din("wgu2", [NF, 128, 2, 8, 128])
    wd2 = din("wd2", [128, NF, D])
    w_dsa_in = din("w_dsa_in", [128, 8, 904])
    w_q = din("w_q", [128, 3, 2048])
    w_kv = din("w_kv", [128, 2, 1024])
    w_dil = din("w_dil", [3, 128, 8, 1280])
    w_gates = din("w_gates", [128, 8, 2048])
    w_upa = din("w_upa", [128, 4, D])
    w_upb = din("w_upb", [64, 4, D])
    w_o = din("w_o", [128, 8, D])
    ident_d = din("ident", [128, 128])
    cvec_d = din("cvec", [128, 8])
    tri_d = din("tri", [128, 256])
    cm_d = din("cm", [128, 128])
    out_l = nc.dram_tensor("out_l", [2048, D], F32, kind="ExternalOutput").ap()

    modS = dscr("modS", [128, 9 * D], F32)
    x1S = dscr("x1S", [2048, D], F32)
    uTS = dscr("uTS", [128, 8, T], BF16)
    x2S = dscr("x2S", [2048, D], F32)
    h2TS = dscr("h2TS", [128, 8, 2048], BF16)
    obTS = dscr("obTS", [64, 4, 2048], BF16)
    oaTS = dscr("oaTS", [128, 4, 2048], BF16)
    cos64S = dscr("cos64S", [128, T], BF16)
    sin64S = dscr("sin64S", [128, T], BF16)
    cos32S = dscr("cos32S", [96, T], BF16)
    sin32S = dscr("sin32S", [96, T], BF16)

    with ExitStack() as top:
        fw = FW(nc, top)

        uniq = {"n": 0}

        def alloc(st, name, shape, dt):
            uniq["n"] += 1
            nm = "sb%d_%s" % (uniq["n"], name)
            return Tn(st.enter_context(nc.sbuf_tensor(nm, list(shape), dt)), nm)

        psf = [Tn(top.enter_context(nc.psum_tensor("psf%d" % i, [128, 512], F32)), "psf%d" % i) for i in range(7)]
        psb = [Tn(top.enter_context(nc.psum_tensor("psb%d" % i, [128, 1024], BF16)), "psb%d" % i) for i in range(1)]
        ctr = {"f": 0, "n": 7}

        def PSF():
            p = psf[ctr["f"] % ctr["n"]]
            ctr["f"] += 1
            return p

        def PSB():
            return psb[0]

        def PE(fn, r, w):
            fw.op("pe", fn, r, w, pe_acc=True)

        def ACT(fn, r, w):
            fw.op("act", fn, r, w)

        def DVE(fn, r, w):
            fw.op("dve", fn, r, w)

        def POOL(fn, r, w):
            fw.op("pool", fn, r, w)

        def LD(fn, r, w):
            fw.dma("sp", fn, r, w)

        def LDC(fn, r, w):
            fw.dma("pool", fn, r, w)

        ident = alloc(top, "ident", [128, 128], BF16)
        cvec = alloc(top, "cvec", [128, 8], F32)
        kvalid = alloc(top, "kvalid_sb", [128, NT], F32)
        ones8 = alloc(top, "ones8", [128, 8], F32)
        epsc = alloc(top, "epsc", [128, 1], F32)
        LDC(lambda e: e.dma_start(out=ident.t[:], in_=ident_d[:, :]), [], [ident])
        LD(lambda e: e.dma_start(out=cvec.t[:], in_=cvec_d[:, :]), [], [cvec])
        LD(lambda e: e.dma_start(out=kvalid.t[:], in_=kvalid_d[:, :]), [], [kvalid])
        DVE(lambda e: e.memset(ones8.t[:], 1.0), [], [ones8])
        DVE(lambda e: e.memset(epsc.t[:], EPS), [], [epsc])

        small = {"i": 0}

        def rstd_of(st_pool, ss_ap, ss_tn, n):
            r = st_pool[small["i"] % len(st_pool)]
            small["i"] += 1
            ACT(lambda e: e.activation(out=r.t[:], in_=ss_ap, func=AF.Sqrt, scale=1.0 / n, bias=epsc.t[:]), [ss_tn, epsc], [r])
            DVE(lambda e: e.reciprocal(out=r.t[:], in_=r.t[:]), [r], [r])
            return r

        def transpose_to(src, ncols_chunks, dst_ap, dst_tn, eng_copy="act"):
            pb = PSB()
            for k in range(ncols_chunks):
                PE(lambda e, k=k: e.transpose(pb.t[:, k * 128:(k + 1) * 128], src.t[:, k * 128:(k + 1) * 128], ident.t[:]),
                   [src, ident], [pb])
            view = pb.t[:, 0:ncols_chunks * 128].rearrange("p (k t) -> p k t", k=ncols_chunks)
            if eng_copy == "act":
                ACT(lambda e: e.activation(out=dst_ap, in_=view, func=AF.Copy), [pb], [dst_tn])
            else:
                DVE(lambda e: e.tensor_copy(out=dst_ap, in_=view), [pb], [dst_tn])

        tabS_b = Buf("tabS")

        def rope_chunk(tp_, nrows, inv_col, sgn_col, cosS, sinS, c):
            posi, xs, kf, ki, ang = tp_["posi"], tp_["xs"], tp_["kf"], tp_["ki"], tp_["ang"]
            R = slice(0, nrows)
            cs = slice(c * 2048, (c + 1) * 2048)
            LD(lambda e: e.dma_start(out=posi.t[R, :], in_=pos_l[0:1, cs].to_broadcast([nrows, 2048])), [], [posi])
            DVE(lambda e: e.tensor_copy(out=ang.t[R, :], in_=posi.t[R, :]), [posi], [ang])
            DVE(lambda e: e.tensor_scalar(out=ang.t[R, :], in0=ang.t[R, :], scalar1=cvec.t[R, inv_col:inv_col + 1], scalar2=None,
                                          op0=ALU.mult), [ang, cvec], [ang])
            for which in range(2):
                off = 0.0 if which == 1 else PI / 2.0
                dstS = sinS if which == 1 else cosS
                ot = tp_["ot"][which]
                DVE(lambda e, off=off: e.tensor_scalar(out=xs.t[R, :], in0=ang.t[R, :], scalar1=off, scalar2=None, op0=ALU.add), [ang], [xs])
                DVE(lambda e: e.tensor_scalar(out=kf.t[R, :], in0=xs.t[R, :], scalar1=1.0 / TWO_PI, scalar2=None, op0=ALU.mult), [xs], [kf])
                DVE(lambda e: e.tensor_copy(out=ki.t[R, :], in_=kf.t[R, :]), [kf], [ki])
                DVE(lambda e: e.tensor_copy(out=kf.t[R, :], in_=ki.t[R, :]), [ki], [kf])
                DVE(lambda e: e.scalar_tensor_tensor(out=xs.t[R, :], in0=kf.t[R, :], scalar=-C_HI, in1=xs.t[R, :], op0=ALU.mult, op1=ALU.add),
                    [kf, xs], [xs])
                DVE(lambda e: e.scalar_tensor_tensor(out=xs.t[R, :], in0=kf.t[R, :], scalar=-C_LO, in1=xs.t[R, :], op0=ALU.mult, op1=ALU.add),
                    [kf, xs], [xs])
                DVE(lambda e: e.tensor_scalar(out=kf.t[R, :], in0=xs.t[R, :], scalar1=PI, scalar2=-TWO_PI, op0=ALU.is_gt, op1=ALU.mult), [xs], [kf])
                DVE(lambda e: e.tensor_tensor(out=xs.t[R, :], in0=xs.t[R, :], in1=kf.t[R, :], op=ALU.add), [xs, kf], [xs])
                DVE(lambda e: e.tensor_scalar(out=kf.t[R, :], in0=xs.t[R, :], scalar1=-PI, scalar2=TWO_PI, op0=ALU.is_lt, op1=ALU.mult), [xs], [kf])
                DVE(lambda e: e.tensor_tensor(out=xs.t[R, :], in0=xs.t[R, :], in1=kf.t[R, :], op=ALU.add), [xs, kf], [xs])
                ACT(lambda e: e.activation(out=xs.t[R, :], in_=xs.t[R, :], func=AF.Sin), [xs], [xs])
                if which == 1:
                    DVE(lambda e, ot=ot: e.tensor_scalar(out=ot.t[R, :], in0=xs.t[R, :], scalar1=cvec.t[R, sgn_col:sgn_col + 1], scalar2=None,
                                                         op0=ALU.mult), [xs, cvec], [ot])
                else:
                    DVE(lambda e, ot=ot: e.tensor_copy(out=ot.t[R, :], in_=xs.t[R, :]), [xs], [ot])
                LD(lambda e, ot=ot, dstS=dstS: e.dma_start(out=dstS[0:nrows, cs], in_=ot.t[R, :]), [ot], [tabS_b])

        def rope_tables(st, nrows, inv_col, sgn_col, cos_t, sin_t):
            with ExitStack() as tmp:
                posi = alloc(tmp, "posi", [128, 1024], I32)
                xs = alloc(tmp, "rt_xs", [128, 1024], F32)
                kf = alloc(tmp, "rt_kf", [128, 1024], F32)
                ki = alloc(tmp, "rt_ki", [128, 1024], I32)
                ang = alloc(tmp, "rt_ang", [128, 1024], F32)
                R = slice(0, nrows)
                for c in range(4):
                    cs = slice(c * 1024, (c + 1) * 1024)
                    LD(lambda e, cs=cs: e.dma_start(out=posi.t[R, :], in_=pos_l[0:1, cs].to_broadcast([nrows, 1024])), [], [posi])
                    DVE(lambda e: e.tensor_copy(out=ang.t[R, :], in_=posi.t[R, :]), [posi], [ang])
                    DVE(lambda e: e.tensor_scalar(out=ang.t[R, :], in0=ang.t[R, :], scalar1=cvec.t[R, inv_col:inv_col + 1], scalar2=None,
                                                  op0=ALU.mult), [ang, cvec], [ang])
                    for which in range(2):
                        off = 0.0 if which == 1 else PI / 2.0
                        dst = sin_t if which == 1 else cos_t
                        DVE(lambda e, off=off: e.tensor_scalar(out=xs.t[R, :], in0=ang.t[R, :], scalar1=off, scalar2=None, op0=ALU.add),
                            [ang], [xs])
                        DVE(lambda e: e.tensor_scalar(out=kf.t[R, :], in0=xs.t[R, :], scalar1=1.0 / TWO_PI, scalar2=None, op0=ALU.mult),
                            [xs], [kf])
                        DVE(lambda e: e.tensor_copy(out=ki.t[R, :], in_=kf.t[R, :]), [kf], [ki])
                        DVE(lambda e: e.tensor_copy(out=kf.t[R, :], in_=ki.t[R, :]), [ki], [kf])
                        DVE(lambda e: e.scalar_tensor_tensor(out=xs.t[R, :], in0=kf.t[R, :], scalar=-C_HI, in1=xs.t[R, :], op0=ALU.mult,
                                                             op1=ALU.add), [kf, xs], [xs])
                        DVE(lambda e: e.scalar_tensor_tensor(out=xs.t[R, :], in0=kf.t[R, :], scalar=-C_LO, in1=xs.t[R, :], op0=ALU.mult,
                                                             op1=ALU.add), [kf, xs], [xs])
                        DVE(lambda e: e.tensor_scalar(out=kf.t[R, :], in0=xs.t[R, :], scalar1=PI, scalar2=-TWO_PI, op0=ALU.is_gt,
                                                      op1=ALU.mult), [xs], [kf])
                        DVE(lambda e: e.tensor_tensor(out=xs.t[R, :], in0=xs.t[R, :], in1=kf.t[R, :], op=ALU.add), [xs, kf], [xs])
                        DVE(lambda e: e.tensor_scalar(out=kf.t[R, :], in0=xs.t[R, :], scalar1=-PI, scalar2=TWO_PI, op0=ALU.is_lt,
                                                      op1=ALU.mult), [xs], [kf])
                        DVE(lambda e: e.tensor_tensor(out=xs.t[R, :], in0=xs.t[R, :], in1=kf.t[R, :], op=ALU.add), [xs, kf], [xs])
                        ACT(lambda e: e.activation(out=xs.t[R, :], in_=xs.t[R, :], func=AF.Sin), [xs], [xs])
                        if which == 1:
                            DVE(lambda e, cs=cs, dst=dst: e.tensor_scalar(out=dst.t[R, cs], in0=xs.t[R, :], scalar1=cvec.t[R, sgn_col:sgn_col + 1],
                                                                         scalar2=None, op0=ALU.mult), [xs, cvec], [dst])
                        else:
                            DVE(lambda e, cs=cs, dst=dst: e.tensor_copy(out=dst.t[R, cs], in_=xs.t[R, :]), [xs], [dst])
                fw.flush()

        with ExitStack() as ph:
            ccol = alloc(ph, "ccol", [128, 8], F32)
            scl = alloc(ph, "scl", [128, 8], F32)
            ones = alloc(ph, "ones", [128, 128], F32)
            scb = alloc(ph, "scb", [128, 8, 128], F32)
            mod = alloc(ph, "mod", [128, 9 * D], F32)
            wm = [alloc(ph, "wm%d" % i, [128, 8, 512], F32) for i in range(2)]
            bm = [alloc(ph, "bm%d" % i, [128, 512], F32) for i in range(2)]
            gt = [alloc(ph, "gt%d" % i, [128, D], F32) for i in range(2)]
            tp_ = {"posi": alloc(ph, "posi", [128, 2048], I32), "xs": alloc(ph, "rt_xs", [128, 2048], F32),
                   "kf": alloc(ph, "rt_kf", [128, 2048], F32), "ki": alloc(ph, "rt_ki", [128, 2048], I32),
                   "ang": alloc(ph, "rt_ang", [128, 2048], F32),
                   "ot": [alloc(ph, "rt_ot%d" % i, [128, 2048], BF16) for i in range(2)]}
            tjobs = [(128, 0, 1, cos64S, sin64S, c) for c in range(2)] + [(96, 2, 3, cos32S, sin32S, c) for c in range(2)]
            LD(lambda e: e.dma_start(out=ccol.t[:], in_=c_col[:, :]), [], [ccol])
            ACT(lambda e: e.activation(out=scl.t[:], in_=ccol.t[:], func=AF.Silu), [ccol], [scl])
            DVE(lambda e: e.memset(ones.t[:], 1.0), [], [ones])
            for kc in range(8):
                DVE(lambda e, kc=kc: e.tensor_scalar(out=scb.t[:, kc, :], in0=ones.t[:], scalar1=scl.t[:, kc:kc + 1], scalar2=None,
                                                     op0=ALU.mult), [ones, scl], [scb])
            for g in range(18):
                w = wm[g % 2]
                b = bm[g % 2]
                LD(lambda e, w=w, g=g: e.dma_start(out=w.t[:], in_=wmod_t[g]), [], [w])
                LD(lambda e, b=b, g=g: e.dma_start(out=b.t[:], in_=bmod[0:1, g * 512:(g + 1) * 512].to_broadcast([128, 512])), [], [b])
                ps = PSF()
                for kc in range(8):
                    PE(lambda e, kc=kc, w=w, ps=ps: e.matmul(ps.t[:], scb.t[:, kc, :], w.t[:, kc, :], start=(kc == 0), stop=(kc == 7)),
                       [scb, w], [ps])
                DVE(lambda e, g=g, ps=ps, b=b: e.tensor_tensor(out=mod.t[:, g * 512:(g + 1) * 512], in0=ps.t[:], in1=b.t[:], op=ALU.add),
                    [ps, b], [mod])
                if g % 4 == 0 and tjobs:
                    rope_chunk(tp_, *tjobs.pop(0))
            for i in range(3):
                coef = 1.0 if i == 1 else 0.5
                ga, gb = gt[0], gt[1]
                LD(lambda e, i=i, ga=ga: e.dma_start(out=ga.t[:], in_=gv[2 * i:2 * i + 1, :].to_broadcast([128, D])), [], [ga])
                LD(lambda e, i=i, gb=gb: e.dma_start(out=gb.t[:], in_=gv[2 * i + 1:2 * i + 2, :].to_broadcast([128, D])), [], [gb])
                s0 = (3 * i + 1) * D
                g0 = (3 * i + 2) * D
                DVE(lambda e, s0=s0, ga=ga: e.scalar_tensor_tensor(out=mod.t[:, s0:s0 + D], in0=mod.t[:, s0:s0 + D], scalar=1.0, in1=ga.t[:],
                                                                   op0=ALU.add, op1=ALU.mult), [mod, ga], [mod])
                DVE(lambda e, g0=g0, gb=gb, coef=coef: e.scalar_tensor_tensor(out=mod.t[:, g0:g0 + D], in0=mod.t[:, g0:g0 + D], scalar=coef,
                                                                              in1=gb.t[:], op0=ALU.mult, op1=ALU.mult), [mod, gb], [mod])
            modS_b = Buf("modS")
            LD(lambda e: e.dma_start(out=modS[:, :], in_=mod.t[:]), [mod], [modS_b])
            while tjobs:
                rope_chunk(tp_, *tjobs.pop(0))
            fw.wait_all("sp", [modS_b, tabS_b])
            fw.flush()

        x1S_b = Buf("x1S")
        uTS_b = Buf("uTS")
        x2S_b = Buf("x2S")
        h2TS_b = Buf("h2TS")
        obTS_b = Buf("obTS")
        oaTS_b = Buf("oaTS")
        out_b = Buf("out")

        def load_mod(st, name, idx):
            t = alloc(st, name, [128, D], F32)
            LD(lambda e: e.dma_start(out=t.t[:], in_=modS[:, idx * D:(idx + 1) * D]), [modS_b], [t])
            return t

        def ffn_phase(first):
            ntok = T if first else 2048
            ngrp = ntok // 1024
            wgu = wgu1 if first else wgu2
            wdd = wd1 if first else wd2
            with ExitStack() as ph:
                wd = alloc(ph, "wd", [128, NF, D], BF16)
                for f0 in range(0, NF, 2):
                    LDC(lambda e, f0=f0: e.dma_start(out=wd.t[:, f0:f0 + 2, :], in_=wdd[:, f0:f0 + 2, :]), [], [wd])
                wring = [alloc(ph, "wring%d" % i, [128, 2, 8, 128], BF16) for i in range(4)]
                hTs = [alloc(ph, "hT%d" % i, [128, 8, 1024], BF16) for i in range(2)]
                aT = alloc(ph, "aT", [128, NF, 1024], BF16)
                xring = [alloc(ph, "xr%d" % i, [128, D], F32) for i in range(2)]
                tmpf = alloc(ph, "tmpf", [128, D], F32)
                junk = alloc(ph, "junk", [128, D], BF16)
                hbr = [alloc(ph, "hb%d" % i, [128, D], BF16) for i in range(2)]
                sg = [alloc(ph, "sg%d" % i, [128, 512], F32) for i in range(2)]
                ssr = [alloc(ph, "ss%d" % i, [128, 2], F32) for i in range(4)]
                rsr = [alloc(ph, "rs%d" % i, [128, 1], F32) for i in range(4)]
                if first:
                    sh1 = load_mod(ph, "sh1", 0)
                    gm1 = load_mod(ph, "gm1", 1)
                    cp = load_mod(ph, "cp1", 2)
                    sh2 = load_mod(ph, "sh2", 3)
                    gm2 = load_mod(ph, "gm2", 4)
                    x1r = [alloc(ph, "x1t%d" % i, [128, D], F32) for i in range(2)]
                    uTt = [alloc(ph, "uTt%d" % i, [128, 8, 128], BF16) for i in range(2)]
                else:
                    cp = load_mod(ph, "cp3", 8)
                    x1r = [alloc(ph, "x1t%d" % i, [128, D], F32) for i in range(2)]
                ssi = {"i": 0}

                def sumsq(src_aps, src_tns):
                    ss = ssr[ssi["i"] % 4]
                    ssi["i"] += 1
                    DVE(lambda e: e.memset(ss.t[:], 0.0), [], [ss])
                    for i, ap in enumerate(src_aps):
                        w = ap.shape[-1]
                        ACT(lambda e, i=i, ap=ap, w=w: e.activation(out=junk.t[:, 0:w], in_=ap, func=AF.Square, accum_out=ss.t[:, i:i + 1]),
                            src_tns, [junk, ss])
                    if len(src_aps) == 2:
                        DVE(lambda e: e.tensor_tensor(out=ss.t[:, 0:1], in0=ss.t[:, 0:1], in1=ss.t[:, 1:2], op=ALU.add), [ss], [ss])
                    return ss

                xi = {"i": 0}
                hbi = {"i": 0}

                def next_x():
                    xt = xring[xi["i"] % 2]
                    xi["i"] += 1
                    return xt

                def next_hb():
                    h_ = hbr[hbi["i"] % 2]
                    hbi["i"] += 1
                    return h_

                def chain_h(G, t):
                    tile = G * 8 + t
                    xt = next_x()
                    LD(lambda e, xt=xt, tile=tile: e.dma_start(out=xt.t[:], in_=x_l[tile * 128:(tile + 1) * 128, :]), [], [xt])
                    ss = sumsq([xt.t[:]], [xt])
                    rs = rstd_of(rsr, ss.t[:, 0:1], ss, D)
                    DVE(lambda e, xt=xt, rs=rs: e.scalar_tensor_tensor(out=tmpf.t[:], in0=xt.t[:], scalar=rs.t[:, 0:1], in1=gm1.t[:],
                                                                       op0=ALU.mult, op1=ALU.mult), [xt, rs, gm1], [tmpf])
                    h_ = next_hb()
                    DVE(lambda e, h_=h_: e.tensor_tensor(out=h_.t[:], in0=tmpf.t[:], in1=sh1.t[:], op=ALU.add), [tmpf, sh1], [h_])
                    return h_

                def trans_h(G, t, h_):
                    hT_ = hTs[G % 2]
                    transpose_to(h_, 8, hT_.t[:, :, t * 128:(t + 1) * 128], hT_)

                def load_hT(G):
                    hT_ = hTs[G % 2]
                    LD(lambda e, G=G, hT_=hT_: e.dma_start(out=hT_.t[:], in_=h2TS[:, :, G * 1024:(G + 1) * 1024]), [h2TS_b], [hT_])

                if first:
                    prev = None
                    for t in range(8):
                        h_ = chain_h(0, t)
                        if prev is not None:
                            trans_h(0, prev[0], prev[1])
                        prev = (t, h_)
                    trans_h(0, prev[0], prev[1])
                else:
                    load_hT(0)
                SLOTS = {1: 0, 3: 1, 6: 2, 8: 3, 11: 4, 13: 5, 16: 6, 18: 7}
                for G in range(ngrp):
                    hT = hTs[G % 2]
                    prev = None
                    for f in range(NF):
                        wr = wring[f % 4]
                        LDC(lambda e, wr=wr, f=f: e.dma_start(out=wr.t[:], in_=wgu[f]), [], [wr])
                        for half in range(2):
                            hs = slice(half * 512, (half + 1) * 512)
                            gp = PSF()
                            up = PSF()
                            for kc in range(8):
                                PE(lambda e, kc=kc, gp=gp, wr=wr, hs=hs, hT=hT: e.matmul(gp.t[:], wr.t[:, 0, kc, :], hT.t[:, kc, hs], start=(kc == 0),
                                                                                          stop=(kc == 7)), [wr, hT], [gp])
                            for kc in range(8):
                                PE(lambda e, kc=kc, up=up, wr=wr, hs=hs, hT=hT: e.matmul(up.t[:], wr.t[:, 1, kc, :], hT.t[:, kc, hs], start=(kc == 0),
                                                                                          stop=(kc == 7)), [wr, hT], [up])
                            s_ = sg[half]
                            ACT(lambda e, s_=s_, gp=gp: e.activation(out=s_.t[:], in_=gp.t[:], func=AF.Silu), [gp], [s_])
                            DVE(lambda e, s_=s_, up=up, f=f, hs=hs: e.tensor_tensor(out=aT.t[:, f, hs], in0=s_.t[:], in1=up.t[:], op=ALU.mult),
                                [s_, up], [aT])
                        if G + 1 < ngrp:
                            if first and f in SLOTS:
                                t = SLOTS[f]
                                h_ = chain_h(G + 1, t)
                                if prev is not None:
                                    trans_h(G + 1, prev[0], prev[1])
                                prev = (t, h_)
                            if (not first) and f == 0:
                                load_hT(G + 1)
                    if prev is not None:
                        trans_h(G + 1, prev[0], prev[1])
                    pend = None
                    for t in range(8):
                        tile = G * 8 + t
                        xt = next_x()
                        if first:
                            LD(lambda e, xt=xt, tile=tile: e.dma_start(out=xt.t[:], in_=x_l[tile * 128:(tile + 1) * 128, :]), [], [xt])
                        else:
                            LD(lambda e, xt=xt, tile=tile: e.dma_start(out=xt.t[:], in_=x2S[tile * 128:(tile + 1) * 128, :]), [x2S_b], [xt])
                        yps = [PSF(), PSF()]
                        for half in range(2):
                            yp = yps[half]
                            for f in range(NF):
                                PE(lambda e, f=f, yp=yp, t=t, half=half: e.matmul(yp.t[:], aT.t[:, f, t * 128:(t + 1) * 128],
                                                                                   wd.t[:, f, half * 512:(half + 1) * 512], start=(f == 0),
                                                                                   stop=(f == NF - 1)), [aT, wd], [yp])
                        if pend is not None:
                            ptile, ph_ = pend
                            ut = uTt[ptile % 2]
                            transpose_to(ph_, 8, ut.t[:], ut)
                            LD(lambda e, ut=ut, ptile=ptile: e.dma_start(out=uTS[:, :, ptile * 128:(ptile + 1) * 128], in_=ut.t[:]), [ut], [uTS_b])
                            pend = None
                        ss = sumsq([yps[0].t[:], yps[1].t[:]], yps)
                        rs = rstd_of(rsr, ss.t[:, 0:1], ss, D)
                        for half in range(2):
                            hs = slice(half * 512, (half + 1) * 512)
                            DVE(lambda e, half=half, hs=hs, rs=rs, yp=yps[half]: e.scalar_tensor_tensor(
                                out=tmpf.t[:, hs], in0=yp.t[:], scalar=rs.t[:, 0:1], in1=cp.t[:, hs], op0=ALU.mult, op1=ALU.mult),
                                [yps[half], rs, cp], [tmpf])
                        x1t = x1r[t % 2]
                        DVE(lambda e, xt=xt, x1t=x1t: e.tensor_tensor(out=x1t.t[:], in0=tmpf.t[:], in1=xt.t[:], op=ALU.add), [tmpf, xt], [x1t])
                        if not first:
                            LD(lambda e, tile=tile, x1t=x1t: e.dma_start(out=out_l[tile * 128:(tile + 1) * 128, :], in_=x1t.t[:]), [x1t], [out_b])
                            continue
                        if tile >= 16:
                            LD(lambda e, tile=tile, x1t=x1t: e.dma_start(out=x1S[(tile - 16) * 128:(tile - 15) * 128, :], in_=x1t.t[:]), [x1t], [x1S_b])
                        ss = sumsq([x1t.t[:]], [x1t])
                        rs = rstd_of(rsr, ss.t[:, 0:1], ss, D)
                        DVE(lambda e, rs=rs, x1t=x1t: e.scalar_tensor_tensor(out=tmpf.t[:], in0=x1t.t[:], scalar=rs.t[:, 0:1], in1=gm2.t[:],
                                                                             op0=ALU.mult, op1=ALU.mult), [x1t, rs, gm2], [tmpf])
                        h_ = next_hb()
                        DVE(lambda e, h_=h_: e.tensor_tensor(out=h_.t[:], in0=tmpf.t[:], in1=sh2.t[:], op=ALU.add), [tmpf, sh2], [h_])
                        pend = (tile, h_)
                    if pend is not None:
                        ptile, ph_ = pend
                        ut = uTt[ptile % 2]
                        transpose_to(ph_, 8, ut.t[:], ut)
                        LD(lambda e, ut=ut, ptile=ptile: e.dma_start(out=uTS[:, :, ptile * 128:(ptile + 1) * 128], in_=ut.t[:]), [ut], [uTS_b])
                        pend = None
                    if G % 2 == 1:
                        fw.flush()
                fw.wait_all("sp", [x1S_b, uTS_b, out_b])
                fw.flush()

        ffn_phase(True)

        with ExitStack() as ph:
            cos64 = alloc(ph, "cos64", [128, T], BF16)
            sin64 = alloc(ph, "sin64", [128, T], BF16)
            LD(lambda e: e.dma_start(out=cos64.t[:], in_=cos64S[:, :]), [tabS_b], [cos64])
            LD(lambda e: e.dma_start(out=sin64.t[:], in_=sin64S[:, :]), [tabS_b], [sin64])
            acc = alloc(ph, "acc", [128, 4, 2048], F32)
            tri = alloc(ph, "tri", [128, 256], BF16)
            LDC(lambda e: e.dma_start(out=tri.t[:], in_=tri_d[:, :]), [], [tri])
            gs = ExitStack()
            wdl = alloc(gs, "wdl", [128, 8, 1280], BF16)
            KT = alloc(gs, "KT", [128, 2, T], BF16)
            QT = alloc(gs, "QT", [128, 4, 2048], BF16)
            VA = alloc(gs, "VA", [128, NT, 4, 128], BF16)
            uTh = alloc(gs, "uTh", [128, 8, 2048], BF16)
            t1 = [alloc(gs, "t1_%d" % i, [128, 512], F32) for i in range(2)]
            t2 = [alloc(gs, "t2_%d" % i, [128, 512], F32) for i in range(2)]
            Eb = [alloc(gs, "Eb%d" % i, [128, 4, 128], BF16) for i in range(2)]
            PTb = [alloc(gs, "PTb%d" % i, [128, 4, 128], BF16) for i in range(2)]
            for g, d in enumerate((1, 4, 16)):
                LDC(lambda e, g=g: e.dma_start(out=wdl.t[:], in_=w_dil[g]), [], [wdl])
                DVE(lambda e: e.memset(VA.t[:], 1.0), [], [VA])
                DVE(lambda e: e.memset(QT.t[:], 0.0), [], [QT])
                ci = 0
                for hf in range(2):
                    LD(lambda e, hf=hf: e.dma_start(out=uTh.t[:], in_=uTS[:, :, hf * 2048:(hf + 1) * 2048]), [uTS_b], [uTh])
                    for c in range(4):
                        t0 = hf * 2048 + c * 512
                        cs = slice(c * 512, (c + 1) * 512)
                        jobs = [("k", 512, 768)]
                        if hf == 1:
                            jobs.append(("q", 0, 256))
                        for (nm, c0, c1) in jobs:
                            for pp in range(2):
                                pa = PSF()
                                pr = PSF()
                                for kc in range(8):
                                    PE(lambda e, kc=kc, pa=pa, pp=pp, c0=c0, cs=cs: e.matmul(pa.t[:, :], wdl.t[:, kc, c0 + pp * 128:c0 + (pp + 1) * 128],
                                                                                           uTh.t[:, kc, cs], start=(kc == 0), stop=(kc == 7)),
                                       [wdl, uTh], [pa])
                                for kc in range(8):
                                    PE(lambda e, kc=kc, pr=pr, pp=pp, c1=c1, cs=cs: e.matmul(pr.t[:, :], wdl.t[:, kc, c1 + pp * 128:c1 + (pp + 1) * 128],
                                                                                           uTh.t[:, kc, cs], start=(kc == 0), stop=(kc == 7)),
                                       [wdl, uTh], [pr])
                                a1 = t1[ci % 2]
                                a2 = t2[ci % 2]
                                ci += 1
                                DVE(lambda e, a1=a1, pa=pa, t0=t0: e.tensor_tensor(out=a1.t[:], in0=pa.t[:, :], in1=cos64.t[:, t0:t0 + 512],
                                                                                  op=ALU.mult), [pa, cos64], [a1])
                                DVE(lambda e, a2=a2, pr=pr, t0=t0: e.tensor_tensor(out=a2.t[:], in0=pr.t[:, :], in1=sin64.t[:, t0:t0 + 512],
                                                                                  op=ALU.mult), [pr, sin64], [a2])
                                if nm == "k":
                                    dview = KT.t[:, pp, :].rearrange("p (r m) -> p m r", r=d)[:, t0 // d:(t0 + 512) // d, :]
                                    POOL(lambda e, a1=a1, a2=a2, dview=dview: e.tensor_tensor(
                                        out=dview, in0=a1.t[:].rearrange("p (m r) -> p m r", r=d), in1=a2.t[:].rearrange("p (m r) -> p m r", r=d),
                                        op=ALU.add), [a1, a2], [KT])
                                else:
                                    n0 = c * 512
                                    for hh in range(2):
                                        R_ = slice(hh * 64, (hh + 1) * 64)
                                        dview = QT.t[R_, 2 * pp + hh, :].rearrange("p (r m) -> p m r", r=d)[:, n0 // d:(n0 + 512) // d, :]
                                        POOL(lambda e, a1=a1, a2=a2, dview=dview, R_=R_: e.tensor_tensor(
                                            out=dview, in0=a1.t[R_, :].rearrange("p (m r) -> p m r", r=d), in1=a2.t[R_, :].rearrange("p (m r) -> p m r", r=d),
                                            op=ALU.add), [a1, a2], [QT])
                    nb = 16 // d
                    for r in range(d):
                        for mbl in range(nb):
                            blk = r * (32 // d) + hf * nb + mbl
                            st0 = r + d * 128 * mbl
                            sl = slice(st0, st0 + d * 127 + 1, d)
                            vp = PSF()
                            for kc in range(8):
                                PE(lambda e, kc=kc, vp=vp, sl=sl: e.matmul(vp.t[:, 0:256], uTh.t[:, kc, sl], wdl.t[:, kc, 1024:1280],
                                                                          start=(kc == 0), stop=(kc == 7)), [uTh, wdl], [vp])
                            kvc = hf * 16
                            ACT(lambda e, vp=vp, blk=blk, kvc=kvc: e.activation(out=VA.t[:, blk, :, 0:64],
                                                                                 in_=vp.t[:, 0:256].rearrange("p (h e) -> p h e", h=4),
                                                                                 func=AF.Copy, scale=kvalid.t[:, kvc:kvc + 1]), [vp, kvalid], [VA])
                            if hf == 0:
                                DVE(lambda e, blk=blk: e.tensor_scalar(out=VA.t[:, blk, :, 64:128], in0=VA.t[:, blk, :, 64:128],
                                                                       scalar1=kvalid.t[:, 0:1], scalar2=None, op0=ALU.mult), [VA, kvalid], [VA])
                ei = 0
                for r in range(d):
                    for mbl in range(nb):
                        qb = r * nb + mbl
                        blk_same = r * (32 // d) + nb + mbl
                        op_ = PSF()
                        for bi, (blk, m0) in enumerate(((blk_same - 1, 0), (blk_same, 128))):
                            sp_ = PSF()
                            for h in range(4):
                                PE(lambda e, h=h, sp_=sp_, blk=blk, qb=qb: e.matmul(sp_.t[:, h * 128:(h + 1) * 128], KT.t[:, h // 2, blk * 128:(blk + 1) * 128],
                                                                                    QT.t[:, h, qb * 128:(qb + 1) * 128], start=True, stop=True),
                                   [KT, QT], [sp_])
                            E_ = Eb[ei % 2]
                            P_ = PTb[ei % 2]
                            ei += 1
                            ACT(lambda e, E_=E_, sp_=sp_: e.activation(out=E_.t[:], in_=sp_.t[:].rearrange("p (h q) -> p h q", h=4), func=AF.Exp,
                                                                        scale=0.125), [sp_], [E_])
                            POOL(lambda e, E_=E_, P_=P_, m0=m0: e.tensor_tensor(out=P_.t[:], in0=E_.t[:],
                                                                                 in1=tri.t[:, m0:m0 + 128].unsqueeze(1).to_broadcast([128, 4, 128]),
                                                                                 op=ALU.mult), [E_, tri], [P_])
                            for h in range(4):
                                PE(lambda e, h=h, op_=op_, P_=P_, blk=blk, bi=bi: e.matmul(op_.t[:, h * 128:(h + 1) * 128], VA.t[:, blk, h, :], P_.t[:, h, :],
                                                                                           start=(bi == 0 and h == 0), stop=(bi == 1)), [VA, P_], [op_])
                        st0 = r + d * 128 * mbl
                        aview = acc.t[:, :, st0:st0 + d * 127 + 1:d]
                        oview = op_.t[:].rearrange("p (h q) -> p h q", h=4)
                        if g == 0:
                            DVE(lambda e, aview=aview, oview=oview: e.tensor_copy(out=aview, in_=oview), [op_], [acc])
                        else:
                            DVE(lambda e, aview=aview, oview=oview: e.tensor_tensor(out=aview, in0=aview, in1=oview, op=ALU.add), [op_, acc], [acc])
                fw.flush()
            gs.close()
            rd = alloc(ph, "rd", [128, 4, 512], F32)
            rd0 = alloc(ph, "rd0", [64, 4, 512], F32)
            obt = alloc(ph, "obt", [64, 4, 512], BF16)
            for c in range(4):
                cs = slice(c * 512, (c + 1) * 512)
                DVE(lambda e, cs=cs: e.reciprocal(out=rd.t[64:128, :, :], in_=acc.t[64:128, :, cs]), [acc], [rd])
                DVE(lambda e: e.tensor_copy(out=rd0.t[0:64, :, :], in_=rd.t[64:128, :, :]), [rd], [rd0])
                DVE(lambda e, cs=cs: e.tensor_tensor(out=obt.t[:], in0=acc.t[0:64, :, cs], in1=rd0.t[:], op=ALU.mult), [acc, rd0], [obt])
                LD(lambda e, cs=cs: e.dma_start(out=obTS[:, :, cs], in_=obt.t[:]), [obt], [obTS_b])
            fw.wait_all("sp", [obTS_b])
            fw.flush()

        with ExitStack() as ph:
            cosq = alloc(ph, "cosq", [96, 2048], BF16)
            sinq = alloc(ph, "sinq", [96, 2048], BF16)
            kb0 = alloc(ph, "kb0", [128, 1], F32)
            DVE(lambda e: e.tensor_scalar(out=kb0.t[:], in0=kvalid.t[:, 0:1], scalar1=-1.0, scalar2=1.0e30, op0=ALU.add, op1=ALU.mult), [kvalid], [kb0])
            cm = alloc(ph, "cm", [128, 128], F32)
            LD(lambda e: e.dma_start(out=cm.t[:], in_=cm_d[:, :]), [], [cm])
            KH = alloc(ph, "KH", [96, 8, T], BF16)
            VA = alloc(ph, "VA2", [128, NT, 8, 65], BF16)
            KI = alloc(ph, "KI", [32, T], BF16)
            CQNT = alloc(ph, "CQNT", [128, 3, 2048], BF16)
            WSC = alloc(ph, "WSC", [128, 16, 8], F32)
            ssr = [alloc(ph, "bss%d" % i, [128, 2], F32) for i in range(4)]
            rsr = [alloc(ph, "brs%d" % i, [128, 1], F32) for i in range(4)]
            ks = ExitStack()
            cos32 = alloc(ks, "cos32", [96, T], BF16)
            sin32 = alloc(ks, "sin32", [96, T], BF16)
            LD(lambda e: e.dma_start(out=cos32.t[:], in_=cos32S[:, :]), [tabS_b], [cos32])
            LD(lambda e: e.dma_start(out=sin32.t[:], in_=sin32S[:, :]), [tabS_b], [sin32])
            LD(lambda e: e.dma_start(out=cosq.t[:], in_=cos32S[:, 2048:T]), [tabS_b], [cosq])
            LD(lambda e: e.dma_start(out=sinq.t[:], in_=sin32S[:, 2048:T]), [tabS_b], [sinq])
            wdi = alloc(ks, "wdi", [128, 8, 904], BF16)
            wkv = alloc(ks, "wkv", [128, 2, 1024], BF16)
            LDC(lambda e: e.dma_start(out=wdi.t[:], in_=w_dsa_in[:, :, :]), [], [wdi])
            LDC(lambda e: e.dma_start(out=wkv.t[:], in_=w_kv[:, :, :]), [], [wkv])
            gqb = alloc(ks, "gqb", [128, 384], F32)
            gkb = alloc(ks, "gkb", [128, 256], F32)
            LD(lambda e: e.dma_start(out=gqb.t[:], in_=gcq[0:1, :].to_broadcast([128, 384])), [], [gqb])
            LD(lambda e: e.dma_start(out=gkb.t[:], in_=gckv[0:1, :].to_broadcast([128, 256])), [], [gkb])
            if True:
                uTc = [alloc(ks, "uTc%d" % i, [128, 8, 512], BF16) for i in range(2)]
                ckvnT = [alloc(ks, "ckvnT%d" % i, [128, 2, 512], BF16) for i in range(2)]
                junk = alloc(ks, "kjunk", [128, 384], BF16)
                nb16 = alloc(ks, "nb16", [128, 384], BF16)
                r1 = alloc(ks, "r1", [96, 512], F32)
                r2 = alloc(ks, "r2", [96, 512], F32)
                r3 = alloc(ks, "r3", [96, 512], BF16)
                si = 0
                for c in range(8):
                    u = uTc[c % 2]
                    ck = ckvnT[c % 2]
                    cs = slice(c * 512, (c + 1) * 512)
                    LD(lambda e, u=u, cs=cs: e.dma_start(out=u.t[:], in_=uTS[:, :, cs]), [uTS_b], [u])
                    for t in range(4):
                        tile = c * 4 + t
                        ts_ = slice(t * 128, (t + 1) * 128)
                        jobs = [("kv", 0, 256, gkb)]
                        if tile >= 16:
                            jobs.append(("q", 520, 384, gqb))
                        for (nm, c0, wdt, gb_) in jobs:
                            cp_ = PSF()
                            for kc in range(8):
                                PE(lambda e, kc=kc, cp_=cp_, u=u, ts_=ts_, c0=c0, wdt=wdt: e.matmul(cp_.t[:, 0:wdt], u.t[:, kc, ts_], wdi.t[:, kc, c0:c0 + wdt],
                                                                                                 start=(kc == 0), stop=(kc == 7)), [u, wdi], [cp_])
                            ss = ssr[si % 4]
                            si += 1
                            DVE(lambda e, ss=ss: e.memset(ss.t[:], 0.0), [], [ss])
                            ACT(lambda e, ss=ss, cp_=cp_, wdt=wdt: e.activation(out=junk.t[:, 0:wdt], in_=cp_.t[:, 0:wdt], func=AF.Square,
                                                                                 accum_out=ss.t[:, 0:1]), [cp_], [junk, ss])
                            rs = rstd_of(rsr, ss.t[:, 0:1], ss, wdt)
                            DVE(lambda e, rs=rs, cp_=cp_, wdt=wdt, gb_=gb_: e.scalar_tensor_tensor(out=nb16.t[:, 0:wdt], in0=cp_.t[:, 0:wdt], scalar=rs.t[:, 0:1],
                                                                                                 in1=gb_.t[:, 0:wdt], op0=ALU.mult, op1=ALU.mult),
                                [cp_, rs, gb_], [nb16])
                            if nm == "kv":
                                transpose_to(nb16, 2, ck.t[:, :, ts_], ck)
                            else:
                                q0 = (tile - 16) * 128
                                transpose_to(nb16, 3, CQNT.t[:, :, q0:q0 + 128], CQNT)
                        if tile >= 16:
                            wp = PSF()
                            for kc in range(8):
                                PE(lambda e, kc=kc, wp=wp, u=u, ts_=ts_: e.matmul(wp.t[:, 0:8], u.t[:, kc, ts_], wdi.t[:, kc, 512:520], start=(kc == 0),
                                                                                   stop=(kc == 7)), [u, wdi], [wp])
                            ACT(lambda e, wp=wp, tile=tile: e.activation(out=WSC.t[:, tile - 16, :], in_=wp.t[:, 0:8], func=AF.Copy,
                                                                         scale=float(8 ** -0.5 * 32 ** -0.5)), [wp], [WSC])
                        vp = PSF()
                        for rc in range(2):
                            PE(lambda e, rc=rc, vp=vp, ck=ck, ts_=ts_: e.matmul(vp.t[:], ck.t[:, rc, ts_], wkv.t[:, rc, 512:1024], start=(rc == 0),
                                                                                 stop=(rc == 1)), [ck, wkv], [vp])
                        ACT(lambda e, vp=vp, tile=tile: e.activation(out=VA.t[:, tile, :, 0:64], in_=vp.t[:].rearrange("p (h e) -> p h e", h=8),
                                                                     func=AF.Copy, scale=kvalid.t[:, tile:tile + 1]), [vp, kvalid], [VA])
                        DVE(lambda e, tile=tile: e.tensor_scalar(out=VA.t[:, tile, :, 64:65], in0=ones8.t[:, :].unsqueeze(2), scalar1=kvalid.t[:, tile:tile + 1],
                                                                 scalar2=None, op0=ALU.mult), [ones8, kvalid], [VA])
                    for h in range(8):
                        kp = PSF()
                        for rc in range(2):
                            PE(lambda e, rc=rc, kp=kp, ck=ck, h=h: e.matmul(kp.t[0:64, :], wkv.t[:, rc, h * 64:(h + 1) * 64], ck.t[:, rc, :], start=(rc == 0),
                                                                             stop=(rc == 1)), [ck, wkv], [kp])
                        if h % 2 == 0:
                            ACT(lambda e, kp=kp, h=h, cs=cs: e.activation(out=KH.t[0:64, h, cs], in_=kp.t[0:64, :], func=AF.Copy), [kp], [KH])
                        else:
                            DVE(lambda e, kp=kp, h=h, cs=cs: e.tensor_copy(out=KH.t[0:64, h, cs], in_=kp.t[0:64, :]), [kp], [KH])
                    for (c0, c1, R, dst_is_kh) in ((256, 352, slice(64, 96), True), (448, 480, slice(0, 32), False)):
                        nr = 96 if dst_is_kh else 32
                        pa = PSF()
                        pr = PSF()
                        for kc in range(8):
                            PE(lambda e, kc=kc, pa=pa, u=u, c0=c0, nr=nr: e.matmul(pa.t[0:nr, :], wdi.t[:, kc, c0:c0 + nr], u.t[:, kc, :], start=(kc == 0),
                                                                                   stop=(kc == 7)), [u, wdi], [pa])
                        for kc in range(8):
                            PE(lambda e, kc=kc, pr=pr, u=u, c1=c1, nr=nr: e.matmul(pr.t[0:nr, :], wdi.t[:, kc, c1:c1 + nr], u.t[:, kc, :], start=(kc == 0),
                                                                                   stop=(kc == 7)), [u, wdi], [pr])
                        DVE(lambda e, pa=pa, R=R, cs=cs: e.tensor_tensor(out=r1.t[R, :], in0=pa.t[R, :], in1=cos32.t[R, cs], op=ALU.mult), [pa, cos32], [r1])
                        DVE(lambda e, pr=pr, R=R, cs=cs: e.tensor_tensor(out=r2.t[R, :], in0=pr.t[R, :], in1=sin32.t[R, cs], op=ALU.mult), [pr, sin32], [r2])
                        if dst_is_kh:
                            DVE(lambda e, R=R: e.tensor_tensor(out=r3.t[R, :], in0=r1.t[R, :], in1=r2.t[R, :], op=ALU.add), [r1, r2], [r3])
                            DVE(lambda e, R=R, cs=cs: e.tensor_copy(out=KH.t[R, :, cs], in_=r3.t[R, :].unsqueeze(1).to_broadcast([32, 8, 512])), [r3], [KH])
                        else:
                            DVE(lambda e, R=R, cs=cs: e.tensor_tensor(out=KI.t[R, cs], in0=r1.t[R, :], in1=r2.t[R, :], op=ALU.add), [r1, r2], [KI])
                fw.flush()
                ks.close()
            with ExitStack() as ms:
                ctr["n"] = 5
                wq = alloc(ms, "wq", [128, 3, 2048], BF16)
                LDC(lambda e: e.dma_start(out=wq.t[:], in_=w_q[:, :, :]), [], [wq])
                QH = [alloc(ms, "QH%d" % i, [96, 8, 128], BF16) for i in range(2)]
                QI = [alloc(ms, "QI%d" % i, [32, 8, 128], BF16) for i in range(2)]
                q1 = alloc(ms, "q1", [96, 4, 128], F32)
                q2 = alloc(ms, "q2", [96, 4, 128], F32)
                isc = alloc(ms, "isc", [128, T], F32)
                Rb = [alloc(ms, "Rb%d" % i, [128, 512], BF16) for i in range(3)]
                mk = alloc(ms, "mk", [128, T], BF16)
                MTs = [alloc(ms, "MT%d" % i, [128, NT, 128], BF16) for i in range(2)]
                mid = alloc(ms, "mid", [128, 1], F32)
                cnt = alloc(ms, "cnt", [128, BIS_ITERS], F32)
                stp = alloc(ms, "stp", [128, 1], F32)
                PTb = [alloc(ms, "P2_%d" % i, [128, 4, 128], BF16) for i in range(3)]
                rden = alloc(ms, "rden", [128, 8], F32)
                oab = alloc(ms, "oab", [128, 512], BF16)
                oat = [alloc(ms, "oat%d" % i, [128, 4, 128], BF16) for i in range(2)]
                attn_scale = float(96 ** -0.5)
                ri = 0
                ei = 0
                pi_ = 0
                def stageA(qt):
                    nonlocal ri
                    NKB = 17 + qt
                    N = NKB * 128
                    q0 = qt * 128
                    qtok = (16 + qt) * 128
                    qh = QH[qt % 2]
                    qi = QI[qt % 2]
                    MT = MTs[qt % 2]
                    for hg in range(2):
                        pa = PSF()
                        pr = PSF()
                        for hl in range(4):
                            h = hg * 4 + hl
                            for rc in range(3):
                                PE(lambda e, rc=rc, pa=pa, hl=hl, h=h, q0=q0: e.matmul(pa.t[0:96, hl * 128:(hl + 1) * 128], wq.t[:, rc, h * 96:(h + 1) * 96],
                                                                                    CQNT.t[:, rc, q0:q0 + 128], start=(rc == 0), stop=(rc == 2)), [wq, CQNT], [pa])
                            for rc in range(3):
                                PE(lambda e, rc=rc, pr=pr, hl=hl, h=h, q0=q0: e.matmul(pr.t[0:96, hl * 128:(hl + 1) * 128], wq.t[:, rc, 768 + h * 96:768 + (h + 1) * 96],
                                                                                    CQNT.t[:, rc, q0:q0 + 128], start=(rc == 0), stop=(rc == 2)), [wq, CQNT], [pr])
                        hsl = slice(hg * 4, hg * 4 + 4)
                        ACT(lambda e, pa=pa, qh=qh, hsl=hsl: e.activation(out=qh.t[0:64, hsl, :], in_=pa.t[0:64, :].rearrange("p (h q) -> p h q", h=4),
                                                                          func=AF.Copy), [pa], [qh])
                        R = slice(64, 96)
                        cosb = cosq.t[R, q0:q0 + 128].unsqueeze(1).to_broadcast([32, 4, 128])
                        sinb = sinq.t[R, q0:q0 + 128].unsqueeze(1).to_broadcast([32, 4, 128])
                        DVE(lambda e, pa=pa, cosb=cosb, R=R: e.tensor_tensor(out=q1.t[R, :, :], in0=pa.t[R, :].rearrange("p (h q) -> p h q", h=4), in1=cosb,
                                                                            op=ALU.mult), [pa, cosq], [q1])
                        DVE(lambda e, pr=pr, sinb=sinb, R=R: e.tensor_tensor(out=q2.t[R, :, :], in0=pr.t[R, :].rearrange("p (h q) -> p h q", h=4), in1=sinb,
                                                                            op=ALU.mult), [pr, sinq], [q2])
                        DVE(lambda e, qh=qh, hsl=hsl, R=R: e.tensor_tensor(out=qh.t[R, hsl, :], in0=q1.t[R, :, :], in1=q2.t[R, :, :], op=ALU.add), [q1, q2], [qh])
                        pa = PSF()
                        pr = PSF()
                        for hl in range(4):
                            h = hg * 4 + hl
                            for rc in range(3):
                                PE(lambda e, rc=rc, pa=pa, hl=hl, h=h, q0=q0: e.matmul(pa.t[0:32, hl * 128:(hl + 1) * 128], wq.t[:, rc, 1536 + h * 32:1536 + (h + 1) * 32],
                                                                                    CQNT.t[:, rc, q0:q0 + 128], start=(rc == 0), stop=(rc == 2)), [wq, CQNT], [pa])
                            for rc in range(3):
                                PE(lambda e, rc=rc, pr=pr, hl=hl, h=h, q0=q0: e.matmul(pr.t[0:32, hl * 128:(hl + 1) * 128], wq.t[:, rc, 1792 + h * 32:1792 + (h + 1) * 32],
                                                                                    CQNT.t[:, rc, q0:q0 + 128], start=(rc == 0), stop=(rc == 2)), [wq, CQNT], [pr])
                        R = slice(0, 32)
                        cosb = cosq.t[R, q0:q0 + 128].unsqueeze(1).to_broadcast([32, 4, 128])
                        sinb = sinq.t[R, q0:q0 + 128].unsqueeze(1).to_broadcast([32, 4, 128])
                        DVE(lambda e, pa=pa, cosb=cosb, R=R: e.tensor_tensor(out=q1.t[R, :, :], in0=pa.t[R, :].rearrange("p (h q) -> p h q", h=4), in1=cosb,
                                                                            op=ALU.mult), [pa, cosq], [q1])
                        DVE(lambda e, pr=pr, sinb=sinb, R=R: e.tensor_tensor(out=q2.t[R, :, :], in0=pr.t[R, :].rearrange("p (h q) -> p h q", h=4), in1=sinb,
                                                                            op=ALU.mult), [pr, sinq], [q2])
                        DVE(lambda e, qi=qi, hsl=hsl, R=R: e.tensor_tensor(out=qi.t[R, hsl, :], in0=q1.t[R, :, :], in1=q2.t[R, :, :], op=ALU.add), [q1, q2], [qi])
                    nch = (NKB + 3) // 4
                    for c in range(nch):
                        w_ = min(512, N - c * 512)
                        cs = slice(c * 512, c * 512 + w_)
                        for h in range(8):
                            lp = PSF()
                            PE(lambda e, lp=lp, qi=qi, h=h, cs=cs, w_=w_: e.matmul(lp.t[:, 0:w_], qi.t[0:32, h, :], KI.t[0:32, cs], start=True, stop=True),
                               [qi, KI], [lp])
                            rb = Rb[ri % 3]
                            ri += 1
                            ACT(lambda e, lp=lp, rb=rb, w_=w_: e.activation(out=rb.t[:, 0:w_], in_=lp.t[:, 0:w_], func=AF.Relu), [lp], [rb])
                            if h == 0:
                                sc2 = kb0.t[:, 0:1] if c < 4 else 0.0
                                DVE(lambda e, rb=rb, cs=cs, w_=w_, qt=qt, sc2=sc2: e.tensor_scalar(
                                    out=isc.t[:, cs], in0=rb.t[:, 0:w_], scalar1=WSC.t[:, qt, 0:1], scalar2=sc2, op0=ALU.mult, op1=ALU.add),
                                    [rb, WSC, kb0], [isc])
                            else:
                                DVE(lambda e, rb=rb, h=h, cs=cs, w_=w_, qt=qt: e.scalar_tensor_tensor(
                                    out=isc.t[:, cs], in0=rb.t[:, 0:w_], scalar=WSC.t[:, qt, h:h + 1], in1=isc.t[:, cs], op0=ALU.mult, op1=ALU.add),
                                    [rb, WSC, isc], [isc])
                    dsl = slice(N - 128, N)
                    DVE(lambda e, dsl=dsl: e.tensor_tensor(out=isc.t[:, dsl], in0=isc.t[:, dsl], in1=cm.t[:], op=ALU.add), [isc, cm], [isc])
                    DVE(lambda e: e.memset(mid.t[:], 0.0), [], [mid])
                    DVE(lambda e: e.memset(cnt.t[:], 0.0), [], [cnt])
                    wdt = BIS_W0
                    for it in range(BIS_ITERS):
                        wdt *= 0.5
                        DVE(lambda e, N=N, it=it: e.tensor_scalar(out=mk.t[:, 0:N], in0=isc.t[:, 0:N], scalar1=mid.t[:, 0:1], scalar2=0.0, op0=ALU.is_ge,
                                                                  op1=ALU.add, accum_out=cnt.t[:, it:it + 1]), [isc, mid], [mk, cnt])
                        DVE(lambda e, wdt=wdt, it=it: e.tensor_scalar(out=stp.t[:], in0=cnt.t[:, it:it + 1], scalar1=255.5, scalar2=2.0 * wdt, op0=ALU.is_ge,
                                                                      op1=ALU.mult), [cnt], [stp])
                        DVE(lambda e, wdt=wdt: e.scalar_tensor_tensor(out=mid.t[:], in0=stp.t[:], scalar=-wdt, in1=mid.t[:], op0=ALU.add,
                                                                      op1=ALU.add), [stp, mid], [mid])
                def stageA2(qt):
                    NKB = 17 + qt
                    N = NKB * 128
                    MT = MTs[qt % 2]
                    DVE(lambda e, N=N: e.tensor_scalar(out=mk.t[:, 0:N], in0=isc.t[:, 0:N], scalar1=mid.t[:, 0:1], scalar2=MASK_NEG, op0=ALU.is_lt,
                                                       op1=ALU.mult), [isc, mid], [mk])
                    for k0 in range(0, NKB, 8):
                        nk = min(8, NKB - k0)
                        pb = PSB()
                        for k in range(nk):
                            PE(lambda e, k=k, k0=k0, pb=pb: e.transpose(pb.t[:, k * 128:(k + 1) * 128], mk.t[:, (k0 + k) * 128:(k0 + k + 1) * 128], ident.t[:]),
                               [mk, ident], [pb])
                        ACT(lambda e, pb=pb, k0=k0, nk=nk: e.activation(out=MT.t[:, k0:k0 + nk, :], in_=pb.t[:, 0:nk * 128].rearrange("p (k q) -> p k q", k=nk),
                                                                        func=AF.Copy), [pb], [MT])
                def stageB(qt):
                    nonlocal pi_
                    NKB = 17 + qt
                    q0 = qt * 128
                    qh = QH[qt % 2]
                    MT = MTs[qt % 2]
                    accp = [psf[5], psf[6]]
                    steps = [(kb, hg) for kb in range(NKB) for hg in range(2)]

                    def emit_scores(kb, hg):
                        ks_ = slice(kb * 128, (kb + 1) * 128)
                        sp_ = PSF()
                        for hl in range(4):
                            h = hg * 4 + hl
                            PE(lambda e, sp_=sp_, hl=hl, h=h, ks_=ks_: e.matmul(sp_.t[:, hl * 128:(hl + 1) * 128], KH.t[0:96, h, ks_], qh.t[0:96, h, :],
                                                                                 start=(hl == 0), stop=False), [KH, qh], [sp_])
                        for hl in range(4):
                            PE(lambda e, sp_=sp_, hl=hl, kb=kb: e.matmul(sp_.t[:, hl * 128:(hl + 1) * 128], ident.t[:], MT.t[:, kb, :], start=False, stop=True),
                               [ident, MT], [sp_])
                        return sp_

                    sp_next = emit_scores(*steps[0])
                    for si_, (kb, hg) in enumerate(steps):
                        sp_ = sp_next
                        if si_ + 1 < len(steps):
                            sp_next = emit_scores(*steps[si_ + 1])
                        P_ = PTb[pi_ % 3]
                        pi_ += 1
                        ACT(lambda e, P_=P_, sp_=sp_: e.activation(out=P_.t[:], in_=sp_.t[:].rearrange("p (h q) -> p h q", h=4), func=AF.Exp,
                                                                    scale=attn_scale), [sp_], [P_])
                        ap_ = accp[hg]
                        for hl in range(4):
                            h = hg * 4 + hl
                            PE(lambda e, ap_=ap_, hl=hl, h=h, kb=kb, P_=P_, NKB=NKB: e.matmul(ap_.t[:, hl * 65:(hl + 1) * 65], P_.t[:, hl, :], VA.t[:, kb, h, :],
                                                                                           start=(kb == 0 and hl == 0), stop=(kb == NKB - 1)), [P_, VA], [ap_])
                    for hg in range(2):
                        ap_ = accp[hg]
                        av = ap_.t[:, 0:260].rearrange("p (h e) -> p h e", h=4)
                        DVE(lambda e, av=av, hg=hg: e.reciprocal(out=rden.t[:, hg * 4:(hg + 1) * 4], in_=av[:, :, 64]), [ap_], [rden])
                        for hl in range(4):
                            h = hg * 4 + hl
                            ACT(lambda e, av=av, hl=hl, h=h: e.activation(out=oab.t[:, h * 64:(h + 1) * 64], in_=av[:, hl, 0:64], func=AF.Copy,
                                                                          scale=rden.t[:, h:h + 1]), [ap_, rden], [oab])
                    ot = oat[qt % 2]
                    transpose_to(oab, 4, ot.t[:], ot, eng_copy="dve")
                    LD(lambda e, ot=ot, q0=q0: e.dma_start(out=oaTS[:, :, q0:q0 + 128], in_=ot.t[:]), [ot], [oaTS_b])
                stageA(0)
                stageA2(0)
                for qt in range(16):
                    if qt + 1 < 16:
                        stageA(qt + 1)
                    stageB(qt)
                    if qt + 1 < 16:
                        stageA2(qt + 1)
                    if qt % 2 == 1:
                        fw.flush()
                fw.wait_all("sp", [oaTS_b])
                fw.flush()
                ctr["n"] = 7

        with ExitStack() as ph:
            wg_ = alloc(ph, "wgates", [128, 8, 2048], BF16)
            wua = alloc(ph, "wua", [128, 4, D], BF16)
            wub = alloc(ph, "wub", [64, 4, D], BF16)
            wo_ = alloc(ph, "wo", [128, 8, D], BF16)
            for kc in range(8):
                LDC(lambda e, kc=kc: e.dma_start(out=wg_.t[:, kc, :], in_=w_gates[:, kc, :]), [], [wg_])
            LDC(lambda e: e.dma_start(out=wua.t[:], in_=w_upa[:, :, :]), [], [wua])
            LDC(lambda e: e.dma_start(out=wub.t[:], in_=w_upb[:, :, :]), [], [wub])
            for kc in range(0, 8, 2):
                LDC(lambda e, kc=kc: e.dma_start(out=wo_.t[:, kc:kc + 2, :], in_=w_o[:, kc:kc + 2, :]), [], [wo_])
            cp2 = load_mod(ph, "cp2", 5)
            sh3 = load_mod(ph, "sh3", 6)
            gm3 = load_mod(ph, "gm3", 7)
            uT = [alloc(ph, "muT%d" % i, [128, 8, 128], BF16) for i in range(2)]
            oaT = [alloc(ph, "moa%d" % i, [128, 4, 128], BF16) for i in range(2)]
            obT = [alloc(ph, "mob%d" % i, [64, 4, 128], BF16) for i in range(2)]
            x1r = [alloc(ph, "mx1%d" % i, [128, D], F32) for i in range(2)]
            sga = alloc(ph, "sga", [128, D], F32)
            sgb = alloc(ph, "sgb", [128, D], F32)
            zf = alloc(ph, "zf", [128, D], F32)
            zb = alloc(ph, "zb", [128, D], BF16)
            zT = alloc(ph, "zT", [128, 8, 128], BF16)
            tmpf = alloc(ph, "mtmp", [128, D], F32)
            x2t = alloc(ph, "x2t", [128, D], F32)
            hb = alloc(ph, "mhb", [128, D], BF16)
            h2t = [alloc(ph, "h2t%d" % i, [128, 8, 128], BF16) for i in range(2)]
            junk = alloc(ph, "mjunk", [128, D], BF16)
            ssr = [alloc(ph, "mss%d" % i, [128, 2], F32) for i in range(4)]
            rsr = [alloc(ph, "mrs%d" % i, [128, 1], F32) for i in range(4)]
            si = 0
            for qt in range(16):
                u = uT[qt % 2]
                oa = oaT[qt % 2]
                ob = obT[qt % 2]
                x1 = x1r[qt % 2]
                tok = (16 + qt) * 128
                q0 = qt * 128
                LD(lambda e, u=u, tok=tok: e.dma_start(out=u.t[:], in_=uTS[:, :, tok:tok + 128]), [uTS_b], [u])
                LD(lambda e, oa=oa, q0=q0: e.dma_start(out=oa.t[:], in_=oaTS[:, :, q0:q0 + 128]), [oaTS_b], [oa])
                LD(lambda e, ob=ob, q0=q0: e.dma_start(out=ob.t[:], in_=obTS[:, :, q0:q0 + 128]), [obTS_b], [ob])
                LD(lambda e, x1=x1, q0=q0: e.dma_start(out=x1.t[:], in_=x1S[q0:q0 + 128, :]), [x1S_b], [x1])
                for half in range(2):
                    hs = slice(half * 512, (half + 1) * 512)
                    pga = PSF()
                    pgb = PSF()
                    for kc in range(8):
                        PE(lambda e, kc=kc, pga=pga, u=u, half=half: e.matmul(pga.t[:], u.t[:, kc, :], wg_.t[:, kc, half * 512:(half + 1) * 512], start=(kc == 0),
                                                                               stop=(kc == 7)), [u, wg_], [pga])
                    for kc in range(8):
                        PE(lambda e, kc=kc, pgb=pgb, u=u, half=half: e.matmul(pgb.t[:], u.t[:, kc, :], wg_.t[:, kc, 1024 + half * 512:1024 + (half + 1) * 512],
                                                                               start=(kc == 0), stop=(kc == 7)), [u, wg_], [pgb])
                    ACT(lambda e, pga=pga, hs=hs: e.activation(out=sga.t[:, hs], in_=pga.t[:], func=AF.Sigmoid), [pga], [sga])
                    ACT(lambda e, pgb=pgb, hs=hs: e.activation(out=sgb.t[:, hs], in_=pgb.t[:], func=AF.Sigmoid), [pgb], [sgb])
                    pza = PSF()
                    pzb = PSF()
                    for c4 in range(4):
                        PE(lambda e, c4=c4, pza=pza, oa=oa, hs=hs: e.matmul(pza.t[:], oa.t[:, c4, :], wua.t[:, c4, hs], start=(c4 == 0), stop=(c4 == 3)),
                           [oa, wua], [pza])
                    for c4 in range(4):
                        PE(lambda e, c4=c4, pzb=pzb, ob=ob, hs=hs: e.matmul(pzb.t[:], ob.t[0:64, c4, :], wub.t[0:64, c4, hs], start=(c4 == 0), stop=(c4 == 3)),
                           [ob, wub], [pzb])
                    DVE(lambda e, pza=pza, hs=hs: e.tensor_tensor(out=zf.t[:, hs], in0=sga.t[:, hs], in1=pza.t[:], op=ALU.mult), [sga, pza], [zf])
                    DVE(lambda e, pzb=pzb, hs=hs: e.tensor_tensor(out=tmpf.t[:, hs], in0=sgb.t[:, hs], in1=pzb.t[:], op=ALU.mult), [sgb, pzb], [tmpf])
                    DVE(lambda e, hs=hs: e.tensor_tensor(out=zb.t[:, hs], in0=zf.t[:, hs], in1=tmpf.t[:, hs], op=ALU.add), [zf, tmpf], [zb])
                transpose_to(zb, 8, zT.t[:], zT)
                yps = [PSF(), PSF()]
                for half in range(2):
                    yp = yps[half]
                    for kc in range(8):
                        PE(lambda e, kc=kc, yp=yp, half=half: e.matmul(yp.t[:], zT.t[:, kc, :], wo_.t[:, kc, half * 512:(half + 1) * 512], start=(kc == 0),
                                                                        stop=(kc == 7)), [zT, wo_], [yp])
                ss = ssr[si % 4]
                si += 1
                DVE(lambda e, ss=ss: e.memset(ss.t[:], 0.0), [], [ss])
                for half in range(2):
                    ACT(lambda e, half=half, ss=ss, yp=yps[half]: e.activation(out=junk.t[:, 0:512], in_=yp.t[:], func=AF.Square, accum_out=ss.t[:, half:half + 1]),
                        [yps[half]], [junk, ss])
                DVE(lambda e, ss=ss: e.tensor_tensor(out=ss.t[:, 0:1], in0=ss.t[:, 0:1], in1=ss.t[:, 1:2], op=ALU.add), [ss], [ss])
                rs = rstd_of(rsr, ss.t[:, 0:1], ss, D)
                for half in range(2):
                    hs = slice(half * 512, (half + 1) * 512)
                    DVE(lambda e, hs=hs, rs=rs, yp=yps[half]: e.scalar_tensor_tensor(out=tmpf.t[:, hs], in0=yp.t[:], scalar=rs.t[:, 0:1], in1=cp2.t[:, hs],
                                                                                     op0=ALU.mult, op1=ALU.mult), [yps[half], rs, cp2], [tmpf])
                DVE(lambda e, x1=x1: e.tensor_tensor(out=x2t.t[:], in0=tmpf.t[:], in1=x1.t[:], op=ALU.add), [tmpf, x1], [x2t])
                LD(lambda e, q0=q0: e.dma_start(out=x2S[q0:q0 + 128, :], in_=x2t.t[:]), [x2t], [x2S_b])
                ss = ssr[si % 4]
                si += 1
                DVE(lambda e, ss=ss: e.memset(ss.t[:], 0.0), [], [ss])
                ACT(lambda e, ss=ss: e.activation(out=junk.t[:], in_=x2t.t[:], func=AF.Square, accum_out=ss.t[:, 0:1]), [x2t], [junk, ss])
                rs = rstd_of(rsr, ss.t[:, 0:1], ss, D)
                DVE(lambda e, rs=rs: e.scalar_tensor_tensor(out=tmpf.t[:], in0=x2t.t[:], scalar=rs.t[:, 0:1], in1=gm3.t[:], op0=ALU.mult, op1=ALU.mult),
                    [x2t, rs, gm3], [tmpf])
                DVE(lambda e: e.tensor_tensor(out=hb.t[:], in0=tmpf.t[:], in1=sh3.t[:], op=ALU.add), [tmpf, sh3], [hb])
                ht = h2t[qt % 2]
                transpose_to(hb, 8, ht.t[:], ht)
                LD(lambda e, ht=ht, q0=q0: e.dma_start(out=h2TS[:, :, q0:q0 + 128], in_=ht.t[:]), [ht], [h2TS_b])
            fw.wait_all("sp", [x2S_b, h2TS_b])
            fw.flush()

        ffn_phase(False)
    return nc


def _tile_k(w):
    K, N = w.shape
    return np.ascontiguousarray(w.reshape(K // 128, 128, N).transpose(1, 0, 2))


def _swap(w, half):
    return np.concatenate([w[..., half:2 * half], w[..., :half]], axis=-1)


_NC_CACHE = {}


def _prep_shared(inp):
    f = np.float32
    A = {}
    w_mod = inp["w_mod"][0]
    A["wmod_t"] = np.ascontiguousarray(w_mod.reshape(8, 128, 18, 512).transpose(2, 1, 0, 3))
    A["bmod"] = np.ascontiguousarray(inp["b_mod"][0:1])
    A["gv"] = np.ascontiguousarray(np.stack([inp["g_pre_ffn1"][0], inp["g_post_ffn1"][0], inp["g_pre_mix"][0], inp["g_post_mix"][0],
                                             inp["g_pre_ffn2"][0], inp["g_post_ffn2"][0]]).astype(f))
    A["gcq"] = np.ascontiguousarray(inp["g_cq"][0:1])
    A["gckv"] = np.ascontiguousarray(inp["g_ckv"][0:1])
    for i, (g, u, dn) in enumerate((("w_gate1", "w_up1", "w_down1"), ("w_gate2", "w_up2", "w_down2"))):
        wg = inp[g][0].reshape(8, 128, NF, 128)
        wu = inp[u][0].reshape(8, 128, NF, 128)
        wgu = np.stack([wg, wu], 0)
        A["wgu%d" % (i + 1)] = np.ascontiguousarray(wgu.transpose(3, 2, 0, 1, 4))
        A["wd%d" % (i + 1)] = _tile_k(inp[dn][0])
    w_in = inp["w_in"][0]
    z64 = np.zeros((D, 64), f)
    kr = w_in[:, 640:672]
    ki = w_in[:, 672:704]
    cols = [w_in[:, 384:640], np.concatenate([z64, kr], 1), np.concatenate([z64, _swap(kr, 16)], 1), ki, _swap(ki, 16), w_in[:, 704:712],
            w_in[:, 0:384]]
    A["w_dsa_in"] = _tile_k(np.concatenate(cols, 1))
    w_uq = inp["w_uq"][0]
    uq_rot = np.concatenate([np.zeros((384, 8, 64), f), _swap(w_uq[:, :, 64:96], 16)], -1)
    w_iq = inp["w_iq"][0]
    A["w_q"] = _tile_k(np.concatenate([w_uq.reshape(384, 768), uq_rot.reshape(384, 768), w_iq.reshape(384, 256),
                                       _swap(w_iq, 16).reshape(384, 256)], 1))
    A["w_kv"] = _tile_k(np.concatenate([inp["w_uk"][0].reshape(256, 512), inp["w_uv"][0].reshape(256, 512)], 1))
    wd = []
    for g in range(3):
        parts = []
        for j in range(3):
            base = 712 + (j * 3 + g) * 256
            wj = w_in[:, base:base + 256].reshape(D, 4, 64)
            parts.append(wj.reshape(D, 256))
            if j < 2:
                parts.append(_swap(wj, 32).reshape(D, 256))
        wd.append(_tile_k(np.concatenate(parts, 1)))
    A["w_dil"] = np.ascontiguousarray(np.stack(wd, 0))
    A["w_gates"] = _tile_k(w_in[:, 3016:5064])
    A["w_upa"] = _tile_k(inp["w_up_a"][0])
    A["w_upb"] = np.ascontiguousarray(inp["w_up_b"][0].reshape(4, 64, D).transpose(1, 0, 2))
    A["w_o"] = _tile_k(inp["w_o"][0])
    A["ident"] = np.eye(128, dtype=f)
    p = np.arange(128)
    cvec = np.zeros((128, 8), f)
    cvec[:, 0] = THETA ** (-(p % 32).astype(np.float64) / 32.0)
    cvec[:, 1] = np.where((p % 64) < 32, -1.0, 1.0)
    cvec[:, 2] = THETA ** (-(p % 16).astype(np.float64) / 16.0)
    cvec[:, 3] = np.where((p % 32) < 16, -1.0, 1.0)
    A["cvec"] = cvec
    s = p[:, None]
    q = p[None, :]
    A["tri"] = np.concatenate([(s >= q), (s <= q)], 1).astype(f)
    A["cm"] = np.where(p[None, :] <= p[:, None], 0.0, NEG).astype(f)
    return {k: np.ascontiguousarray(v, dtype=f) for k, v in A.items()}


def _run(inputs, debug=False):
    inp = {k: np.asarray(v) for k, v in inputs.items()}
    shared = _prep_shared(inp)
    x = inp["x"].astype(np.float32)
    c = inp["c"].astype(np.float32)
    pos = inp["positions"].astype(np.int32)
    in_maps = []
    for core in range(8):
        b, j = core // 2, core % 2
        m = dict(shared)
        if j == 1:
            xl = x[b]
            pl = pos[b]
            kval = np.ones((128, NT), np.float32)
            kb = np.zeros((1, T), np.float32)
        else:
            xl = np.concatenate([x[b, 2048:], x[b, :2048]], 0)
            pl = np.concatenate([pos[b, 2048:], pos[b, :2048]], 0)
            kval = np.ones((128, NT), np.float32)
            kval[:, :16] = 0.0
            kb = np.zeros((1, T), np.float32)
            kb[:, :2048] = NEG
        m["x_l"] = np.ascontiguousarray(xl)
        m["pos_l"] = np.ascontiguousarray(pl.reshape(1, T))
        m["c_col"] = np.ascontiguousarray(c[b].reshape(8, 128).T)
        m["kvalid"] = kval
        m["kbias"] = kb
        in_maps.append(m)
    key = bool(debug)
    if key not in _NC_CACHE:
        _NC_CACHE[key] = build_program(debug=debug)
    nc = _NC_CACHE[key]
    res = run_bass_kernel_spmd(nc, in_maps, core_ids=list(range(8)))
    out = np.zeros((4, T, D), np.float32)
    for core in range(8):
        b, j = core // 2, core % 2
        o = np.asarray(res.results[core]["out_l"], dtype=np.float32)
        if j == 1:
            out[b, 2048:] = o
        else:
            out[b, :2048] = o
    return out, res


def kernel(**inputs):
    out, _ = _run(inputs, debug=False)
    return out
```

```python
import numpy as np
from contextlib import ExitStack
import concourse.bass as bass
import concourse.mybir as mybir
from concourse.bass_utils import run_bass_kernel_spmd

F32 = mybir.dt.float32
BF16 = mybir.dt.bfloat16
I32 = mybir.dt.int32
ALU = mybir.AluOpType
AF = mybir.ActivationFunctionType

D = 1024
T = 4096
NT = 32
DFF = 2816
NF = 22
EPS = 1e-6
THETA = 10000.0
SEM_EPOCH = 12000
PI = float(np.pi)
TWO_PI = 2.0 * PI
C_HI = float(np.float32(6.28125))
C_LO = float(TWO_PI - 6.28125)
NEG = -1.0e30
BIS_ITERS = 16
BIS_W0 = 8.0
MASK_NEG = -30000.0


class Buf:
    __slots__ = ("name", "last_w", "readers")

    def __init__(self, name):
        self.name = name
        self.last_w = None
        self.readers = {}


class Tn:
    __slots__ = ("t", "b")

    def __init__(self, t, name):
        self.t = t
        self.b = Buf(name)


class Eng:
    def __init__(self, name):
        self.name = name
        self.is_pe = name == "pe"
        self.count = 0
        self.epoch = 0
        self.known = {}
        self.thunks = []
        self.dma_slot = 0
        self.dma_uses = {}


class FW:
    def __init__(self, nc, stack, n_dma_sems=12):
        self.nc = nc
        self.stack = stack
        self.engs = {n: Eng(n) for n in ("pe", "act", "dve", "pool", "sp")}
        self.sems = {}
        self.n_dma_sems = n_dma_sems

    def _sem(self, key):
        if key not in self.sems:
            self.sems[key] = self.stack.enter_context(self.nc.semaphore("s_%s_%d" % key))
        return self.sems[key]

    def _collect(self, E, reads, writes, skip_self_pe=False):
        waits = []

        def need(ev):
            key, val = ev
            if skip_self_pe and key[0] == "pe":
                return
            if E.known.get(key, 0) >= val:
                return
            E.known[key] = val
            waits.append((key, val))

        for b in reads:
            if b.last_w is not None:
                need(b.last_w)
        for b in writes:
            if b.last_w is not None:
                need(b.last_w)
            for k, v in b.readers.items():
                need((k, v))
        return waits

    def _record(self, ev, reads, writes):
        key, val = ev
        for b in reads:
            if b.readers.get(key, 0) < val:
                b.readers[key] = val
        for b in writes:
            b.last_w = ev
            b.readers = {}

    def op(self, eng, fn, reads=(), writes=(), pe_acc=False):
        E = self.engs[eng]
        reads = [x.b if isinstance(x, Tn) else x for x in reads]
        writes = [x.b if isinstance(x, Tn) else x for x in writes]
        waits = self._collect(E, reads, writes, skip_self_pe=(E.is_pe and pe_acc))
        if E.count >= SEM_EPOCH:
            E.epoch += 1
            E.count = 0
        E.count += 1
        key = (E.name, E.epoch)
        ev = (key, E.count)
        E.thunks.append((waits, fn, (key, 1)))
        self._record(ev, reads, writes)

    def dma(self, eng, fn, reads=(), writes=()):
        E = self.engs[eng]
        reads = [x.b if isinstance(x, Tn) else x for x in reads]
        writes = [x.b if isinstance(x, Tn) else x for x in writes]
        waits = self._collect(E, reads, writes)
        slot = E.dma_slot
        E.dma_slot = (E.dma_slot + 1) % self.n_dma_sems
        key = ("dma_" + eng, slot)
        uses = E.dma_uses.get(slot, 0)
        if uses > 0 and E.known.get(key, 0) < 16 * uses:
            waits.append((key, 16 * uses))
            E.known[key] = 16 * uses
        uses += 1
        E.dma_uses[slot] = uses
        ev = (key, 16 * uses)
        E.thunks.append((waits, fn, (key, 16)))
        self._record(ev, reads, writes)

    def wait_all(self, eng, bufs):
        E = self.engs[eng]
        bufs = [x.b if isinstance(x, Tn) else x for x in bufs]
        waits = self._collect(E, bufs, ())
        E.thunks.append((waits, None, None))

    def flush(self):
        nc = self.nc
        for E in self.engs.values():
            for waits, fn, inc in E.thunks:
                for key, _ in waits:
                    self._sem(key)
                if inc is not None:
                    self._sem(inc[0])
        sems = self.sems
        with nc.Block() as block:
            def replay(E):
                def run(eo):
                    for waits, fn, inc in E.thunks:
                        for key, val in waits:
                            eo.wait_ge(sems[key], val)
                        if fn is not None:
                            fn(eo).then_inc(sems[inc[0]], inc[1])
                return run

            block.tensor(replay(self.engs["pe"]))
            block.scalar(replay(self.engs["act"]))
            block.vector(replay(self.engs["dve"]))
            block.gpsimd(replay(self.engs["pool"]))
            block.sync(replay(self.engs["sp"]))
        for E in self.engs.values():
            E.thunks = []


def build_program(debug=False):
    nc = bass.Bass("TRN2", target_bir_lowering=False)

    def din(name, shape, dt=F32):
        return nc.dram_tensor(name, list(shape), dt, kind="ExternalInput").ap()

    def dscr(name, shape, dt):
        return nc.dram_tensor(name, list(shape), dt, kind=("ExternalOutput" if debug else "Internal")).ap()

    x_l = din("x_l", [T, D])
    pos_l = din("pos_l", [1, T], I32)
    c_col = din("c_col", [128, 8])
    kvalid_d = din("kvalid", [128, NT])
    kbias_d = din("kbias", [1, T])
    wmod_t = din("wmod_t", [18, 128, 8, 512])
    bmod = din("bmod", [1, 9 * D])
    gv = din("gv", [6, D])
    gcq = din("gcq", [1, 384])
    gckv = din("gckv", [1, 256])
    wgu1 = din("wgu1", [NF, 128, 2, 8, 128])
    wd1 = din("wd1", [128, NF, D])
    wgu2 = din("wgu2", [NF, 128, 2, 8, 128])
    wd2 = din("wd2", [128, NF, D])
    w_dsa_in = din("w_dsa_in", [128, 8, 904])
    w_q = din("w_q", [128, 3, 2048])
    w_kv = din("w_kv", [128, 2, 1024])
    w_dil = din("w_dil", [3, 128, 8, 1280])
    w_gates = din("w_gates", [128, 8, 2048])
    w_upa = din("w_upa", [128, 4, D])
    w_upb = din("w_upb", [64, 4, D])
    w_o = din("w_o", [128, 8, D])
    ident_d = din("ident", [128, 128])
    cvec_d = din("cvec", [128, 8])
    tri_d = din("tri", [128, 256])
    cm_d = din("cm", [128, 128])
    out_l = nc.dram_tensor("out_l", [2048, D], F32, kind="ExternalOutput").ap()

    modS = dscr("modS", [128, 9 * D], F32)
    x1S = dscr("x1S", [2048, D], F32)
    uTS = dscr("uTS", [128, 8, T], BF16)
    x2S = dscr("x2S", [2048, D], F32)
    h2TS = dscr("h2TS", [128, 8, 2048], BF16)
    obTS = dscr("obTS", [64, 4, 2048], BF16)
    oaTS = dscr("oaTS", [128, 4, 2048], BF16)
    cos64S = dscr("cos64S", [128, T], BF16)
    sin64S = dscr("sin64S", [128, T], BF16)
    cos32S = dscr("cos32S", [96, T], BF16)
    sin32S = dscr("sin32S", [96, T], BF16)

    with ExitStack() as top:
        fw = FW(nc, top)

        uniq = {"n": 0}

        def alloc(st, name, shape, dt):
            uniq["n"] += 1
            nm = "sb%d_%s" % (uniq["n"], name)
            return Tn(st.enter_context(nc.sbuf_tensor(nm, list(shape), dt)), nm)

        psf = [Tn(top.enter_context(nc.psum_tensor("psf%d" % i, [128, 512], F32)), "psf%d" % i) for i in range(7)]
        psb = [Tn(top.enter_context(nc.psum_tensor("psb%d" % i, [128, 1024], BF16)), "psb%d" % i) for i in range(1)]
        ctr = {"f": 0, "n": 7}

        def PSF():
            p = psf[ctr["f"] % ctr["n"]]
            ctr["f"] += 1
            return p

        def PSB():
            return psb[0]

        def PE(fn, r, w):
            fw.op("pe", fn, r, w, pe_acc=True)

        def ACT(fn, r, w):
            fw.op("act", fn, r, w)

        def DVE(fn, r, w):
            fw.op("dve", fn, r, w)

        def POOL(fn, r, w):
            fw.op("pool", fn, r, w)

        def LD(fn, r, w):
            fw.dma("sp", fn, r, w)

        def LDC(fn, r, w):
            fw.dma("pool", fn, r, w)

        ident = alloc(top, "ident", [128, 128], BF16)
        cvec = alloc(top, "cvec", [128, 8], F32)
        kvalid = alloc(top, "kvalid_sb", [128, NT], F32)
        ones8 = alloc(top, "ones8", [128, 8], F32)
        epsc = alloc(top, "epsc", [128, 1], F32)
        LDC(lambda e: e.dma_start(out=ident.t[:], in_=ident_d[:, :]), [], [ident])
        LD(lambda e: e.dma_start(out=cvec.t[:], in_=cvec_d[:, :]), [], [cvec])
        LD(lambda e: e.dma_start(out=kvalid.t[:], in_=kvalid_d[:, :]), [], [kvalid])
        DVE(lambda e: e.memset(ones8.t[:], 1.0), [], [ones8])
        DVE(lambda e: e.memset(epsc.t[:], EPS), [], [epsc])

        small = {"i": 0}

        def rstd_of(st_pool, ss_ap, ss_tn, n):
            r = st_pool[small["i"] % len(st_pool)]
            small["i"] += 1
            ACT(lambda e: e.activation(out=r.t[:], in_=ss_ap, func=AF.Sqrt, scale=1.0 / n, bias=epsc.t[:]), [ss_tn, epsc], [r])
            DVE(lambda e: e.reciprocal(out=r.t[:], in_=r.t[:]), [r], [r])
            return r

        def transpose_to(src, ncols_chunks, dst_ap, dst_tn, eng_copy="act"):
            pb = PSB()
            for k in range(ncols_chunks):
                PE(lambda e, k=k: e.transpose(pb.t[:, k * 128:(k + 1) * 128], src.t[:, k * 128:(k + 1) * 128], ident.t[:]),
                   [src, ident], [pb])
            view = pb.t[:, 0:ncols_chunks * 128].rearrange("p (k t) -> p k t", k=ncols_chunks)
            if eng_copy == "act":
                ACT(lambda e: e.activation(out=dst_ap, in_=view, func=AF.Copy), [pb], [dst_tn])
            else:
                DVE(lambda e: e.tensor_copy(out=dst_ap, in_=view), [pb], [dst_tn])

        tabS_b = Buf("tabS")

        def rope_chunk(tp_, nrows, inv_col, sgn_col, cosS, sinS, c):
            posi, xs, kf, ki, ang = tp_["posi"], tp_["xs"], tp_["kf"], tp_["ki"], tp_["ang"]
            R = slice(0, nrows)
            cs = slice(c * 2048, (c + 1) * 2048)
            LD(lambda e: e.dma_start(out=posi.t[R, :], in_=pos_l[0:1, cs].to_broadcast([nrows, 2048])), [], [posi])
            DVE(lambda e: e.tensor_copy(out=ang.t[R, :], in_=posi.t[R, :]), [posi], [ang])
            DVE(lambda e: e.tensor_scalar(out=ang.t[R, :], in0=ang.t[R, :], scalar1=cvec.t[R, inv_col:inv_col + 1], scalar2=None,
                                          op0=ALU.mult), [ang, cvec], [ang])
            for which in range(2):
                off = 0.0 if which == 1 else PI / 2.0
                dstS = sinS if which == 1 else cosS
                ot = tp_["ot"][which]
                DVE(lambda e, off=off: e.tensor_scalar(out=xs.t[R, :], in0=ang.t[R, :], scalar1=off, scalar2=None, op0=ALU.add), [ang], [xs])
                DVE(lambda e: e.tensor_scalar(out=kf.t[R, :], in0=xs.t[R, :], scalar1=1.0 / TWO_PI, scalar2=None, op0=ALU.mult), [xs], [kf])
                DVE(lambda e: e.tensor_copy(out=ki.t[R, :], in_=kf.t[R, :]), [kf], [ki])
                DVE(lambda e: e.tensor_copy(out=kf.t[R, :], in_=ki.t[R, :]), [ki], [kf])
                DVE(lambda e: e.scalar_tensor_tensor(out=xs.t[R, :], in0=kf.t[R, :], scalar=-C_HI, in1=xs.t[R, :], op0=ALU.mult, op1=ALU.add),
                    [kf, xs], [xs])
                DVE(lambda e: e.scalar_tensor_tensor(out=xs.t[R, :], in0=kf.t[R, :], scalar=-C_LO, in1=xs.t[R, :], op0=ALU.mult, op1=ALU.add),
                    [kf, xs], [xs])
                DVE(lambda e: e.tensor_scalar(out=kf.t[R, :], in0=xs.t[R, :], scalar1=PI, scalar2=-TWO_PI, op0=ALU.is_gt, op1=ALU.mult), [xs], [kf])
                DVE(lambda e: e.tensor_tensor(out=xs.t[R, :], in0=xs.t[R, :], in1=kf.t[R, :], op=ALU.add), [xs, kf], [xs])
                DVE(lambda e: e.tensor_scalar(out=kf.t[R, :], in0=xs.t[R, :], scalar1=-PI, scalar2=TWO_PI, op0=ALU.is_lt, op1=ALU.mult), [xs], [kf])
                DVE(lambda e: e.tensor_tensor(out=xs.t[R, :], in0=xs.t[R, :], in1=kf.t[R, :], op=ALU.add), [xs, kf], [xs])
                ACT(lambda e: e.activation(out=xs.t[R, :], in_=xs.t[R, :], func=AF.Sin), [xs], [xs])
                if which == 1:
                    DVE(lambda e, ot=ot: e.tensor_scalar(out=ot.t[R, :], in0=xs.t[R, :], scalar1=cvec.t[R, sgn_col:sgn_col + 1], scalar2=None,
                                                         op0=ALU.mult), [xs, cvec], [ot])
                else:
                    DVE(lambda e, ot=ot: e.tensor_copy(out=ot.t[R, :], in_=xs.t[R, :]), [xs], [ot])
                LD(lambda e, ot=ot, dstS=dstS: e.dma_start(out=dstS[0:nrows, cs], in_=ot.t[R, :]), [ot], [tabS_b])

        def rope_tables(st, nrows, inv_col, sgn_col, cos_t, sin_t):
            with ExitStack() as tmp:
                posi = alloc(tmp, "posi", [128, 1024], I32)
                xs = alloc(tmp, "rt_xs", [128, 1024], F32)
                kf = alloc(tmp, "rt_kf", [128, 1024], F32)
                ki = alloc(tmp, "rt_ki", [128, 1024], I32)
                ang = alloc(tmp, "rt_ang", [128, 1024], F32)
                R = slice(0, nrows)
                for c in range(4):
                    cs = slice(c * 1024, (c + 1) * 1024)
                    LD(lambda e, cs=cs: e.dma_start(out=posi.t[R, :], in_=pos_l[0:1, cs].to_broadcast([nrows, 1024])), [], [posi])
                    DVE(lambda e: e.tensor_copy(out=ang.t[R, :], in_=posi.t[R, :]), [posi], [ang])
                    DVE(lambda e: e.tensor_scalar(out=ang.t[R, :], in0=ang.t[R, :], scalar1=cvec.t[R, inv_col:inv_col + 1], scalar2=None,
                                                  op0=ALU.mult), [ang, cvec], [ang])
                    for which in range(2):
                        off = 0.0 if which == 1 else PI / 2.0
                        dst = sin_t if which == 1 else cos_t
                        DVE(lambda e, off=off: e.tensor_scalar(out=xs.t[R, :], in0=ang.t[R, :], scalar1=off, scalar2=None, op0=ALU.add),
                            [ang], [xs])
                        DVE(lambda e: e.tensor_scalar(out=kf.t[R, :], in0=xs.t[R, :], scalar1=1.0 / TWO_PI, scalar2=None, op0=ALU.mult),
                            [xs], [kf])
                        DVE(lambda e: e.tensor_copy(out=ki.t[R, :], in_=kf.t[R, :]), [kf], [ki])
                        DVE(lambda e: e.tensor_copy(out=kf.t[R, :], in_=ki.t[R, :]), [ki], [kf])
                        DVE(lambda e: e.scalar_tensor_tensor(out=xs.t[R, :], in0=kf.t[R, :], scalar=-C_HI, in1=xs.t[R, :], op0=ALU.mult,
                                                             op1=ALU.add), [kf, xs], [xs])
                        DVE(lambda e: e.scalar_tensor_tensor(out=xs.t[R, :], in0=kf.t[R, :], scalar=-C_LO, in1=xs.t[R, :], op0=ALU.mult,
                                                             op1=ALU.add), [kf, xs], [xs])
                        DVE(lambda e: e.tensor_scalar(out=kf.t[R, :], in0=xs.t[R, :], scalar1=PI, scalar2=-TWO_PI, op0=ALU.is_gt,
                                                      op1=ALU.mult), [xs], [kf])
                        DVE(lambda e: e.tensor_tensor(out=xs.t[R, :], in0=xs.t[R, :], in1=kf.t[R, :], op=ALU.add), [xs, kf], [xs])
                        DVE(lambda e: e.tensor_scalar(out=kf.t[R, :], in0=xs.t[R, :], scalar1=-PI, scalar2=TWO_PI, op0=ALU.is_lt,
                                                      op1=ALU.mult), [xs], [kf])
                        DVE(lambda e: e.tensor_tensor(out=xs.t[R, :], in0=xs.t[R, :], in1=kf.t[R, :], op=ALU.add), [xs, kf], [xs])
                        ACT(lambda e: e.activation(out=xs.t[R, :], in_=xs.t[R, :], func=AF.Sin), [xs], [xs])
                        if which == 1:
                            DVE(lambda e, cs=cs, dst=dst: e.tensor_scalar(out=dst.t[R, cs], in0=xs.t[R, :], scalar1=cvec.t[R, sgn_col:sgn_col + 1],
                                                                         scalar2=None, op0=ALU.mult), [xs, cvec], [dst])
                        else:
                            DVE(lambda e, cs=cs, dst=dst: e.tensor_copy(out=dst.t[R, cs], in_=xs.t[R, :]), [xs], [dst])
                fw.flush()

        with ExitStack() as ph:
            ccol = alloc(ph, "ccol", [128, 8], F32)
            scl = alloc(ph, "scl", [128, 8], F32)
            ones = alloc(ph, "ones", [128, 128], F32)
            scb = alloc(ph, "scb", [128, 8, 128], F32)
            mod = alloc(ph, "mod", [128, 9 * D], F32)
            wm = [alloc(ph, "wm%d" % i, [128, 8, 512], F32) for i in range(2)]
            bm = [alloc(ph, "bm%d" % i, [128, 512], F32) for i in range(2)]
            gt = [alloc(ph, "gt%d" % i, [128, D], F32) for i in range(2)]
            tp_ = {"posi": alloc(ph, "posi", [128, 2048], I32), "xs": alloc(ph, "rt_xs", [128, 2048], F32),
                   "kf": alloc(ph, "rt_kf", [128, 2048], F32), "ki": alloc(ph, "rt_ki", [128, 2048], I32),
                   "ang": alloc(ph, "rt_ang", [128, 2048], F32),
                   "ot": [alloc(ph, "rt_ot%d" % i, [128, 2048], BF16) for i in range(2)]}
            tjobs = [(128, 0, 1, cos64S, sin64S, c) for c in range(2)] + [(96, 2, 3, cos32S, sin32S, c) for c in range(2)]
            LD(lambda e: e.dma_start(out=ccol.t[:], in_=c_col[:, :]), [], [ccol])
            ACT(lambda e: e.activation(out=scl.t[:], in_=ccol.t[:], func=AF.Silu), [ccol], [scl])
            DVE(lambda e: e.memset(ones.t[:], 1.0), [], [ones])
            for kc in range(8):
                DVE(lambda e, kc=kc: e.tensor_scalar(out=scb.t[:, kc, :], in0=ones.t[:], scalar1=scl.t[:, kc:kc + 1], scalar2=None,
                                                     op0=ALU.mult), [ones, scl], [scb])
            for g in range(18):
                w = wm[g % 2]
                b = bm[g % 2]
                LD(lambda e, w=w, g=g: e.dma_start(out=w.t[:], in_=wmod_t[g]), [], [w])
                LD(lambda e, b=b, g=g: e.dma_start(out=b.t[:], in_=bmod[0:1, g * 512:(g + 1) * 512].to_broadcast([128, 512])), [], [b])
                ps = PSF()
                for kc in range(8):
                    PE(lambda e, kc=kc, w=w, ps=ps: e.matmul(ps.t[:], scb.t[:, kc, :], w.t[:, kc, :], start=(kc == 0), stop=(kc == 7)),
                       [scb, w], [ps])
                DVE(lambda e, g=g, ps=ps, b=b: e.tensor_tensor(out=mod.t[:, g * 512:(g + 1) * 512], in0=ps.t[:], in1=b.t[:], op=ALU.add),
                    [ps, b], [mod])
                if g % 4 == 0 and tjobs:
                    rope_chunk(tp_, *tjobs.pop(0))
            for i in range(3):
                coef = 1.0 if i == 1 else 0.5
                ga, gb = gt[0], gt[1]
                LD(lambda e, i=i, ga=ga: e.dma_start(out=ga.t[:], in_=gv[2 * i:2 * i + 1, :].to_broadcast([128, D])), [], [ga])
                LD(lambda e, i=i, gb=gb: e.dma_start(out=gb.t[:], in_=gv[2 * i + 1:2 * i + 2, :].to_broadcast([128, D])), [], [gb])
                s0 = (3 * i + 1) * D
                g0 = (3 * i + 2) * D
                DVE(lambda e, s0=s0, ga=ga: e.scalar_tensor_tensor(out=mod.t[:, s0:s0 + D], in0=mod.t[:, s0:s0 + D], scalar=1.0, in1=ga.t[:],
                                                                   op0=ALU.add, op1=ALU.mult), [mod, ga], [mod])
                DVE(lambda e, g0=g0, gb=gb, coef=coef: e.scalar_tensor_tensor(out=mod.t[:, g0:g0 + D], in0=mod.t[:, g0:g0 + D], scalar=coef,
                                                                              in1=gb.t[:], op0=ALU.mult, op1=ALU.mult), [mod, gb], [mod])
            modS_b = Buf("modS")
            LD(lambda e: e.dma_start(out=modS[:, :], in_=mod.t[:]), [mod], [modS_b])
            while tjobs:
                rope_chunk(tp_, *tjobs.pop(0))
            fw.wait_all("sp", [modS_b, tabS_b])
            fw.flush()

        x1S_b = Buf("x1S")
        uTS_b = Buf("uTS")
        x2S_b = Buf("x2S")
        h2TS_b = Buf("h2TS")
        obTS_b = Buf("obTS")
        oaTS_b = Buf("oaTS")
        out_b = Buf("out")

        def load_mod(st, name, idx):
            t = alloc(st, name, [128, D], F32)
            LD(lambda e: e.dma_start(out=t.t[:], in_=modS[:, idx * D:(idx + 1) * D]), [modS_b], [t])
            return t

        def ffn_phase(first):
            ntok = T if first else 2048
            ngrp = ntok // 1024
            wgu = wgu1 if first else wgu2
            wdd = wd1 if first else wd2
            with ExitStack() as ph:
                wd = alloc(ph, "wd", [128, NF, D], BF16)
                for f0 in range(0, NF, 2):
                    LDC(lambda e, f0=f0: e.dma_start(out=wd.t[:, f0:f0 + 2, :], in_=wdd[:, f0:f0 + 2, :]), [], [wd])
                wring = [alloc(ph, "wring%d" % i, [128, 2, 8, 128], BF16) for i in range(4)]
                hTs = [alloc(ph, "hT%d" % i, [128, 8, 1024], BF16) for i in range(2)]
                aT = alloc(ph, "aT", [128, NF, 1024], BF16)
                xring = [alloc(ph, "xr%d" % i, [128, D], F32) for i in range(2)]
                tmpf = alloc(ph, "tmpf", [128, D], F32)
                junk = alloc(ph, "junk", [128, D], BF16)
                hbr = [alloc(ph, "hb%d" % i, [128, D], BF16) for i in range(2)]
                sg = [alloc(ph, "sg%d" % i, [128, 512], F32) for i in range(2)]
                ssr = [alloc(ph, "ss%d" % i, [128, 2], F32) for i in range(4)]
                rsr = [alloc(ph, "rs%d" % i, [128, 1], F32) for i in range(4)]
                if first:
                    sh1 = load_mod(ph, "sh1", 0)
                    gm1 = load_mod(ph, "gm1", 1)
                    cp = load_mod(ph, "cp1", 2)
                    sh2 = load_mod(ph, "sh2", 3)
                    gm2 = load_mod(ph, "gm2", 4)
                    x1r = [alloc(ph, "x1t%d" % i, [128, D], F32) for i in range(2)]
                    uTt = [alloc(ph, "uTt%d" % i, [128, 8, 128], BF16) for i in range(2)]
                else:
                    cp = load_mod(ph, "cp3", 8)
                    x1r = [alloc(ph, "x1t%d" % i, [128, D], F32) for i in range(2)]
                ssi = {"i": 0}

                def sumsq(src_aps, src_tns):
                    ss = ssr[ssi["i"] % 4]
                    ssi["i"] += 1
                    DVE(lambda e: e.memset(ss.t[:], 0.0), [], [ss])
                    for i, ap in enumerate(src_aps):
                        w = ap.shape[-1]
                        ACT(lambda e, i=i, ap=ap, w=w: e.activation(out=junk.t[:, 0:w], in_=ap, func=AF.Square, accum_out=ss.t[:, i:i + 1]),
                            src_tns, [junk, ss])
                    if len(src_aps) == 2:
                        DVE(lambda e: e.tensor_tensor(out=ss.t[:, 0:1], in0=ss.t[:, 0:1], in1=ss.t[:, 1:2], op=ALU.add), [ss], [ss])
                    return ss

                xi = {"i": 0}
                hbi = {"i": 0}

                def next_x():
                    xt = xring[xi["i"] % 2]
                    xi["i"] += 1
                    return xt

                def next_hb():
                    h_ = hbr[hbi["i"] % 2]
                    hbi["i"] += 1
                    return h_

                def chain_h(G, t):
                    tile = G * 8 + t
                    xt = next_x()
                    LD(lambda e, xt=xt, tile=tile: e.dma_start(out=xt.t[:], in_=x_l[tile * 128:(tile + 1) * 128, :]), [], [xt])
                    ss = sumsq([xt.t[:]], [xt])
                    rs = rstd_of(rsr, ss.t[:, 0:1], ss, D)
                    DVE(lambda e, xt=xt, rs=rs: e.scalar_tensor_tensor(out=tmpf.t[:], in0=xt.t[:], scalar=rs.t[:, 0:1], in1=gm1.t[:],
                                                                       op0=ALU.mult, op1=ALU.mult), [xt, rs, gm1], [tmpf])
                    h_ = next_hb()
                    DVE(lambda e, h_=h_: e.tensor_tensor(out=h_.t[:], in0=tmpf.t[:], in1=sh1.t[:], op=ALU.add), [tmpf, sh1], [h_])
                    return h_

                def trans_h(G, t, h_):
                    hT_ = hTs[G % 2]
                    transpose_to(h_, 8, hT_.t[:, :, t * 128:(t + 1) * 128], hT_)

                def load_hT(G):
                    hT_ = hTs[G % 2]
                    LD(lambda e, G=G, hT_=hT_: e.dma_start(out=hT_.t[:], in_=h2TS[:, :, G * 1024:(G + 1) * 1024]), [h2TS_b], [hT_])

                if first:
                    prev = None
                    for t in range(8):
                        h_ = chain_h(0, t)
                        if prev is not None:
                            trans_h(0, prev[0], prev[1])
                        prev = (t, h_)
                    trans_h(0, prev[0], prev[1])
                else:
                    load_hT(0)
                SLOTS = {1: 0, 3: 1, 6: 2, 8: 3, 11: 4, 13: 5, 16: 6, 18: 7}
                for G in range(ngrp):
                    hT = hTs[G % 2]
                    prev = None
                    for f in range(NF):
                        wr = wring[f % 4]
                        LDC(lambda e, wr=wr, f=f: e.dma_start(out=wr.t[:], in_=wgu[f]), [], [wr])
                        for half in range(2):
                            hs = slice(half * 512, (half + 1) * 512)
                            gp = PSF()
                            up = PSF()
                            for kc in range(8):
                                PE(lambda e, kc=kc, gp=gp, wr=wr, hs=hs, hT=hT: e.matmul(gp.t[:], wr.t[:, 0, kc, :], hT.t[:, kc, hs], start=(kc == 0),
                                                                                          stop=(kc == 7)), [wr, hT], [gp])
                            for kc in range(8):
                                PE(lambda e, kc=kc, up=up, wr=wr, hs=hs, hT=hT: e.matmul(up.t[:], wr.t[:, 1, kc, :], hT.t[:, kc, hs], start=(kc == 0),
                                                                                          stop=(kc == 7)), [wr, hT], [up])
                            s_ = sg[half]
                            ACT(lambda e, s_=s_, gp=gp: e.activation(out=s_.t[:], in_=gp.t[:], func=AF.Silu), [gp], [s_])
                            DVE(lambda e, s_=s_, up=up, f=f, hs=hs: e.tensor_tensor(out=aT.t[:, f, hs], in0=s_.t[:], in1=up.t[:], op=ALU.mult),
                                [s_, up], [aT])
                        if G + 1 < ngrp:
                            if first and f in SLOTS:
                                t = SLOTS[f]
                                h_ = chain_h(G + 1, t)
                                if prev is not None:
                                    trans_h(G + 1, prev[0], prev[1])
                                prev = (t, h_)
                            if (not first) and f == 0:
                                load_hT(G + 1)
                    if prev is not None:
                        trans_h(G + 1, prev[0], prev[1])
                    pend = None
                    for t in range(8):
                        tile = G * 8 + t
                        xt = next_x()
                        if first:
                            LD(lambda e, xt=xt, tile=tile: e.dma_start(out=xt.t[:], in_=x_l[tile * 128:(tile + 1) * 128, :]), [], [xt])
                        else:
                            LD(lambda e, xt=xt, tile=tile: e.dma_start(out=xt.t[:], in_=x2S[tile * 128:(tile + 1) * 128, :]), [x2S_b], [xt])
                        yps = [PSF(), PSF()]
                        for half in range(2):
                            yp = yps[half]
                            for f in range(NF):
                                PE(lambda e, f=f, yp=yp, t=t, half=half: e.matmul(yp.t[:], aT.t[:, f, t * 128:(t + 1) * 128],
                                                                                   wd.t[:, f, half * 512:(half + 1) * 512], start=(f == 0),
                                                                                   stop=(f == NF - 1)), [aT, wd], [yp])
                        if pend is not None:
                            ptile, ph_ = pend
                            ut = uTt[ptile % 2]
                            transpose_to(ph_, 8, ut.t[:], ut)
                            LD(lambda e, ut=ut, ptile=ptile: e.dma_start(out=uTS[:, :, ptile * 128:(ptile + 1) * 128], in_=ut.t[:]), [ut], [uTS_b])
                            pend = None
                        ss = sumsq([yps[0].t[:], yps[1].t[:]], yps)
                        rs = rstd_of(rsr, ss.t[:, 0:1], ss, D)
                        for half in range(2):
                            hs = slice(half * 512, (half + 1) * 512)
                            DVE(lambda e, half=half, hs=hs, rs=rs, yp=yps[half]: e.scalar_tensor_tensor(
                                out=tmpf.t[:, hs], in0=yp.t[:], scalar=rs.t[:, 0:1], in1=cp.t[:, hs], op0=ALU.mult, op1=ALU.mult),
                                [yps[half], rs, cp], [tmpf])
                        x1t = x1r[t % 2]
                        DVE(lambda e, xt=xt, x1t=x1t: e.tensor_tensor(out=x1t.t[:], in0=tmpf.t[:], in1=xt.t[:], op=ALU.add), [tmpf, xt], [x1t])
                        if not first:
                            LD(lambda e, tile=tile, x1t=x1t: e.dma_start(out=out_l[tile * 128:(tile + 1) * 128, :], in_=x1t.t[:]), [x1t], [out_b])
                            continue
                        if tile >= 16:
                            LD(lambda e, tile=tile, x1t=x1t: e.dma_start(out=x1S[(tile - 16) * 128:(tile - 15) * 128, :], in_=x1t.t[:]), [x1t], [x1S_b])
                        ss = sumsq([x1t.t[:]], [x1t])
                        rs = rstd_of(rsr, ss.t[:, 0:1], ss, D)
                        DVE(lambda e, rs=rs, x1t=x1t: e.scalar_tensor_tensor(out=tmpf.t[:], in0=x1t.t[:], scalar=rs.t[:, 0:1], in1=gm2.t[:],
                                                                             op0=ALU.mult, op1=ALU.mult), [x1t, rs, gm2], [tmpf])
                        h_ = next_hb()
                        DVE(lambda e, h_=h_: e.tensor_tensor(out=h_.t[:], in0=tmpf.t[:], in1=sh2.t[:], op=ALU.add), [tmpf, sh2], [h_])
                        pend = (tile, h_)
                    if pend is not None:
                        ptile, ph_ = pend
                        ut = uTt[ptile % 2]
                        transpose_to(ph_, 8, ut.t[:], ut)
                        LD(lambda e, ut=ut, ptile=ptile: e.dma_start(out=uTS[:, :, ptile * 128:(ptile + 1) * 128], in_=ut.t[:]), [ut], [uTS_b])
                        pend = None
                    if G % 2 == 1:
                        fw.flush()
                fw.wait_all("sp", [x1S_b, uTS_b, out_b])
                fw.flush()

        ffn_phase(True)

        with ExitStack() as ph:
            cos64 = alloc(ph, "cos64", [128, T], BF16)
            sin64 = alloc(ph, "sin64", [128, T], BF16)
            LD(lambda e: e.dma_start(out=cos64.t[:], in_=cos64S[:, :]), [tabS_b], [cos64])
            LD(lambda e: e.dma_start(out=sin64.t[:], in_=sin64S[:, :]), [tabS_b], [sin64])
            acc = alloc(ph, "acc", [128, 4, 2048], F32)
            tri = alloc(ph, "tri", [128, 256], BF16)
            LDC(lambda e: e.dma_start(out=tri.t[:], in_=tri_d[:, :]), [], [tri])
            gs = ExitStack()
            wdl = alloc(gs, "wdl", [128, 8, 1280], BF16)
            KT = alloc(gs, "KT", [128, 2, T], BF16)
            QT = alloc(gs, "QT", [128, 4, 2048], BF16)
            VA = alloc(gs, "VA", [128, NT, 4, 128], BF16)
            uTh = alloc(gs, "uTh", [128, 8, 2048], BF16)
            t1 = [alloc(gs, "t1_%d" % i, [128, 512], F32) for i in range(2)]
            t2 = [alloc(gs, "t2_%d" % i, [128, 512], F32) for i in range(2)]
            Eb = [alloc(gs, "Eb%d" % i, [128, 4, 128], BF16) for i in range(2)]
            PTb = [alloc(gs, "PTb%d" % i, [128, 4, 128], BF16) for i in range(2)]
            for g, d in enumerate((1, 4, 16)):
                LDC(lambda e, g=g: e.dma_start(out=wdl.t[:], in_=w_dil[g]), [], [wdl])
                DVE(lambda e: e.memset(VA.t[:], 1.0), [], [VA])
                DVE(lambda e: e.memset(QT.t[:], 0.0), [], [QT])
                ci = 0
                for hf in range(2):
                    LD(lambda e, hf=hf: e.dma_start(out=uTh.t[:], in_=uTS[:, :, hf * 2048:(hf + 1) * 2048]), [uTS_b], [uTh])
                    for c in range(4):
                        t0 = hf * 2048 + c * 512
                        cs = slice(c * 512, (c + 1) * 512)
                        jobs = [("k", 512, 768)]
                        if hf == 1:
                            jobs.append(("q", 0, 256))
                        for (nm, c0, c1) in jobs:
                            for pp in range(2):
                                pa = PSF()
                                pr = PSF()
                                for kc in range(8):
                                    PE(lambda e, kc=kc, pa=pa, pp=pp, c0=c0, cs=cs: e.matmul(pa.t[:, :], wdl.t[:, kc, c0 + pp * 128:c0 + (pp + 1) * 128],
                                                                                           uTh.t[:, kc, cs], start=(kc == 0), stop=(kc == 7)),
                                       [wdl, uTh], [pa])
                                for kc in range(8):
                                    PE(lambda e, kc=kc, pr=pr, pp=pp, c1=c1, cs=cs: e.matmul(pr.t[:, :], wdl.t[:, kc, c1 + pp * 128:c1 + (pp + 1) * 128],
                                                                                           uTh.t[:, kc, cs], start=(kc == 0), stop=(kc == 7)),
                                       [wdl, uTh], [pr])
                                a1 = t1[ci % 2]
                                a2 = t2[ci % 2]
                                ci += 1
                                DVE(lambda e, a1=a1, pa=pa, t0=t0: e.tensor_tensor(out=a1.t[:], in0=pa.t[:, :], in1=cos64.t[:, t0:t0 + 512],
                                                                                  op=ALU.mult), [pa, cos64], [a1])
                                DVE(lambda e, a2=a2, pr=pr, t0=t0: e.tensor_tensor(out=a2.t[:], in0=pr.t[:, :], in1=sin64.t[:, t0:t0 + 512],
                                                                                  op=ALU.mult), [pr, sin64], [a2])
                                if nm == "k":
                                    dview = KT.t[:, pp, :].rearrange("p (r m) -> p m r", r=d)[:, t0 // d:(t0 + 512) // d, :]
                                    POOL(lambda e, a1=a1, a2=a2, dview=dview: e.tensor_tensor(
                                        out=dview, in0=a1.t[:].rearrange("p (m r) -> p m r", r=d), in1=a2.t[:].rearrange("p (m r) -> p m r", r=d),
                                        op=ALU.add), [a1, a2], [KT])
                                else:
                                    n0 = c * 512
                                    for hh in range(2):
                                        R_ = slice(hh * 64, (hh + 1) * 64)
                                        dview = QT.t[R_, 2 * pp + hh, :].rearrange("p (r m) -> p m r", r=d)[:, n0 // d:(n0 + 512) // d, :]
                                        POOL(lambda e, a1=a1, a2=a2, dview=dview, R_=R_: e.tensor_tensor(
                                            out=dview, in0=a1.t[R_, :].rearrange("p (m r) -> p m r", r=d), in1=a2.t[R_, :].rearrange("p (m r) -> p m r", r=d),
                                            op=ALU.add), [a1, a2], [QT])
                    nb = 16 // d
                    for r in range(d):
                        for mbl in range(nb):
                            blk = r * (32 // d) + hf * nb + mbl
                            st0 = r + d * 128 * mbl
                            sl = slice(st0, st0 + d * 127 + 1, d)
                            vp = PSF()
                            for kc in range(8):
                                PE(lambda e, kc=kc, vp=vp, sl=sl: e.matmul(vp.t[:, 0:256], uTh.t[:, kc, sl], wdl.t[:, kc, 1024:1280],
                                                                          start=(kc == 0), stop=(kc == 7)), [uTh, wdl], [vp])
                            kvc = hf * 16
                            ACT(lambda e, vp=vp, blk=blk, kvc=kvc: e.activation(out=VA.t[:, blk, :, 0:64],
                                                                                 in_=vp.t[:, 0:256].rearrange("p (h e) -> p h e", h=4),
                                                                                 func=AF.Copy, scale=kvalid.t[:, kvc:kvc + 1]), [vp, kvalid], [VA])
                            if hf == 0:
                                DVE(lambda e, blk=blk: e.tensor_scalar(out=VA.t[:, blk, :, 64:128], in0=VA.t[:, blk, :, 64:128],
                                                                       scalar1=kvalid.t[:, 0:1], scalar2=None, op0=ALU.mult), [VA, kvalid], [VA])
                ei = 0
                for r in range(d):
                    for mbl in range(nb):
                        qb = r * nb + mbl
                        blk_same = r * (32 // d) + nb + mbl
                        op_ = PSF()
                        for bi, (blk, m0) in enumerate(((blk_same - 1, 0), (blk_same, 128))):
                            sp_ = PSF()
                            for h in range(4):
                                PE(lambda e, h=h, sp_=sp_, blk=blk, qb=qb: e.matmul(sp_.t[:, h * 128:(h + 1) * 128], KT.t[:, h // 2, blk * 128:(blk + 1) * 128],
                                                                                    QT.t[:, h, qb * 128:(qb + 1) * 128], start=True, stop=True),
                                   [KT, QT], [sp_])
                            E_ = Eb[ei % 2]
                            P_ = PTb[ei % 2]
                            ei += 1
                            ACT(lambda e, E_=E_, sp_=sp_: e.activation(out=E_.t[:], in_=sp_.t[:].rearrange("p (h q) -> p h q", h=4), func=AF.Exp,
                                                                        scale=0.125), [sp_], [E_])
                            POOL(lambda e, E_=E_, P_=P_, m0=m0: e.tensor_tensor(out=P_.t[:], in0=E_.t[:],
                                                                                 in1=tri.t[:, m0:m0 + 128].unsqueeze(1).to_broadcast([128, 4, 128]),
                                                                                 op=ALU.mult), [E_, tri], [P_])
                            for h in range(4):
                                PE(lambda e, h=h, op_=op_, P_=P_, blk=blk, bi=bi: e.matmul(op_.t[:, h * 128:(h + 1) * 128], VA.t[:, blk, h, :], P_.t[:, h, :],
                                                                                           start=(bi == 0 and h == 0), stop=(bi == 1)), [VA, P_], [op_])
                        st0 = r + d * 128 * mbl
                        aview = acc.t[:, :, st0:st0 + d * 127 + 1:d]
                        oview = op_.t[:].rearrange("p (h q) -> p h q", h=4)
                        if g == 0:
                            DVE(lambda e, aview=aview, oview=oview: e.tensor_copy(out=aview, in_=oview), [op_], [acc])
                        else:
                            DVE(lambda e, aview=aview, oview=oview: e.tensor_tensor(out=aview, in0=aview, in1=oview, op=ALU.add), [op_, acc], [acc])
                fw.flush()
            gs.close()
            rd = alloc(ph, "rd", [128, 4, 512], F32)
            rd0 = alloc(ph, "rd0", [64, 4, 512], F32)
            obt = alloc(ph, "obt", [64, 4, 512], BF16)
            for c in range(4):
                cs = slice(c * 512, (c + 1) * 512)
                DVE(lambda e, cs=cs: e.reciprocal(out=rd.t[64:128, :, :], in_=acc.t[64:128, :, cs]), [acc], [rd])
                DVE(lambda e: e.tensor_copy(out=rd0.t[0:64, :, :], in_=rd.t[64:128, :, :]), [rd], [rd0])
                DVE(lambda e, cs=cs: e.tensor_tensor(out=obt.t[:], in0=acc.t[0:64, :, cs], in1=rd0.t[:], op=ALU.mult), [acc, rd0], [obt])
                LD(lambda e, cs=cs: e.dma_start(out=obTS[:, :, cs], in_=obt.t[:]), [obt], [obTS_b])
            fw.wait_all("sp", [obTS_b])
            fw.flush()

        with ExitStack() as ph:
            cosq = alloc(ph, "cosq", [96, 2048], BF16)
            sinq = alloc(ph, "sinq", [96, 2048], BF16)
            kb0 = alloc(ph, "kb0", [128, 1], F32)
            DVE(lambda e: e.tensor_scalar(out=kb0.t[:], in0=kvalid.t[:, 0:1], scalar1=-1.0, scalar2=1.0e30, op0=ALU.add, op1=ALU.mult), [kvalid], [kb0])
            cm = alloc(ph, "cm", [128, 128], F32)
            LD(lambda e: e.dma_start(out=cm.t[:], in_=cm_d[:, :]), [], [cm])
            KH = alloc(ph, "KH", [96, 8, T], BF16)
            VA = alloc(ph, "VA2", [128, NT, 8, 65], BF16)
            KI = alloc(ph, "KI", [32, T], BF16)
            CQNT = alloc(ph, "CQNT", [128, 3, 2048], BF16)
            WSC = alloc(ph, "WSC", [128, 16, 8], F32)
            ssr = [alloc(ph, "bss%d" % i, [128, 2], F32) for i in range(4)]
            rsr = [alloc(ph, "brs%d" % i, [128, 1], F32) for i in range(4)]
            ks = ExitStack()
            cos32 = alloc(ks, "cos32", [96, T], BF16)
            sin32 = alloc(ks, "sin32", [96, T], BF16)
            LD(lambda e: e.dma_start(out=cos32.t[:], in_=cos32S[:, :]), [tabS_b], [cos32])
            LD(lambda e: e.dma_start(out=sin32.t[:], in_=sin32S[:, :]), [tabS_b], [sin32])
            LD(lambda e: e.dma_start(out=cosq.t[:], in_=cos32S[:, 2048:T]), [tabS_b], [cosq])
            LD(lambda e: e.dma_start(out=sinq.t[:], in_=sin32S[:, 2048:T]), [tabS_b], [sinq])
            wdi = alloc(ks, "wdi", [128, 8, 904], BF16)
            wkv = alloc(ks, "wkv", [128, 2, 1024], BF16)
            LDC(lambda e: e.dma_start(out=wdi.t[:], in_=w_dsa_in[:, :, :]), [], [wdi])
            LDC(lambda e: e.dma_start(out=wkv.t[:], in_=w_kv[:, :, :]), [], [wkv])
            gqb = alloc(ks, "gqb", [128, 384], F32)
            gkb = alloc(ks, "gkb", [128, 256], F32)
            LD(lambda e: e.dma_start(out=gqb.t[:], in_=gcq[0:1, :].to_broadcast([128, 384])), [], [gqb])
            LD(lambda e: e.dma_start(out=gkb.t[:], in_=gckv[0:1, :].to_broadcast([128, 256])), [], [gkb])
            if True:
                uTc = [alloc(ks, "uTc%d" % i, [128, 8, 512], BF16) for i in range(2)]
                ckvnT = [alloc(ks, "ckvnT%d" % i, [128, 2, 512], BF16) for i in range(2)]
                junk = alloc(ks, "kjunk", [128, 384], BF16)
                nb16 = alloc(ks, "nb16", [128, 384], BF16)
                r1 = alloc(ks, "r1", [96, 512], F32)
                r2 = alloc(ks, "r2", [96, 512], F32)
                r3 = alloc(ks, "r3", [96, 512], BF16)
                si = 0
                for c in range(8):
                    u = uTc[c % 2]
                    ck = ckvnT[c % 2]
                    cs = slice(c * 512, (c + 1) * 512)
                    LD(lambda e, u=u, cs=cs: e.dma_start(out=u.t[:], in_=uTS[:, :, cs]), [uTS_b], [u])
                    for t in range(4):
                        tile = c * 4 + t
                        ts_ = slice(t * 128, (t + 1) * 128)
                        jobs = [("kv", 0, 256, gkb)]
                        if tile >= 16:
                            jobs.append(("q", 520, 384, gqb))
                        for (nm, c0, wdt, gb_) in jobs:
                            cp_ = PSF()
                            for kc in range(8):
                                PE(lambda e, kc=kc, cp_=cp_, u=u, ts_=ts_, c0=c0, wdt=wdt: e.matmul(cp_.t[:, 0:wdt], u.t[:, kc, ts_], wdi.t[:, kc, c0:c0 + wdt],
                                                                                                 start=(kc == 0), stop=(kc == 7)), [u, wdi], [cp_])
                            ss = ssr[si % 4]
                            si += 1
                            DVE(lambda e, ss=ss: e.memset(ss.t[:], 0.0), [], [ss])
                            ACT(lambda e, ss=ss, cp_=cp_, wdt=wdt: e.activation(out=junk.t[:, 0:wdt], in_=cp_.t[:, 0:wdt], func=AF.Square,
                                                                                 accum_out=ss.t[:, 0:1]), [cp_], [junk, ss])
                            rs = rstd_of(rsr, ss.t[:, 0:1], ss, wdt)
                            DVE(lambda e, rs=rs, cp_=cp_, wdt=wdt, gb_=gb_: e.scalar_tensor_tensor(out=nb16.t[:, 0:wdt], in0=cp_.t[:, 0:wdt], scalar=rs.t[:, 0:1],
                                                                                                 in1=gb_.t[:, 0:wdt], op0=ALU.mult, op1=ALU.mult),
                                [cp_, rs, gb_], [nb16])
                            if nm == "kv":
                                transpose_to(nb16, 2, ck.t[:, :, ts_], ck)
                            else:
                                q0 = (tile - 16) * 128
                                transpose_to(nb16, 3, CQNT.t[:, :, q0:q0 + 128], CQNT)
                        if tile >= 16:
                            wp = PSF()
                            for kc in range(8):
                                PE(lambda e, kc=kc, wp=wp, u=u, ts_=ts_: e.matmul(wp.t[:, 0:8], u.t[:, kc, ts_], wdi.t[:, kc, 512:520], start=(kc == 0),
                                                                                   stop=(kc == 7)), [u, wdi], [wp])
                            ACT(lambda e, wp=wp, tile=tile: e.activation(out=WSC.t[:, tile - 16, :], in_=wp.t[:, 0:8], func=AF.Copy,
                                                                         scale=float(8 ** -0.5 * 32 ** -0.5)), [wp], [WSC])
                        vp = PSF()
                        for rc in range(2):
                            PE(lambda e, rc=rc, vp=vp, ck=ck, ts_=ts_: e.matmul(vp.t[:], ck.t[:, rc, ts_], wkv.t[:, rc, 512:1024], start=(rc == 0),
                                                                                 stop=(rc == 1)), [ck, wkv], [vp])
                        ACT(lambda e, vp=vp, tile=tile: e.activation(out=VA.t[:, tile, :, 0:64], in_=vp.t[:].rearrange("p (h e) -> p h e", h=8),
                                                                     func=AF.Copy, scale=kvalid.t[:, tile:tile + 1]), [vp, kvalid], [VA])
                        DVE(lambda e, tile=tile: e.tensor_scalar(out=VA.t[:, tile, :, 64:65], in0=ones8.t[:, :].unsqueeze(2), scalar1=kvalid.t[:, tile:tile + 1],
                                                                 scalar2=None, op0=ALU.mult), [ones8, kvalid], [VA])
                    for h in range(8):
                        kp = PSF()
                        for rc in range(2):
                            PE(lambda e, rc=rc, kp=kp, ck=ck, h=h: e.matmul(kp.t[0:64, :], wkv.t[:, rc, h * 64:(h + 1) * 64], ck.t[:, rc, :], start=(rc == 0),
                                                                             stop=(rc == 1)), [ck, wkv], [kp])
                        if h % 2 == 0:
                            ACT(lambda e, kp=kp, h=h, cs=cs: e.activation(out=KH.t[0:64, h, cs], in_=kp.t[0:64, :], func=AF.Copy), [kp], [KH])
                        else:
                            DVE(lambda e, kp=kp, h=h, cs=cs: e.tensor_copy(out=KH.t[0:64, h, cs], in_=kp.t[0:64, :]), [kp], [KH])
                    for (c0, c1, R, dst_is_kh) in ((256, 352, slice(64, 96), True), (448, 480, slice(0, 32), False)):
                        nr = 96 if dst_is_kh else 32
                        pa = PSF()
                        pr = PSF()
                        for kc in range(8):
                            PE(lambda e, kc=kc, pa=pa, u=u, c0=c0, nr=nr: e.matmul(pa.t[0:nr, :], wdi.t[:, kc, c0:c0 + nr], u.t[:, kc, :], start=(kc == 0),
                                                                                   stop=(kc == 7)), [u, wdi], [pa])
                        for kc in range(8):
                            PE(lambda e, kc=kc, pr=pr, u=u, c1=c1, nr=nr: e.matmul(pr.t[0:nr, :], wdi.t[:, kc, c1:c1 + nr], u.t[:, kc, :], start=(kc == 0),
                                                                                   stop=(kc == 7)), [u, wdi], [pr])
                        DVE(lambda e, pa=pa, R=R, cs=cs: e.tensor_tensor(out=r1.t[R, :], in0=pa.t[R, :], in1=cos32.t[R, cs], op=ALU.mult), [pa, cos32], [r1])
                        DVE(lambda e, pr=pr, R=R, cs=cs: e.tensor_tensor(out=r2.t[R, :], in0=pr.t[R, :], in1=sin32.t[R, cs], op=ALU.mult), [pr, sin32], [r2])
                        if dst_is_kh:
                            DVE(lambda e, R=R: e.tensor_tensor(out=r3.t[R, :], in0=r1.t[R, :], in1=r2.t[R, :], op=ALU.add), [r1, r2], [r3])
                            DVE(lambda e, R=R, cs=cs: e.tensor_copy(out=KH.t[R, :, cs], in_=r3.t[R, :].unsqueeze(1).to_broadcast([32, 8, 512])), [r3], [KH])
                        else:
                            DVE(lambda e, R=R, cs=cs: e.tensor_tensor(out=KI.t[R, cs], in0=r1.t[R, :], in1=r2.t[R, :], op=ALU.add), [r1, r2], [KI])
                fw.flush()
                ks.close()
            with ExitStack() as ms:
                ctr["n"] = 5
                wq = alloc(ms, "wq", [128, 3, 2048], BF16)
                LDC(lambda e: e.dma_start(out=wq.t[:], in_=w_q[:, :, :]), [], [wq])
                QH = [alloc(ms, "QH%d" % i, [96, 8, 128], BF16) for i in range(2)]
                QI = [alloc(ms, "QI%d" % i, [32, 8, 128], BF16) for i in range(2)]
                q1 = alloc(ms, "q1", [96, 4, 128], F32)
                q2 = alloc(ms, "q2", [96, 4, 128], F32)
                isc = alloc(ms, "isc", [128, T], F32)
                Rb = [alloc(ms, "Rb%d" % i, [128, 512], BF16) for i in range(3)]
                mk = alloc(ms, "mk", [128, T], BF16)
                MTs = [alloc(ms, "MT%d" % i, [128, NT, 128], BF16) for i in range(2)]
                mid = alloc(ms, "mid", [128, 1], F32)
                cnt = alloc(ms, "cnt", [128, BIS_ITERS], F32)
                stp = alloc(ms, "stp", [128, 1], F32)
                PTb = [alloc(ms, "P2_%d" % i, [128, 4, 128], BF16) for i in range(3)]
                rden = alloc(ms, "rden", [128, 8], F32)
                oab = alloc(ms, "oab", [128, 512], BF16)
                oat = [alloc(ms, "oat%d" % i, [128, 4, 128], BF16) for i in range(2)]
                attn_scale = float(96 ** -0.5)
                ri = 0
                ei = 0
                pi_ = 0
                def stageA(qt):
                    nonlocal ri
                    NKB = 17 + qt
                    N = NKB * 128
                    q0 = qt * 128
                    qtok = (16 + qt) * 128
                    qh = QH[qt % 2]
                    qi = QI[qt % 2]
                    MT = MTs[qt % 2]
                    for hg in range(2):
                        pa = PSF()
                        pr = PSF()
                        for hl in range(4):
                            h = hg * 4 + hl
                            for rc in range(3):
                                PE(lambda e, rc=rc, pa=pa, hl=hl, h=h, q0=q0: e.matmul(pa.t[0:96, hl * 128:(hl + 1) * 128], wq.t[:, rc, h * 96:(h + 1) * 96],
                                                                                    CQNT.t[:, rc, q0:q0 + 128], start=(rc == 0), stop=(rc == 2)), [wq, CQNT], [pa])
                            for rc in range(3):
                                PE(lambda e, rc=rc, pr=pr, hl=hl, h=h, q0=q0: e.matmul(pr.t[0:96, hl * 128:(hl + 1) * 128], wq.t[:, rc, 768 + h * 96:768 + (h + 1) * 96],
                                                                                    CQNT.t[:, rc, q0:q0 + 128], start=(rc == 0), stop=(rc == 2)), [wq, CQNT], [pr])
                        hsl = slice(hg * 4, hg * 4 + 4)
                        ACT(lambda e, pa=pa, qh=qh, hsl=hsl: e.activation(out=qh.t[0:64, hsl, :], in_=pa.t[0:64, :].rearrange("p (h q) -> p h q", h=4),
                                                                          func=AF.Copy), [pa], [qh])
                        R = slice(64, 96)
                        cosb = cosq.t[R, q0:q0 + 128].unsqueeze(1).to_broadcast([32, 4, 128])
                        sinb = sinq.t[R, q0:q0 + 128].unsqueeze(1).to_broadcast([32, 4, 128])
                        DVE(lambda e, pa=pa, cosb=cosb, R=R: e.tensor_tensor(out=q1.t[R, :, :], in0=pa.t[R, :].rearrange("p (h q) -> p h q", h=4), in1=cosb,
                                                                            op=ALU.mult), [pa, cosq], [q1])
                        DVE(lambda e, pr=pr, sinb=sinb, R=R: e.tensor_tensor(out=q2.t[R, :, :], in0=pr.t[R, :].rearrange("p (h q) -> p h q", h=4), in1=sinb,
                                                                            op=ALU.mult), [pr, sinq], [q2])
                        DVE(lambda e, qh=qh, hsl=hsl, R=R: e.tensor_tensor(out=qh.t[R, hsl, :], in0=q1.t[R, :, :], in1=q2.t[R, :, :], op=ALU.add), [q1, q2], [qh])
                        pa = PSF()
                        pr = PSF()
                        for hl in range(4):
                            h = hg * 4 + hl
                            for rc in range(3):
                                PE(lambda e, rc=rc, pa=pa, hl=hl, h=h, q0=q0: e.matmul(pa.t[0:32, hl * 128:(hl + 1) * 128], wq.t[:, rc, 1536 + h * 32:1536 + (h + 1) * 32],
                                                                                    CQNT.t[:, rc, q0:q0 + 128], start=(rc == 0), stop=(rc == 2)), [wq, CQNT], [pa])
                            for rc in range(3):
                                PE(lambda e, rc=rc, pr=pr, hl=hl, h=h, q0=q0: e.matmul(pr.t[0:32, hl * 128:(hl + 1) * 128], wq.t[:, rc, 1792 + h * 32:1792 + (h + 1) * 32],
                                                                                    CQNT.t[:, rc, q0:q0 + 128], start=(rc == 0), stop=(rc == 2)), [wq, CQNT], [pr])
                        R = slice(0, 32)
                        cosb = cosq.t[R, q0:q0 + 128].unsqueeze(1).to_broadcast([32, 4, 128])
                        sinb = sinq.t[R, q0:q0 + 128].unsqueeze(1).to_broadcast([32, 4, 128])
                        DVE(lambda e, pa=pa, cosb=cosb, R=R: e.tensor_tensor(out=q1.t[R, :, :], in0=pa.t[R, :].rearrange("p (h q) -> p h q", h=4), in1=cosb,
                                                                            op=ALU.mult), [pa, cosq], [q1])
                        DVE(lambda e, pr=pr, sinb=sinb, R=R: e.tensor_tensor(out=q2.t[R, :, :], in0=pr.t[R, :].rearrange("p (h q) -> p h q", h=4), in1=sinb,
                                                                            op=ALU.mult), [pr, sinq], [q2])
                        DVE(lambda e, qi=qi, hsl=hsl, R=R: e.tensor_tensor(out=qi.t[R, hsl, :], in0=q1.t[R, :, :], in1=q2.t[R, :, :], op=ALU.add), [q1, q2], [qi])
                    nch = (NKB + 3) // 4
                    for c in range(nch):
                        w_ = min(512, N - c * 512)
                        cs = slice(c * 512, c * 512 + w_)
                        for h in range(8):
                            lp = PSF()
                            PE(lambda e, lp=lp, qi=qi, h=h, cs=cs, w_=w_: e.matmul(lp.t[:, 0:w_], qi.t[0:32, h, :], KI.t[0:32, cs], start=True, stop=True),
                               [qi, KI], [lp])
                            rb = Rb[ri % 3]
                            ri += 1
                            ACT(lambda e, lp=lp, rb=rb, w_=w_: e.activation(out=rb.t[:, 0:w_], in_=lp.t[:, 0:w_], func=AF.Relu), [lp], [rb])
                            if h == 0:
                                sc2 = kb0.t[:, 0:1] if c < 4 else 0.0
                                DVE(lambda e, rb=rb, cs=cs, w_=w_, qt=qt, sc2=sc2: e.tensor_scalar(
                                    out=isc.t[:, cs], in0=rb.t[:, 0:w_], scalar1=WSC.t[:, qt, 0:1], scalar2=sc2, op0=ALU.mult, op1=ALU.add),
                                    [rb, WSC, kb0], [isc])
                            else:
                                DVE(lambda e, rb=rb, h=h, cs=cs, w_=w_, qt=qt: e.scalar_tensor_tensor(
                                    out=isc.t[:, cs], in0=rb.t[:, 0:w_], scalar=WSC.t[:, qt, h:h + 1], in1=isc.t[:, cs], op0=ALU.mult, op1=ALU.add),
                                    [rb, WSC, isc], [isc])
                    dsl = slice(N - 128, N)
                    DVE(lambda e, dsl=dsl: e.tensor_tensor(out=isc.t[:, dsl], in0=isc.t[:, dsl], in1=cm.t[:], op=ALU.add), [isc, cm], [isc])
                    DVE(lambda e: e.memset(mid.t[:], 0.0), [], [mid])
                    DVE(lambda e: e.memset(cnt.t[:], 0.0), [], [cnt])
                    wdt = BIS_W0
                    for it in range(BIS_ITERS):
                        wdt *= 0.5
                        DVE(lambda e, N=N, it=it: e.tensor_scalar(out=mk.t[:, 0:N], in0=isc.t[:, 0:N], scalar1=mid.t[:, 0:1], scalar2=0.0, op0=ALU.is_ge,
                                                                  op1=ALU.add, accum_out=cnt.t[:, it:it + 1]), [isc, mid], [mk, cnt])
                        DVE(lambda e, wdt=wdt, it=it: e.tensor_scalar(out=stp.t[:], in0=cnt.t[:, it:it + 1], scalar1=255.5, scalar2=2.0 * wdt, op0=ALU.is_ge,
                                                                      op1=ALU.mult), [cnt], [stp])
                        DVE(lambda e, wdt=wdt: e.scalar_tensor_tensor(out=mid.t[:], in0=stp.t[:], scalar=-wdt, in1=mid.t[:], op0=ALU.add,
                                                                      op1=ALU.add), [stp, mid], [mid])
                def stageA2(qt):
                    NKB = 17 + qt
                    N = NKB * 128
                    MT = MTs[qt % 2]
                    DVE(lambda e, N=N: e.tensor_scalar(out=mk.t[:, 0:N], in0=isc.t[:, 0:N], scalar1=mid.t[:, 0:1], scalar2=MASK_NEG, op0=ALU.is_lt,
                                                       op1=ALU.mult), [isc, mid], [mk])
                    for k0 in range(0, NKB, 8):
                        nk = min(8, NKB - k0)
                        pb = PSB()
                        for k in range(nk):
                            PE(lambda e, k=k, k0=k0, pb=pb: e.transpose(pb.t[:, k * 128:(k + 1) * 128], mk.t[:, (k0 + k) * 128:(k0 + k + 1) * 128], ident.t[:]),
                               [mk, ident], [pb])
                        ACT(lambda e, pb=pb, k0=k0, nk=nk: e.activation(out=MT.t[:, k0:k0 + nk, :], in_=pb.t[:, 0:nk * 128].rearrange("p (k q) -> p k q", k=nk),
                                                                        func=AF.Copy), [pb], [MT])
                def stageB(qt):
                    nonlocal pi_
                    NKB = 17 + qt
                    q0 = qt * 128
                    qh = QH[qt % 2]
                    MT = MTs[qt % 2]
                    accp = [psf[5], psf[6]]
                    steps = [(kb, hg) for kb in range(NKB) for hg in range(2)]

                    def emit_scores(kb, hg):
                        ks_ = slice(kb * 128, (kb + 1) * 128)
                        sp_ = PSF()
                        for hl in range(4):
                            h = hg * 4 + hl
                            PE(lambda e, sp_=sp_, hl=hl, h=h, ks_=ks_: e.matmul(sp_.t[:, hl * 128:(hl + 1) * 128], KH.t[0:96, h, ks_], qh.t[0:96, h, :],
                                                                                 start=(hl == 0), stop=False), [KH, qh], [sp_])
                        for hl in range(4):
                            PE(lambda e, sp_=sp_, hl=hl, kb=kb: e.matmul(sp_.t[:, hl * 128:(hl + 1) * 128], ident.t[:], MT.t[:, kb, :], start=False, stop=True),
                               [ident, MT], [sp_])
                        return sp_

                    sp_next = emit_scores(*steps[0])
                    for si_, (kb, hg) in enumerate(steps):
                        sp_ = sp_next
                        if si_ + 1 < len(steps):
                            sp_next = emit_scores(*steps[si_ + 1])
                        P_ = PTb[pi_ % 3]
                        pi_ += 1
                        ACT(lambda e, P_=P_, sp_=sp_: e.activation(out=P_.t[:], in_=sp_.t[:].rearrange("p (h q) -> p h q", h=4), func=AF.Exp,
                                                                    scale=attn_scale), [sp_], [P_])
                        ap_ = accp[hg]
                        for hl in range(4):
                            h = hg * 4 + hl
                            PE(lambda e, ap_=ap_, hl=hl, h=h, kb=kb, P_=P_, NKB=NKB: e.matmul(ap_.t[:, hl * 65:(hl + 1) * 65], P_.t[:, hl, :], VA.t[:, kb, h, :],
                                                                                           start=(kb == 0 and hl == 0), stop=(kb == NKB - 1)), [P_, VA], [ap_])
                    for hg in range(2):
                        ap_ = accp[hg]
                        av = ap_.t[:, 0:260].rearrange("p (h e) -> p h e", h=4)
                        DVE(lambda e, av=av, hg=hg: e.reciprocal(out=rden.t[:, hg * 4:(hg + 1) * 4], in_=av[:, :, 64]), [ap_], [rden])
                        for hl in range(4):
                            h = hg * 4 + hl
                            ACT(lambda e, av=av, hl=hl, h=h: e.activation(out=oab.t[:, h * 64:(h + 1) * 64], in_=av[:, hl, 0:64], func=AF.Copy,
                                                                          scale=rden.t[:, h:h + 1]), [ap_, rden], [oab])
                    ot = oat[qt % 2]
                    transpose_to(oab, 4, ot.t[:], ot, eng_copy="dve")
                    LD(lambda e, ot=ot, q0=q0: e.dma_start(out=oaTS[:, :, q0:q0 + 128], in_=ot.t[:]), [ot], [oaTS_b])
                stageA(0)
                stageA2(0)
                for qt in range(16):
                    if qt + 1 < 16:
                        stageA(qt + 1)
                    stageB(qt)
                    if qt + 1 < 16:
                        stageA2(qt + 1)
                    if qt % 2 == 1:
                        fw.flush()
                fw.wait_all("sp", [oaTS_b])
                fw.flush()
                ctr["n"] = 7

        with ExitStack() as ph:
            wg_ = alloc(ph, "wgates", [128, 8, 2048], BF16)
            wua = alloc(ph, "wua", [128, 4, D], BF16)
            wub = alloc(ph, "wub", [64, 4, D], BF16)
            wo_ = alloc(ph, "wo", [128, 8, D], BF16)
            for kc in range(8):
                LDC(lambda e, kc=kc: e.dma_start(out=wg_.t[:, kc, :], in_=w_gates[:, kc, :]), [], [wg_])
            LDC(lambda e: e.dma_start(out=wua.t[:], in_=w_upa[:, :, :]), [], [wua])
            LDC(lambda e: e.dma_start(out=wub.t[:], in_=w_upb[:, :, :]), [], [wub])
            for kc in range(0, 8, 2):
                LDC(lambda e, kc=kc: e.dma_start(out=wo_.t[:, kc:kc + 2, :], in_=w_o[:, kc:kc + 2, :]), [], [wo_])
            cp2 = load_mod(ph, "cp2", 5)
            sh3 = load_mod(ph, "sh3", 6)
            gm3 = load_mod(ph, "gm3", 7)
            uT = [alloc(ph, "muT%d" % i, [128, 8, 128], BF16) for i in range(2)]
            oaT = [alloc(ph, "moa%d" % i, [128, 4, 128], BF16) for i in range(2)]
            obT = [alloc(ph, "mob%d" % i, [64, 4, 128], BF16) for i in range(2)]
            x1r = [alloc(ph, "mx1%d" % i, [128, D], F32) for i in range(2)]
            sga = alloc(ph, "sga", [128, D], F32)
            sgb = alloc(ph, "sgb", [128, D], F32)
            zf = alloc(ph, "zf", [128, D], F32)
            zb = alloc(ph, "zb", [128, D], BF16)
            zT = alloc(ph, "zT", [128, 8, 128], BF16)
            tmpf = alloc(ph, "mtmp", [128, D], F32)
            x2t = alloc(ph, "x2t", [128, D], F32)
            hb = alloc(ph, "mhb", [128, D], BF16)
            h2t = [alloc(ph, "h2t%d" % i, [128, 8, 128], BF16) for i in range(2)]
            junk = alloc(ph, "mjunk", [128, D], BF16)
            ssr = [alloc(ph, "mss%d" % i, [128, 2], F32) for i in range(4)]
            rsr = [alloc(ph, "mrs%d" % i, [128, 1], F32) for i in range(4)]
            si = {"i": 0}
            zbr = [zb, alloc(ph, "zb2", [128, D], BF16)]
            hbr2 = [hb, alloc(ph, "mhb2", [128, D], BF16)]
            st8 = {}

            def S1(qt):
                u = uT[qt % 2]
                oa = oaT[qt % 2]
                ob = obT[qt % 2]
                x1 = x1r[qt % 2]
                zb_ = zbr[qt % 2]
                tok = (16 + qt) * 128
                q0 = qt * 128
                LD(lambda e: e.dma_start(out=u.t[:], in_=uTS[:, :, tok:tok + 128]), [uTS_b], [u])
                LD(lambda e: e.dma_start(out=oa.t[:], in_=oaTS[:, :, q0:q0 + 128]), [oaTS_b], [oa])
                LD(lambda e: e.dma_start(out=ob.t[:], in_=obTS[:, :, q0:q0 + 128]), [obTS_b], [ob])
                LD(lambda e: e.dma_start(out=x1.t[:], in_=x1S[q0:q0 + 128, :]), [x1S_b], [x1])
                for half in range(2):
                    hs = slice(half * 512, (half + 1) * 512)
                    pga = PSF()
                    pgb = PSF()
                    for kc in range(8):
                        PE(lambda e, kc=kc, pga=pga, half=half: e.matmul(pga.t[:], u.t[:, kc, :], wg_.t[:, kc, half * 512:(half + 1) * 512], start=(kc == 0),
                                                                          stop=(kc == 7)), [u, wg_], [pga])
                    for kc in range(8):
                        PE(lambda e, kc=kc, pgb=pgb, half=half: e.matmul(pgb.t[:], u.t[:, kc, :], wg_.t[:, kc, 1024 + half * 512:1024 + (half + 1) * 512],
                                                                          start=(kc == 0), stop=(kc == 7)), [u, wg_], [pgb])
                    ACT(lambda e, pga=pga, hs=hs: e.activation(out=sga.t[:, hs], in_=pga.t[:], func=AF.Sigmoid), [pga], [sga])
                    ACT(lambda e, pgb=pgb, hs=hs: e.activation(out=sgb.t[:, hs], in_=pgb.t[:], func=AF.Sigmoid), [pgb], [sgb])
                    pza = PSF()
                    pzb = PSF()
                    for c4 in range(4):
                        PE(lambda e, c4=c4, pza=pza, hs=hs: e.matmul(pza.t[:], oa.t[:, c4, :], wua.t[:, c4, hs], start=(c4 == 0), stop=(c4 == 3)),
                           [oa, wua], [pza])
                    for c4 in range(4):
                        PE(lambda e, c4=c4, pzb=pzb, hs=hs: e.matmul(pzb.t[:], ob.t[0:64, c4, :], wub.t[0:64, c4, hs], start=(c4 == 0), stop=(c4 == 3)),
                           [ob, wub], [pzb])
                    DVE(lambda e, pza=pza, hs=hs: e.tensor_tensor(out=zf.t[:, hs], in0=sga.t[:, hs], in1=pza.t[:], op=ALU.mult), [sga, pza], [zf])
                    DVE(lambda e, pzb=pzb, hs=hs: e.tensor_tensor(out=tmpf.t[:, hs], in0=sgb.t[:, hs], in1=pzb.t[:], op=ALU.mult), [sgb, pzb], [tmpf])
                    DVE(lambda e, hs=hs: e.tensor_tensor(out=zb_.t[:, hs], in0=zf.t[:, hs], in1=tmpf.t[:, hs], op=ALU.add), [zf, tmpf], [zb_])

            def S2(qt):
                zb_ = zbr[qt % 2]
                transpose_to(zb_, 8, zT.t[:], zT)
                yps = [PSF(), PSF()]
                for half in range(2):
                    yp = yps[half]
                    for kc in range(8):
                        PE(lambda e, kc=kc, yp=yp, half=half: e.matmul(yp.t[:], zT.t[:, kc, :], wo_.t[:, kc, half * 512:(half + 1) * 512], start=(kc == 0),
                                                                        stop=(kc == 7)), [zT, wo_], [yp])
                st8[qt] = yps

            def S3(qt):
                yps = st8.pop(qt)
                x1 = x1r[qt % 2]
                q0 = qt * 128
                ss = ssr[si["i"] % 4]
                si["i"] += 1
                DVE(lambda e: e.memset(ss.t[:], 0.0), [], [ss])
                for half in range(2):
                    ACT(lambda e, half=half, yp=yps[half]: e.activation(out=junk.t[:, 0:512], in_=yp.t[:], func=AF.Square, accum_out=ss.t[:, half:half + 1]),
                        [yps[half]], [junk, ss])
                DVE(lambda e: e.tensor_tensor(out=ss.t[:, 0:1], in0=ss.t[:, 0:1], in1=ss.t[:, 1:2], op=ALU.add), [ss], [ss])
                rs = rstd_of(rsr, ss.t[:, 0:1], ss, D)
                for half in range(2):
                    hs = slice(half * 512, (half + 1) * 512)
                    DVE(lambda e, hs=hs, yp=yps[half]: e.scalar_tensor_tensor(out=tmpf.t[:, hs], in0=yp.t[:], scalar=rs.t[:, 0:1], in1=cp2.t[:, hs],
                                                                              op0=ALU.mult, op1=ALU.mult), [yps[half], rs, cp2], [tmpf])
                DVE(lambda e: e.tensor_tensor(out=x2t.t[:], in0=tmpf.t[:], in1=x1.t[:], op=ALU.add), [tmpf, x1], [x2t])
                LD(lambda e: e.dma_start(out=x2S[q0:q0 + 128, :], in_=x2t.t[:]), [x2t], [x2S_b])
                ss2 = ssr[si["i"] % 4]
                si["i"] += 1
                DVE(lambda e: e.memset(ss2.t[:], 0.0), [], [ss2])
                ACT(lambda e: e.activation(out=junk.t[:], in_=x2t.t[:], func=AF.Square, accum_out=ss2.t[:, 0:1]), [x2t], [junk, ss2])
                rs2 = rstd_of(rsr, ss2.t[:, 0:1], ss2, D)
                DVE(lambda e: e.scalar_tensor_tensor(out=tmpf.t[:], in0=x2t.t[:], scalar=rs2.t[:, 0:1], in1=gm3.t[:], op0=ALU.mult, op1=ALU.mult),
                    [x2t, rs2, gm3], [tmpf])
                hb_ = hbr2[qt % 2]
                DVE(lambda e: e.tensor_tensor(out=hb_.t[:], in0=tmpf.t[:], in1=sh3.t[:], op=ALU.add), [tmpf, sh3], [hb_])

            def S4(qt):
                hb_ = hbr2[qt % 2]
                q0 = qt * 128
                ht = h2t[qt % 2]
                transpose_to(hb_, 8, ht.t[:], ht)
                LD(lambda e: e.dma_start(out=h2TS[:, :, q0:q0 + 128], in_=ht.t[:]), [ht], [h2TS_b])

            S1(0)
            for qt in range(16):
                if qt + 1 < 16:
                    S1(qt + 1)
                S2(qt)
                S3(qt)
                if qt >= 1:
                    S4(qt - 1)
            S4(15)
            fw.wait_all("sp", [x2S_b, h2TS_b])
            fw.flush()

        ffn_phase(False)
    return nc


def _tile_k(w):
    K, N = w.shape
    return np.ascontiguousarray(w.reshape(K // 128, 128, N).transpose(1, 0, 2))


def _swap(w, half):
    return np.concatenate([w[..., half:2 * half], w[..., :half]], axis=-1)


_NC_CACHE = {}


def _prep_shared(inp):
    f = np.float32
    A = {}
    w_mod = inp["w_mod"][0]
    A["wmod_t"] = np.ascontiguousarray(w_mod.reshape(8, 128, 18, 512).transpose(2, 1, 0, 3))
    A["bmod"] = np.ascontiguousarray(inp["b_mod"][0:1])
    A["gv"] = np.ascontiguousarray(np.stack([inp["g_pre_ffn1"][0], inp["g_post_ffn1"][0], inp["g_pre_mix"][0], inp["g_post_mix"][0],
                                             inp["g_pre_ffn2"][0], inp["g_post_ffn2"][0]]).astype(f))
    A["gcq"] = np.ascontiguousarray(inp["g_cq"][0:1])
    A["gckv"] = np.ascontiguousarray(inp["g_ckv"][0:1])
    for i, (g, u, dn) in enumerate((("w_gate1", "w_up1", "w_down1"), ("w_gate2", "w_up2", "w_down2"))):
        wg = inp[g][0].reshape(8, 128, NF, 128)
        wu = inp[u][0].reshape(8, 128, NF, 128)
        wgu = np.stack([wg, wu], 0)
        A["wgu%d" % (i + 1)] = np.ascontiguousarray(wgu.transpose(3, 2, 0, 1, 4))
        A["wd%d" % (i + 1)] = _tile_k(inp[dn][0])
    w_in = inp["w_in"][0]
    z64 = np.zeros((D, 64), f)
    kr = w_in[:, 640:672]
    ki = w_in[:, 672:704]
    cols = [w_in[:, 384:640], np.concatenate([z64, kr], 1), np.concatenate([z64, _swap(kr, 16)], 1), ki, _swap(ki, 16), w_in[:, 704:712],
            w_in[:, 0:384]]
    A["w_dsa_in"] = _tile_k(np.concatenate(cols, 1))
    w_uq = inp["w_uq"][0]
    uq_rot = np.concatenate([np.zeros((384, 8, 64), f), _swap(w_uq[:, :, 64:96], 16)], -1)
    w_iq = inp["w_iq"][0]
    A["w_q"] = _tile_k(np.concatenate([w_uq.reshape(384, 768), uq_rot.reshape(384, 768), w_iq.reshape(384, 256),
                                       _swap(w_iq, 16).reshape(384, 256)], 1))
    A["w_kv"] = _tile_k(np.concatenate([inp["w_uk"][0].reshape(256, 512), inp["w_uv"][0].reshape(256, 512)], 1))
    wd = []
    for g in range(3):
        parts = []
        for j in range(3):
            base = 712 + (j * 3 + g) * 256
            wj = w_in[:, base:base + 256].reshape(D, 4, 64)
            parts.append(wj.reshape(D, 256))
            if j < 2:
                parts.append(_swap(wj, 32).reshape(D, 256))
        wd.append(_tile_k(np.concatenate(parts, 1)))
    A["w_dil"] = np.ascontiguousarray(np.stack(wd, 0))
    A["w_gates"] = _tile_k(w_in[:, 3016:5064])
    A["w_upa"] = _tile_k(inp["w_up_a"][0])
    A["w_upb"] = np.ascontiguousarray(inp["w_up_b"][0].reshape(4, 64, D).transpose(1, 0, 2))
    A["w_o"] = _tile_k(inp["w_o"][0])
    A["ident"] = np.eye(128, dtype=f)
    p = np.arange(128)
    cvec = np.zeros((128, 8), f)
    cvec[:, 0] = THETA ** (-(p % 32).astype(np.float64) / 32.0)
    cvec[:, 1] = np.where((p % 64) < 32, -1.0, 1.0)
    cvec[:, 2] = THETA ** (-(p % 16).astype(np.float64) / 16.0)
    cvec[:, 3] = np.where((p % 32) < 16, -1.0, 1.0)
    A["cvec"] = cvec
    s = p[:, None]
    q = p[None, :]
    A["tri"] = np.concatenate([(s >= q), (s <= q)], 1).astype(f)
    A["cm"] = np.where(p[None, :] <= p[:, None], 0.0, NEG).astype(f)
    return {k: np.ascontiguousarray(v, dtype=f) for k, v in A.items()}


def _run(inputs, debug=False):
    inp = {k: np.asarray(v) for k, v in inputs.items()}
    shared = _prep_shared(inp)
    x = inp["x"].astype(np.float32)
    c = inp["c"].astype(np.float32)
    pos = inp["positions"].astype(np.int32)
    in_maps = []
    for core in range(8):
        b, j = core // 2, core % 2
        m = dict(shared)
        if j == 1:
            xl = x[b]
            pl = pos[b]
            kval = np.ones((128, NT), np.float32)
            kb = np.zeros((1, T), np.float32)
        else:
            xl = np.concatenate([x[b, 2048:], x[b, :2048]], 0)
            pl = np.concatenate([pos[b, 2048:], pos[b, :2048]], 0)
            kval = np.ones((128, NT), np.float32)
            kval[:, :16] = 0.0
            kb = np.zeros((1, T), np.float32)
            kb[:, :2048] = NEG
        m["x_l"] = np.ascontiguousarray(xl)
        m["pos_l"] = np.ascontiguousarray(pl.reshape(1, T))
        m["c_col"] = np.ascontiguousarray(c[b].reshape(8, 128).T)
        m["kvalid"] = kval
        m["kbias"] = kb
        in_maps.append(m)
    key = bool(debug)
    if key not in _NC_CACHE:
        _NC_CACHE[key] = build_program(debug=debug)
    nc = _NC_CACHE[key]
    res = run_bass_kernel_spmd(nc, in_maps, core_ids=list(range(8)))
    out = np.zeros((4, T, D), np.float32)
    for core in range(8):
        b, j = core // 2, core % 2
        o = np.asarray(res.results[core]["out_l"], dtype=np.float32)
        if j == 1:
            out[b, 2048:] = o
        else:
            out[b, :2048] = o
    return out, res


def kernel(**inputs):
    out, _ = _run(inputs, debug=False)
    return out
```
